# Optimizing a Trainium2 kernel written in Bass

```python
import math
import jax, jax.numpy as jnp
from jax import lax
import numpy as np

D_MODEL = 4096
BATCH = 4
SEQ = 2048
DEPTH = 2

DN_ALPHA = (2 * DEPTH) ** 0.25
DN_BETA = (8 * DEPTH) ** -0.25
LN_EPS = 1e-5

D_SSM = D_MODEL
SSM_HEAD_DIM = 64
SSM_HEADS = D_SSM // SSM_HEAD_DIM
SSM_GROUPS = 8
SSM_STATE = 128
SSM_CHUNK = 128
CONV_K = 4
GN = SSM_GROUPS * SSM_STATE
CONV_CH = D_SSM + 2 * GN
POOL_WINDOWS = (2, 4, 8, 16)
N_POOL = len(POOL_WINDOWS)
D_POOL = D_MODEL
POOL_GW = D_POOL // N_POOL
N_IN = D_SSM + CONV_CH + SSM_HEADS + D_POOL
D_MIX = D_SSM + D_POOL

MLA_HEADS = 32
Q_LORA = 1024
KV_LORA = 512
QK_NOPE = 128
QK_ROPE = 64
V_DIM = 128
ROPE_THETA = 10000.0
Q_BLOCK = 128
RMS_EPS = 1e-6

N_EXPERTS = 32
TOP_K = 4
D_EXPERT = 512
SWIGLU_LIMIT = 7.0
SWIGLU_ALPHA = 1.702
MOE_BLOCK = 128

N_EVEN = (DEPTH + 1) // 2
N_ODD = DEPTH // 2

kernel_name = 'hybrid_ssd_pool_mla_moe_deepnorm_adaln'


def layer_norm(x, w, b):
    xf = x.astype(jnp.float32)
    mu = jnp.mean(xf, axis=-1, keepdims=True)
    var = jnp.mean(jnp.square(xf - mu), axis=-1, keepdims=True)
    return ((xf - mu) * lax.rsqrt(var + LN_EPS) * w + b).astype(x.dtype)


def rms_norm(x, w, eps):
    xf = x.astype(jnp.float32)
    return (xf * lax.rsqrt(jnp.mean(jnp.square(xf), -1, keepdims=True) + eps) * w).astype(x.dtype)


def rotary(x, pos):
    half = x.shape[-1] // 2
    inv_freq = ROPE_THETA ** (-jnp.arange(half, dtype=jnp.float32) * 2.0 / x.shape[-1])
    ang = pos.astype(jnp.float32)[:, None] * inv_freq[None, :]
    cos = jnp.cos(ang)[:, None, :]
    sin = jnp.sin(ang)[:, None, :]
    xf = x.astype(jnp.float32)
    x1, x2 = xf[..., 0::2], xf[..., 1::2]
    out = jnp.stack([x1 * cos - x2 * sin, x1 * sin + x2 * cos], axis=-1).reshape(x.shape)
    return out.astype(x.dtype)


def causal_dwconv(u, w, bias):
    s = u.shape[1]
    up = jnp.pad(u, ((0, 0), (CONV_K - 1, 0), (0, 0)))
    return sum(w[k] * up[:, k:k + s] for k in range(CONV_K)) + bias


def ssd_chunked(xs, dt, a, bm, cm):
    b, s, g, r, p = xs.shape
    n = bm.shape[-1]
    nc = s // SSM_CHUNK
    L = SSM_CHUNK
    xdt = (xs * dt[..., None]).reshape(b, nc, L, g, r, p)
    la = (dt * a).reshape(b, nc, L, g, r)
    bm = bm.reshape(b, nc, L, g, n)
    cm = cm.reshape(b, nc, L, g, n)
    cum = jnp.cumsum(la, axis=2)
    causal = jnp.tril(jnp.ones((L, L), bool))[None, None, :, :, None, None]
    seg = cum[:, :, :, None] - cum[:, :, None, :]
    decay = jnp.exp(jnp.where(causal, seg, -jnp.inf))
    cb = jnp.einsum('bclgn,bcsgn->bclsg', cm, bm)
    y_diag = jnp.einsum('bclsgr,bcsgrp->bclgrp', cb[..., None] * decay, xdt)
    to_end = jnp.exp(cum[:, :, -1:] - cum)
    states = jnp.einsum('bclgn,bclgrp->bcgrpn', bm, xdt * to_end[..., None])
    chunk_decay = jnp.exp(cum[:, :, -1])

    def carry_state(h, inp):
        st, dec = inp
        return h * dec[..., None, None] + st, h

    h0 = jnp.zeros((b, g, r, p, n), xdt.dtype)
    _, h_in = lax.scan(carry_state, h0, (jnp.moveaxis(states, 1, 0), jnp.moveaxis(chunk_decay, 1, 0)))
    h_in = jnp.moveaxis(h_in, 0, 1)
    y_off = jnp.einsum('bclgn,bcgrpn->bclgrp', cm, h_in) * jnp.exp(cum)[..., None]
    return (y_diag + y_off).reshape(b, s, g, r, p)


def gated_group_rmsnorm(y, z, w):
    b, s, d = y.shape
    gz = (y * jax.nn.silu(z.astype(jnp.float32))).reshape(b, s, SSM_GROUPS, -1)
    gz = gz * lax.rsqrt(jnp.mean(jnp.square(gz), -1, keepdims=True) + LN_EPS)
    return gz.reshape(b, s, d) * w


def multiscale_pool(u, pool_w, pool_scale):
    b, s, _ = u.shape
    uf = u.astype(jnp.float32)
    cs = jnp.cumsum(uf, axis=1)
    t1 = jnp.arange(1, s + 1, dtype=jnp.float32)[None, :, None]
    groups = []
    for g, win in enumerate(POOL_WINDOWS):
        cg = cs[..., g * POOL_GW:(g + 1) * POOL_GW]
        lag = jnp.pad(cg, ((0, 0), (win, 0), (0, 0)))[:, :s]
        groups.append((cg - lag) / jnp.minimum(t1, float(win)))
    diff = (jnp.concatenate(groups, -1) - uf).astype(u.dtype).reshape(b, s, N_POOL, POOL_GW)
    y = jnp.einsum('bsgc,gcd->bsgd', diff, pool_w).reshape(b, s, D_POOL)
    return y * pool_scale


def ssd_pool_mixer(h, in_proj, conv_w, conv_b, dt_bias, a_log, d_skip, norm_w, pool_w, pool_scale, out_proj):
    b, s, _ = h.shape
    proj = h @ in_proj
    o1 = D_SSM
    o2 = o1 + CONV_CH
    o3 = o2 + SSM_HEADS
    z, xbc, dt, u = proj[..., :o1], proj[..., o1:o2], proj[..., o2:o3], proj[..., o3:]
    xbc = jax.nn.silu(causal_dwconv(xbc, conv_w, conv_b)).astype(jnp.float32)
    r = SSM_HEADS // SSM_GROUPS
    xs = xbc[..., :D_SSM].reshape(b, s, SSM_GROUPS, r, SSM_HEAD_DIM)
    bm = xbc[..., D_SSM:D_SSM + GN].reshape(b, s, SSM_GROUPS, SSM_STATE)
    cm = xbc[..., D_SSM + GN:].reshape(b, s, SSM_GROUPS, SSM_STATE)
    dt = jax.nn.softplus(dt.astype(jnp.float32) + dt_bias.astype(jnp.float32)).reshape(b, s, SSM_GROUPS, r)
    a = -jnp.exp(a_log.astype(jnp.float32)).reshape(SSM_GROUPS, r)
    y = ssd_chunked(xs, dt, a, bm, cm) + d_skip.astype(jnp.float32).reshape(SSM_GROUPS, r)[..., None] * xs
    y = gated_group_rmsnorm(y.reshape(b, s, D_SSM), z, norm_w).astype(h.dtype)
    y_pool = multiscale_pool(u, pool_w, pool_scale).astype(h.dtype)
    return jnp.concatenate([y, y_pool], axis=-1) @ out_proj


def mla_mixer(h, wq_a, q_norm_w, wq_b, wkv_a, kv_norm_w, wkv_b, wo, pos):
    b, s, _ = h.shape
    q = (rms_norm(h @ wq_a, q_norm_w, RMS_EPS) @ wq_b).reshape(b, s, MLA_HEADS, QK_NOPE + QK_ROPE)
    q_nope, q_pe = q[..., :QK_NOPE], rotary(q[..., QK_NOPE:], pos)
    kv = h @ wkv_a
    kv_c = rms_norm(kv[..., :KV_LORA], kv_norm_w, RMS_EPS)
    k_pe = rotary(kv[..., KV_LORA:][:, :, None, :], pos)[:, :, 0]
    kvb = (kv_c @ wkv_b).reshape(b, s, MLA_HEADS, QK_NOPE + V_DIM)
    k_nope, v = kvb[..., :QK_NOPE], kvb[..., QK_NOPE:]
    scale = (QK_NOPE + QK_ROPE) ** -0.5
    nblk = s // Q_BLOCK
    qn = jnp.moveaxis(q_nope.reshape(b, nblk, Q_BLOCK, MLA_HEADS, QK_NOPE), 1, 0)
    qp = jnp.moveaxis(q_pe.reshape(b, nblk, Q_BLOCK, MLA_HEADS, QK_ROPE), 1, 0)
    kpos = jnp.arange(s)

    def attend_block(args):
        qn_b, qp_b, start = args
        sc = jnp.einsum('bqhd,bkhd->bhqk', qn_b, k_nope) + jnp.einsum('bqhr,bkr->bhqk', qp_b, k_pe)
        sc = sc.astype(jnp.float32) * scale
        qpos = start + jnp.arange(Q_BLOCK)
        sc = jnp.where(qpos[:, None] >= kpos[None, :], sc, -jnp.inf)
        pr = jax.nn.softmax(sc, axis=-1).astype(v.dtype)
        return jnp.einsum('bhqk,bkhd->bqhd', pr, v)

    o = lax.map(attend_block, (qn, qp, jnp.arange(nblk) * Q_BLOCK))
    o = jnp.moveaxis(o, 0, 1).reshape(b, s, MLA_HEADS * V_DIM)
    return o @ wo


def moe_ffn(h, router_w, router_b, w_gu, b_gu, w_down, b_down):
    b, s, d = h.shape
    n_tok = b * s
    t = h.reshape(n_tok, d)
    logits = (t @ router_w + router_b).astype(jnp.float32)
    top_v, top_e = lax.top_k(logits, TOP_K)
    top_p = jax.nn.softmax(top_v, axis=-1)
    n_asg = n_tok * TOP_K
    flat_e = top_e.reshape(-1)
    flat_tok = jnp.arange(n_asg) // TOP_K
    flat_p = top_p.reshape(-1)
    order = jnp.argsort(flat_e)
    se, stok, sp = flat_e[order], flat_tok[order], flat_p[order]
    counts = jnp.bincount(flat_e, length=N_EXPERTS)
    nblk_e = (counts + MOE_BLOCK - 1) // MOE_BLOCK
    blk_end = jnp.cumsum(nblk_e)
    row_start = (blk_end - nblk_e) * MOE_BLOCK
    seg_start = jnp.cumsum(counts) - counts
    dest = row_start[se] + jnp.arange(n_asg) - seg_start[se]
    n_blocks = -(-(n_asg + N_EXPERTS * (MOE_BLOCK - 1)) // MOE_BLOCK)
    n_rows = n_blocks * MOE_BLOCK
    row_tok = jnp.zeros((n_rows,), jnp.int32).at[dest].set(stok)
    row_p = jnp.zeros((n_rows,), jnp.float32).at[dest].set(sp)
    blk_expert = jnp.minimum(jnp.searchsorted(blk_end, jnp.arange(n_blocks), side='right'), N_EXPERTS - 1)

    def expert_block(args):
        idx, pe, e = args
        xe = t[idx]
        gu = xe @ w_gu[e] + b_gu[e]
        gate = jnp.minimum(gu[:, 0::2], SWIGLU_LIMIT)
        up = jnp.clip(gu[:, 1::2], -SWIGLU_LIMIT, SWIGLU_LIMIT)
        glu = gate * jax.nn.sigmoid(SWIGLU_ALPHA * gate)
        out = ((up + 1.0) * glu) @ w_down[e] + b_down[e]
        return out.astype(jnp.float32) * pe[:, None]

    yb = lax.map(expert_block, (row_tok.reshape(n_blocks, MOE_BLOCK), row_p.reshape(n_blocks, MOE_BLOCK), blk_expert))
    y = jnp.zeros((n_tok, d), jnp.float32).at[row_tok].add(yb.reshape(n_rows, d))
    return y.astype(h.dtype).reshape(b, s, d)


def setup_inputs(seed: int = 0) -> dict:
    key = jax.random.key(seed)
    ks = iter(jax.random.split(key, 32))
    f32 = jnp.float32
    D = D_MODEL

    def nrm(shape, scale):
        return jax.random.normal(next(ks), shape, f32) * scale

    x = nrm((BATCH, SEQ, D), 1.0)
    c = nrm((BATCH, D), 1.0)
    ada_w = nrm((DEPTH, D, 6 * D), 0.2 * D ** -0.5)
    ada_b = nrm((DEPTH, 6 * D), 0.01)
    ln_w = 1.0 + nrm((DEPTH, 2, D), 0.02)
    ln_b = nrm((DEPTH, 2, D), 0.01)
    in_proj = nrm((N_EVEN, D, N_IN), D ** -0.5)
    conv_w = nrm((N_EVEN, CONV_K, CONV_CH), CONV_K ** -0.5)
    conv_b = nrm((N_EVEN, CONV_CH), 0.01)
    dt0 = jnp.exp(jax.random.uniform(next(ks), (N_EVEN, SSM_HEADS), f32, minval=math.log(1e-3), maxval=math.log(1e-1)))
    dt_bias = dt0 + jnp.log(-jnp.expm1(-dt0))
    a_log = jnp.log(jax.random.uniform(next(ks), (N_EVEN, SSM_HEADS), f32, minval=1.0, maxval=16.0))
    d_skip = 1.0 + nrm((N_EVEN, SSM_HEADS), 0.1)
    ssd_norm_w = 1.0 + nrm((N_EVEN, D_SSM), 0.02)
    pool_w = nrm((N_EVEN, N_POOL, POOL_GW, POOL_GW), POOL_GW ** -0.5)
    pool_scale = 1.0 + nrm((N_EVEN, D_POOL), 0.02)
    out_proj = nrm((N_EVEN, D_MIX, D), DN_BETA * D_MIX ** -0.5)
    wq_a = nrm((N_ODD, D, Q_LORA), D ** -0.5)
    q_norm_w = 1.0 + nrm((N_ODD, Q_LORA), 0.02)
    wq_b = nrm((N_ODD, Q_LORA, MLA_HEADS * (QK_NOPE + QK_ROPE)), Q_LORA ** -0.5)
    wkv_a = nrm((N_ODD, D, KV_LORA + QK_ROPE), D ** -0.5)
    kv_norm_w = 1.0 + nrm((N_ODD, KV_LORA), 0.02)
    wkv_b = nrm((N_ODD, KV_LORA, MLA_HEADS * (QK_NOPE + V_DIM)), KV_LORA ** -0.5)
    wo = nrm((N_ODD, MLA_HEADS * V_DIM, D), DN_BETA * (MLA_HEADS * V_DIM) ** -0.5)
    router_w = nrm((DEPTH, D, N_EXPERTS), D ** -0.5)
    router_b = nrm((DEPTH, N_EXPERTS), 0.01)
    w_gu = nrm((DEPTH, N_EXPERTS, D, 2 * D_EXPERT), D ** -0.5)
    b_gu = nrm((DEPTH, N_EXPERTS, 2 * D_EXPERT), 0.01)
    w_down = nrm((DEPTH, N_EXPERTS, D_EXPERT, D), DN_BETA * D_EXPERT ** -0.5)
    b_down = nrm((DEPTH, N_EXPERTS, D), 0.01)
    return {'x': x, 'c': c, 'ada_w': ada_w, 'ada_b': ada_b, 'ln_w': ln_w, 'ln_b': ln_b,
            'in_proj': in_proj, 'conv_w': conv_w, 'conv_b': conv_b, 'dt_bias': dt_bias, 'a_log': a_log,
            'd_skip': d_skip, 'ssd_norm_w': ssd_norm_w, 'pool_w': pool_w, 'pool_scale': pool_scale,
            'out_proj': out_proj, 'wq_a': wq_a, 'q_norm_w': q_norm_w, 'wq_b': wq_b, 'wkv_a': wkv_a,
            'kv_norm_w': kv_norm_w, 'wkv_b': wkv_b, 'wo': wo, 'router_w': router_w, 'router_b': router_b,
            'w_gu': w_gu, 'b_gu': b_gu, 'w_down': w_down, 'b_down': b_down}


def reference(x, c, ada_w, ada_b, ln_w, ln_b, in_proj, conv_w, conv_b, dt_bias, a_log, d_skip,
              ssd_norm_w, pool_w, pool_scale, out_proj, wq_a, q_norm_w, wq_b, wkv_a, kv_norm_w,
              wkv_b, wo, router_w, router_b, w_gu, b_gu, w_down, b_down):
    pos = jnp.arange(x.shape[1])
    cond = jax.nn.silu(c)
    for i in range(DEPTH):
        mod = (cond @ ada_w[i] + ada_b[i])[:, None, :]
        sh_a, sc_a, g_a, sh_f, sc_f, g_f = jnp.split(mod, 6, axis=-1)
        j = i // 2
        hmix = x * (1.0 + sc_a) + sh_a
        if i % 2 == 0:
            y = ssd_pool_mixer(hmix, in_proj[j], conv_w[j], conv_b[j], dt_bias[j], a_log[j], d_skip[j],
                               ssd_norm_w[j], pool_w[j], pool_scale[j], out_proj[j])
        else:
            y = mla_mixer(hmix, wq_a[j], q_norm_w[j], wq_b[j], wkv_a[j], kv_norm_w[j], wkv_b[j], wo[j], pos)
        x = layer_norm(DN_ALPHA * x + (1.0 + g_a) * y, ln_w[i, 0], ln_b[i, 0])
        hff = x * (1.0 + sc_f) + sh_f
        y = moe_ffn(hff, router_w[i], router_b[i], w_gu[i], b_gu[i], w_down[i], b_down[i])
        x = layer_norm(DN_ALPHA * x + (1.0 + g_f) * y, ln_w[i, 1], ln_b[i, 1])
    return x
```

```python
import numpy as np, contextlib, time
import concourse.bass as bass
import concourse.mybir as mybir
from concourse.bass_utils import run_bass_kernel_spmd

F32 = mybir.dt.float32
BF16 = mybir.dt.bfloat16
I32 = mybir.dt.int32
AF = mybir.ActivationFunctionType
ALU = mybir.AluOpType
AX = mybir.AxisListType


class Tok:
    __slots__ = ("sem", "val", "eng")

    def __init__(self, sem, val, eng):
        self.sem = sem
        self.val = val
        self.eng = eng


class Prog:
    ENGS = ("pe", "act", "dve", "pool", "sp")
    LIMIT = 30000

    def __init__(self, nc, stack):
        self.nc = nc
        self.stack = stack
        self.q = {e: [] for e in self.ENGS}
        self.cur = {}
        self.cnt = {}
        self.nsem = 0
        for e in self.ENGS:
            self._new_eng_sem(e)
        self.lastw = {}
        self.readers = {}
        self.seen = {e: {} for e in self.ENGS}
        self.dsem = {}
        self.dcnt = {}
        self.all_tokens = {}
        self.arena = None
        self.arena_off = 0
        self.arena_marks = []

    def _sem(self, name):
        self.nsem += 1
        return self.stack.enter_context(self.nc.semaphore(name))

    def _new_eng_sem(self, e):
        self.cur[e] = self._sem("s_%s_%d" % (e, self.nsem))
        self.cnt[e] = 0

    def _deps(self, eng, r, w, is_dma):
        deps = []
        for k in r:
            t = self.lastw.get(k)
            if t is not None:
                deps.append(t)
        for k in w:
            t = self.lastw.get(k)
            if t is not None and (t.eng != eng or is_dma or t.eng is None):
                deps.append(t)
            for t in self.readers.get(k, ()):
                if t.eng != eng or is_dma or t.eng is None:
                    deps.append(t)
        out = []
        seen = self.seen[eng]
        for t in deps:
            sid = id(t.sem)
            if seen.get(sid, 0) >= t.val:
                continue
            seen[sid] = t.val
            out.append((t.sem, t.val))
        best = {}
        for s, v in out:
            if id(s) not in best or best[id(s)][1] < v:
                best[id(s)] = (s, v)
        return list(best.values())

    def _commit(self, tok, r, w):
        for k in w:
            self.lastw[k] = tok
            self.readers[k] = []
        for k in r:
            self.readers.setdefault(k, []).append(tok)
        self.all_tokens[id(tok.sem)] = tok

    def op(self, eng, fn, r=(), w=()):
        waits = self._deps(eng, r, w, False)
        if self.cnt[eng] >= self.LIMIT:
            self._new_eng_sem(eng)
        self.cnt[eng] += 1
        tok = Tok(self.cur[eng], self.cnt[eng], eng)
        self.q[eng].append((waits, fn, tok.sem, 1))
        self._commit(tok, r, w)
        return tok

    def _dma_sem(self, semkey, queue):
        if not hasattr(self, "dpool"):
            self.dpool = {}
            self.dfree = {}
        semkey = (queue, semkey)
        pool = self.dpool.setdefault(queue, [])
        free = self.dfree.setdefault(queue, [])
        ent = self.dsem.get(semkey)
        if ent is None or ent[1] >= self.LIMIT:
            ent = None
            while free:
                cand = free.pop()
                if cand[1] < self.LIMIT:
                    ent = cand
                    break
            if ent is None:
                ent = [self._sem("d%d" % self.nsem), 0]
                pool.append(ent)
            self.dsem[semkey] = ent
        ent[1] += 16
        return ent[0], ent[1]

    def dma(self, queue, out, in_, r=(), w=(), semkey=None, **kw):
        waits = self._deps(queue, r, w, True)
        if semkey is None:
            semkey = w[0] if (w and not str(w[0]).startswith("dram")) else r[0]
        sem, val = self._dma_sem(semkey, queue)
        tok = Tok(sem, val, None)
        self.q[queue].append((waits, lambda e: e.dma_start(out=out, in_=in_, **kw), tok.sem, 16))
        self._commit(tok, r, w)
        return tok

    def coll(self, fn, r=(), w=(), semkey=None):
        waits = self._deps("pool", r, w, True)
        sem, val = self._dma_sem(semkey, "pool")
        tok = Tok(sem, val, None)
        self.q["pool"].append((waits, fn, tok.sem, 16))
        self._commit(tok, r, w)
        return tok

    def barrier(self):
        toks = list(self.all_tokens.values())
        for e in self.ENGS:
            seen = self.seen[e]
            waits = []
            for t in toks:
                if t.eng == e:
                    continue
                if seen.get(id(t.sem), 0) >= t.val:
                    continue
                seen[id(t.sem)] = t.val
                waits.append((t.sem, t.val))
            if waits:
                self.q[e].append((waits, None, None, 0))
        self.lastw.clear()
        self.readers.clear()
        if hasattr(self, "dpool"):
            self.dsem.clear()
            self.dfree = {q: [e for e in lst if e[1] < self.LIMIT] for q, lst in self.dpool.items()}

    def final_wait(self, eng="sp"):
        toks = list(self.all_tokens.values())
        waits = [(t.sem, t.val) for t in toks]
        self.q[eng].append((waits, None, None, 0))

    def emit(self):
        nc = self.nc
        q = self.q

        def run(e, ename):
            for waits, fn, sem, inc in q[ename]:
                for s, v in waits:
                    e.wait_ge(s, v)
                if fn is not None:
                    ins = fn(e)
                    ins.then_inc(sem, inc)

        with nc.Block() as block:
            @block.tensor
            def _(e):
                run(e, "pe")

            @block.scalar
            def _(e):
                run(e, "act")

            @block.vector
            def _(e):
                run(e, "dve")

            @block.gpsimd
            def _(e):
                run(e, "pool")

            @block.sync
            def _(e):
                run(e, "sp")

    def init_arena(self, nbytes):
        self.arena = self.stack.enter_context(self.nc.sbuf_tensor("arena", [128, nbytes // 4], F32))
        self.arena_bytes = nbytes
        self.arena_off = 0

    def mark(self):
        self.arena_marks.append(self.arena_off)

    def release(self):
        self.arena_off = self.arena_marks.pop()

    def alloc(self, shape, dt):
        n = 1
        for s in shape[1:]:
            n *= s
        esz = 2 if dt == BF16 else 4
        nb = (n * esz + 31) // 32 * 32
        assert self.arena_off + nb <= self.arena_bytes, ("SBUF arena overflow", self.arena_off, nb)
        a = self.arena[:, self.arena_off // 4:(self.arena_off + nb) // 4]
        self.arena_off += nb
        if dt != F32:
            a = a.bitcast(dt)
        a = a[:, 0:n]
        if len(shape) == 3:
            a = a.rearrange("p (a b) -> p a b", a=shape[1])
        elif len(shape) == 4:
            a = a.rearrange("p (a b c) -> p a b c", a=shape[1], b=shape[2])
        if shape[0] != 128:
            a = a[0:shape[0]]
        return a


D = 4096
KC = 32
NT = 1024
NE = 32
DEPTH = 2
DN_ALPHA = (2 * DEPTH) ** 0.25
LN_EPS = 1e-5
RMS_EPS = 1e-6


class Ctx:
    def __init__(self, nc, stack, ext):
        self.nc = nc
        self.stack = stack
        self.ext = ext
        self.P = Prog(nc, stack)
        self.P.init_arena(190 * 1024)
        self.ps = [stack.enter_context(nc.psum_tensor("ps%d" % i, [128, 512], F32)) for i in range(8)]
        self.pi = 0
        self.consts = {}

    def dram(self, name, shape, dt=F32):
        if not hasattr(self, "_drams"):
            self._drams = {}
        if name in self._drams:
            return self._drams[name]
        k = self.ext.get(name)
        kind = {"in": "ExternalInput", "out": "ExternalOutput", None: "Internal"}[k]
        ap = self.nc.dram_tensor(name, list(shape), dt, kind=kind).ap()
        self._drams[name] = ap
        return ap

    def dram_once(self, name, shape, dt=F32):
        return self.dram(name, shape, dt)

    def bank(self):
        b = self.pi % 8
        self.pi += 1
        return b

    def const(self, name, dram_ap, shape, dt=F32, queue="sp"):
        t = self.P.alloc(shape, dt)
        self.P.dma(queue, t, dram_ap, r=["dram_c_" + name], w=["c_" + name])
        self.consts[name] = t
        return t


def stage_begin(C):
    C.P.barrier()
    C.P.mark()


def stage_end(C):
    C.P.barrier()
    C.P.release()


def stage_ln(C, vT, outT, w_p, b_p, eps, tag):
    P = C.P
    stage_begin(C)
    ones = C.consts["ones_f"]
    zb = [P.alloc([128, NT], F32) for _ in range(3)]
    sq = [P.alloc([128, NT], F32) for _ in range(2)]
    for kc in range(KC):
        s = kc % 3
        q = kc % 2
        P.dma("sp", zb[s], vT[kc * 128:(kc + 1) * 128, :], r=["dram_" + tag + "v"], w=[("zb", s)])
        P.op("act", lambda e, s=s, q=q: e.activation(out=sq[q], in_=zb[s], func=AF.Square), r=[("zb", s)], w=[("sq", q)])
        for hb in range(2):
            P.op("pe", lambda e, s=s, hb=hb, kc=kc: e.matmul(C.ps[hb][:, :], lhsT=ones, rhs=zb[s][:, hb * 512:(hb + 1) * 512],
                                                             start=(kc == 0), stop=(kc == KC - 1)),
                 r=[("zb", s), "c_ones_f"], w=[("ps", hb)])
            P.op("pe", lambda e, q=q, hb=hb, kc=kc: e.matmul(C.ps[2 + hb][:, :], lhsT=ones, rhs=sq[q][:, hb * 512:(hb + 1) * 512],
                                                             start=(kc == 0), stop=(kc == KC - 1)),
                 r=[("sq", q), "c_ones_f"], w=[("ps", 2 + hb)])
    mean = P.alloc([128, NT], F32)
    rstd = P.alloc([128, NT], F32)
    tmp = P.alloc([128, NT], F32)
    for hb in range(2):
        sl = slice(hb * 512, (hb + 1) * 512)
        P.op("dve", lambda e, hb=hb, sl=sl: e.tensor_scalar(out=mean[:, sl], in0=C.ps[hb][:, :], scalar1=1.0 / D, scalar2=None, op0=ALU.mult),
             r=[("ps", hb)], w=[("mean", hb)])
        P.op("dve", lambda e, sl=sl: e.tensor_tensor(out=tmp[:, sl], in0=mean[:, sl], in1=mean[:, sl], op=ALU.mult),
             r=[("mean", hb)], w=[("tmp", hb)])
        P.op("dve", lambda e, hb=hb, sl=sl: e.scalar_tensor_tensor(out=rstd[:, sl], in0=C.ps[2 + hb][:, :], scalar=1.0 / D, in1=tmp[:, sl],
                                                                   op0=ALU.mult, op1=ALU.subtract),
             r=[("ps", 2 + hb), ("tmp", hb)], w=[("rstd", hb)])
        P.op("dve", lambda e, sl=sl: e.tensor_scalar(out=rstd[:, sl], in0=rstd[:, sl], scalar1=float(eps), scalar2=None, op0=ALU.add),
             r=[("rstd", hb)], w=[("rstd", hb)])
        P.op("act", lambda e, sl=sl: e.activation(out=rstd[:, sl], in_=rstd[:, sl], func=AF.Sqrt), r=[("rstd", hb)], w=[("rstd", hb)])
        P.op("dve", lambda e, sl=sl: e.reciprocal(out=rstd[:, sl], in_=rstd[:, sl]), r=[("rstd", hb)], w=[("rstd", hb)])
    for kc in range(KC):
        s = kc % 3
        q = kc % 2
        P.dma("sp", zb[s], vT[kc * 128:(kc + 1) * 128, :], r=["dram_" + tag + "v"], w=[("zb", s)])
        P.op("dve", lambda e, s=s: e.tensor_tensor(out=zb[s], in0=zb[s], in1=mean, op=ALU.subtract),
             r=[("zb", s), ("mean", 0), ("mean", 1)], w=[("zb", s)])
        P.op("pool", lambda e, s=s: e.tensor_tensor(out=zb[s], in0=zb[s], in1=rstd, op=ALU.mult),
             r=[("zb", s), ("rstd", 0), ("rstd", 1)], w=[("zb", s)])
        P.op("act", lambda e, s=s, q=q, kc=kc: e.activation(out=sq[q], in_=zb[s], func=AF.Identity, bias=b_p[:, kc:kc + 1], scale=w_p[:, kc:kc + 1]),
             r=[("zb", s)], w=[("sq", q)])
        P.dma("sp", outT[kc * 128:(kc + 1) * 128, :], sq[q], r=[("sq", q)], w=["dram_" + tag + "o"], semkey=("lnst", q))
    stage_end(C)


def stage_moe(C, li, x1T, vT, HT, modq, W):
    P = C.P
    ones = C.consts["ones_f"]
    ident = C.consts["ident"]
    base = li * 192
    stage_begin(C)
    hff = P.alloc([128, KC, NT], BF16)
    P.mark()
    rw = P.alloc([128, KC, NE], F32)
    P.dma("sp", rw, W["router_w"].rearrange("(kc p) n -> p kc n", p=128), r=["dram_rw"], w=["rw"])
    rb = P.alloc([128, NE], F32)
    P.dma("sp", rb, W["router_b_rep"], r=["dram_rb"], w=["rb"])
    xb = [P.alloc([128, NT], F32) for _ in range(3)]
    for kc in range(KC):
        s = kc % 3
        P.dma("sp", xb[s], x1T[kc * 128:(kc + 1) * 128, :], r=["dram_x1"], w=[("xb", s)])
        P.op("dve", lambda e, s=s, kc=kc: e.tensor_scalar(out=xb[s], in0=xb[s], scalar1=modq[:, base + 4 * 32 + kc:base + 4 * 32 + kc + 1],
                                                          scalar2=modq[:, base + 3 * 32 + kc:base + 3 * 32 + kc + 1], op0=ALU.mult, op1=ALU.add),
             r=[("xb", s), "modq"], w=[("xb", s)])
        P.op("act", lambda e, s=s, kc=kc: e.activation(out=hff[:, kc, :], in_=xb[s], func=AF.Copy), r=[("xb", s)], w=[("hff", kc)])
        for tt in range(8):
            P.op("pe", lambda e, s=s, kc=kc, tt=tt: e.matmul(C.ps[tt][:, 0:NE], lhsT=xb[s][:, tt * 128:(tt + 1) * 128], rhs=rw[:, kc, :],
                                                             start=(kc == 0), stop=(kc == KC - 1)),
                 r=[("xb", s), "rw"], w=[("ps", tt)])
    PT = P.alloc([128, NT], F32)
    lg = [P.alloc([128, NE], F32) for _ in range(2)]
    t8 = [P.alloc([128, 8], F32) for _ in range(2)]
    mk = [P.alloc([128, NE], F32) for _ in range(2)]
    ex = [P.alloc([128, NE], F32) for _ in range(2)]
    sm = [P.alloc([128, 2], F32) for _ in range(2)]
    for tt in range(8):
        s = tt % 2
        K = lambda n, s=s: (n, s)
        P.op("dve", lambda e, s=s, tt=tt: e.tensor_tensor(out=lg[s], in0=C.ps[tt][:, 0:NE], in1=rb, op=ALU.add),
             r=[("ps", tt), "rb"], w=[K("lg")])
        P.op("dve", lambda e, s=s: e.max(out=t8[s], in_=lg[s]), r=[K("lg")], w=[K("t8")])
        P.op("dve", lambda e, s=s: e.tensor_scalar(out=mk[s], in0=lg[s], scalar1=t8[s][:, 3:4], scalar2=None, op0=ALU.is_ge),
             r=[K("lg"), K("t8")], w=[K("mk")])
        P.op("dve", lambda e, s=s: e.tensor_scalar(out=sm[s][:, 0:1], in0=t8[s][:, 0:1], scalar1=-1.0, scalar2=None, op0=ALU.mult),
             r=[K("t8")], w=[K("sm0")])
        P.op("act", lambda e, s=s: e.activation(out=ex[s], in_=lg[s], func=AF.Exp, bias=sm[s][:, 0:1], scale=1.0),
             r=[K("lg"), K("sm0")], w=[K("ex")])
        P.op("dve", lambda e, s=s: e.tensor_tensor(out=ex[s], in0=ex[s], in1=mk[s], op=ALU.mult), r=[K("ex"), K("mk")], w=[K("ex")])
        P.op("dve", lambda e, s=s: e.reduce_sum(out=sm[s][:, 1:2], in_=ex[s], axis=AX.X), r=[K("ex")], w=[K("sm1")])
        P.op("dve", lambda e, s=s: e.reciprocal(out=sm[s][:, 1:2], in_=sm[s][:, 1:2]), r=[K("sm1")], w=[K("sm1")])
        P.op("dve", lambda e, s=s: e.tensor_scalar(out=ex[s], in0=ex[s], scalar1=sm[s][:, 1:2], scalar2=None, op0=ALU.mult),
             r=[K("ex"), K("sm1")], w=[K("ex")])
        pb = tt // 4
        P.op("pe", lambda e, s=s, tt=tt, pb=pb: e.transpose(C.ps[pb][0:NE, (tt % 4) * 128:(tt % 4 + 1) * 128], ex[s], ident),
             r=[K("ex"), "c_ident"] + ([("ps", pb)] if tt % 4 else []), w=[("ps", pb)])
    for pb in range(2):
        P.op("act", lambda e, pb=pb: e.activation(out=PT[0:NE, pb * 512:(pb + 1) * 512], in_=C.ps[pb][0:NE, :], func=AF.Copy),
             r=[("ps", pb)], w=[("PT", pb)])
    P.barrier()
    P.release()
    PTk = P.alloc([128, NT], F32)
    P.op("dve", lambda e: e.tensor_copy(out=PTk[0:NE, :], in_=PT[0:NE, :]), r=[], w=["PTk"])
    P.barrier()
    bgu = P.alloc([128, NE * 8], F32)
    P.dma("sp", bgu, W["bgu_p"], r=["dram_bgu"], w=["bgu"])
    sel = P.alloc([NE, NE * 128], F32)
    P.dma("sp", sel, C.dram_once("c_sel", [NE, NE * 128]), r=["dram_sel"], w=["c_sel"])
    wb = [P.alloc([128, KC, 256], BF16) for _ in range(3)]
    pbc = [P.alloc([128, 512], F32) for _ in range(4)]
    tg = [P.alloc([128, 512], F32) for _ in range(2)]
    tsg = [P.alloc([128, 512], F32) for _ in range(2)]
    tu = [P.alloc([128, 512], F32) for _ in range(2)]
    ho = [P.alloc([128, 512], BF16) for _ in range(4)]
    wi = 0
    ui = 0
    hoi = 0
    for ex_i in range(NE):
        for hb in range(2):
            b = C.bank()
            s4 = (ex_i * 2 + hb) % 4
            P.op("pe", lambda e, b=b, ex_i=ex_i, hb=hb: e.matmul(C.ps[b][:, :], lhsT=sel[0:NE, ex_i * 128:(ex_i + 1) * 128],
                                                                 rhs=PTk[0:NE, hb * 512:(hb + 1) * 512], start=True, stop=True),
                 r=["PTk", "c_sel"], w=[("ps", b)])
            P.op("act", lambda e, b=b, s4=s4: e.activation(out=pbc[s4], in_=C.ps[b][:, :], func=AF.Copy), r=[("ps", b)], w=[("pbc", s4)])
        for fc in range(4):
            s = wi % 3
            wi += 1
            P.dma("pool", wb[s], W["wgu_h"][ex_i].rearrange("(kc p) n -> p kc n", p=128)[:, :, fc * 256:(fc + 1) * 256],
                  r=["dram_wgu"], w=[("wb", s)])
            for hb in range(2):
                bg_ = C.bank()
                bu_ = C.bank()
                for kc in range(KC):
                    P.op("pe", lambda e, b=bg_, s=s, kc=kc, hb=hb: e.matmul(C.ps[b][:, :], lhsT=wb[s][:, kc, 0:128], rhs=hff[:, kc, hb * 512:(hb + 1) * 512],
                                                                            start=(kc == 0), stop=(kc == KC - 1)),
                         r=[("wb", s), ("hff", kc)], w=[("ps", bg_)])
                for kc in range(KC):
                    P.op("pe", lambda e, b=bu_, s=s, kc=kc, hb=hb: e.matmul(C.ps[b][:, :], lhsT=wb[s][:, kc, 128:256], rhs=hff[:, kc, hb * 512:(hb + 1) * 512],
                                                                            start=(kc == 0), stop=(kc == KC - 1)),
                         r=[("wb", s), ("hff", kc)], w=[("ps", bu_)])
                u = ui % 2
                ui += 1
                o = hoi % 4
                hoi += 1
                s4 = (ex_i * 2 + hb) % 4
                cg = (ex_i * 4 + fc) * 2
                P.op("dve", lambda e, b=bg_, u=u, cg=cg: e.tensor_scalar(out=tg[u], in0=C.ps[b][:, :], scalar1=bgu[:, cg:cg + 1], scalar2=7.0, op0=ALU.add, op1=ALU.min),
                     r=[("ps", bg_), "bgu"], w=[("tg", u)])
                P.op("act", lambda e, u=u: e.activation(out=tsg[u], in_=tg[u], func=AF.Sigmoid, scale=1.702), r=[("tg", u)], w=[("tsg", u)])
                P.op("dve", lambda e, b=bu_, u=u, cg=cg: e.tensor_scalar(out=tu[u], in0=C.ps[b][:, :], scalar1=bgu[:, cg + 1:cg + 2], scalar2=7.0, op0=ALU.add, op1=ALU.min),
                     r=[("ps", bu_), "bgu"], w=[("tu", u)])
                P.op("pool", lambda e, u=u: e.tensor_scalar(out=tu[u], in0=tu[u], scalar1=-7.0, scalar2=1.0, op0=ALU.max, op1=ALU.add),
                     r=[("tu", u)], w=[("tu", u)])
                P.op("pool", lambda e, u=u: e.tensor_tensor(out=tg[u], in0=tg[u], in1=tsg[u], op=ALU.mult), r=[("tg", u), ("tsg", u)], w=[("tg", u)])
                P.op("pool", lambda e, u=u: e.tensor_tensor(out=tg[u], in0=tg[u], in1=tu[u], op=ALU.mult), r=[("tg", u), ("tu", u)], w=[("tg", u)])
                P.op("dve", lambda e, u=u, o=o, s4=s4: e.tensor_tensor(out=ho[o], in0=tg[u], in1=pbc[s4], op=ALU.mult),
                     r=[("tg", u), ("pbc", s4)], w=[("ho", o)])
                row = (ex_i * 4 + fc) * 128
                P.dma("sp", HT[row:row + 128, hb * 512:(hb + 1) * 512], ho[o], r=[("ho", o)], w=["dram_HT"], semkey=("host", o))
    P.barrier()
    P.release()
    P.mark()
    PT2 = P.alloc([128, NT], F32)
    P.op("dve", lambda e: e.tensor_copy(out=PT2[0:NE, :], in_=PTk[0:NE, :]), r=[], w=["PT2"])
    P.barrier()
    bd = P.alloc([128, D], F32)
    P.dma("sp", bd[0:NE, :], W["b_down"], r=["dram_bd"], w=["bd"])
    ht = [P.alloc([128, 4, NT], BF16) for _ in range(3)]
    wd = [P.alloc([128, 4, 512], BF16) for _ in range(3)]
    xr = [P.alloc([128, 512], F32) for _ in range(4)]
    gq0 = base + 5 * 32
    hi = 0
    xi = 0
    for dg in range(8):
        for ex_i in range(NE):
            s = hi % 3
            hi += 1
            P.dma("sp", ht[s], HT[ex_i * 512:(ex_i + 1) * 512, :].rearrange("(fc p) t -> p fc t", p=128), r=["dram_HT"], w=[("ht", s)])
            P.dma("pool", wd[s], W["w_down"][ex_i].rearrange("(fc p) n -> p fc n", p=128)[:, :, dg * 512:(dg + 1) * 512],
                  r=["dram_wd"], w=[("wd", s)])
            for dc in range(4):
                for hb in range(2):
                    b = dc * 2 + hb
                    for fc in range(4):
                        P.op("pe", lambda e, b=b, s=s, fc=fc, dc=dc, hb=hb, ex_i=ex_i: e.matmul(
                            C.ps[b][:, :], lhsT=wd[s][:, fc, dc * 128:(dc + 1) * 128], rhs=ht[s][:, fc, hb * 512:(hb + 1) * 512],
                            start=(ex_i == 0 and fc == 0), stop=False),
                            r=[("wd", s), ("ht", s)], w=[("ps", b)])
        for dc in range(4):
            for hb in range(2):
                b = dc * 2 + hb
                col = dg * 512 + dc * 128
                P.op("pe", lambda e, b=b, col=col, hb=hb: e.matmul(C.ps[b][:, :], lhsT=bd[0:NE, col:col + 128], rhs=PT2[0:NE, hb * 512:(hb + 1) * 512],
                                                                   start=False, stop=True),
                     r=["bd", "PT2"], w=[("ps", b)])
                x = xi % 4
                xi += 1
                kcx = dg * 4 + dc
                P.dma("sp", xr[x], x1T[col:col + 128, hb * 512:(hb + 1) * 512], r=["dram_x1"], w=[("xr", x)])
                P.op("dve", lambda e, b=b, x=x, kcx=kcx: e.scalar_tensor_tensor(out=xr[x], in0=C.ps[b][:, :], scalar=modq[:, gq0 + kcx:gq0 + kcx + 1], in1=xr[x],
                                                                                op0=ALU.mult, op1=ALU.add),
                     r=[("ps", b), ("xr", x), "modq"], w=[("xr", x)])
                P.dma("sp", vT[col:col + 128, hb * 512:(hb + 1) * 512], xr[x], r=[("xr", x)], w=["dram_v"], semkey=("xrst", x))
    stage_end(C)


def load_consts(C):
    P = C.P
    C.const("ones_f", C.dram("c_ones", [128, 128]), [128, 128])
    C.const("ident", C.dram("c_ident", [128, 128]), [128, 128])
    C.const("lnw", C.dram("lnw_p", [128, DEPTH * 2 * KC]), [128, DEPTH * 2 * KC])
    C.const("lnb", C.dram("lnb_p", [128, DEPTH * 2 * KC]), [128, DEPTH * 2 * KC])
    modq = C.const("modq", C.dram("modp", [128, DEPTH * 6 * KC]), [128, DEPTH * 6 * KC])
    for i in range(DEPTH):
        for s_ in (1, 4):
            sl = slice(i * 192 + s_ * 32, i * 192 + s_ * 32 + 32)
            P.op("dve", lambda e, sl=sl: e.tensor_scalar(out=modq[:, sl], in0=modq[:, sl], scalar1=1.0, scalar2=None, op0=ALU.add),
                 r=["c_modq"], w=["c_modq"])
        for s_ in (2, 5):
            sl = slice(i * 192 + s_ * 32, i * 192 + s_ * 32 + 32)
            P.op("dve", lambda e, sl=sl: e.tensor_scalar(out=modq[:, sl], in0=modq[:, sl], scalar1=1.0, scalar2=1.0 / DN_ALPHA, op0=ALU.add, op1=ALU.mult),
                 r=["c_modq"], w=["c_modq"])
    P.barrier()
    return modq


def host_consts():
    c = {}
    c["c_ones"] = np.ones((128, 128), np.float32)
    c["c_ident"] = np.eye(128, dtype=np.float32)
    sel = np.zeros((NE, NE, 128), np.float32)
    for e in range(NE):
        sel[e, e, :] = 1.0
    c["c_sel"] = sel.reshape(NE, NE * 128)
    return c


def pp(v):
    return np.ascontiguousarray(v.reshape(-1, 128).T)


def host_moe_weights(li, router_w, router_b, w_gu, b_gu, w_down, b_down, sfx):
    m = {}
    m["router_w" + sfx] = np.ascontiguousarray(router_w[li])
    m["router_b_rep" + sfx] = np.ascontiguousarray(np.broadcast_to(router_b[li][None, :], (128, NE)))
    g = w_gu[li].reshape(NE, D, 4, 128, 2).transpose(0, 1, 2, 4, 3).reshape(NE, D, 1024)
    m["wgu_h" + sfx] = np.ascontiguousarray(g)
    bg = b_gu[li].reshape(NE, 4, 128, 2).transpose(2, 0, 1, 3).reshape(128, NE * 8)
    m["bgu_p" + sfx] = np.ascontiguousarray(bg)
    m["w_down" + sfx] = np.ascontiguousarray(w_down[li])
    m["b_down" + sfx] = np.ascontiguousarray(b_down[li])
    return m


def moe_weight_aps(C, sfx):
    return {
        "router_w": C.dram("router_w" + sfx, [D, NE]),
        "router_b_rep": C.dram("router_b_rep" + sfx, [128, NE]),
        "wgu_h": C.dram("wgu_h" + sfx, [NE, D, 1024]),
        "bgu_p": C.dram("bgu_p" + sfx, [128, NE * 8]),
        "w_down": C.dram("w_down" + sfx, [NE, 512, D]),
        "b_down": C.dram("b_down" + sfx, [NE, D]),
    }


def gemm_fm(C, act, KCn, Wv, col0, ncols, toks, epi, tag, wbufs, actkey):
    P = C.P
    for g0 in range(col0, col0 + ncols, 256):
        gw = min(256, col0 + ncols - g0)
        s = C.wi % len(wbufs)
        C.wi += 1
        P.dma("pool", wbufs[s][:, 0:KCn, 0:gw], Wv[:, :, g0:g0 + gw], r=["dram_w" + tag], w=[("wbuf", s)])
        for j0 in range(0, gw, 128):
            cw = min(128, gw - j0)
            for (t0, tn) in toks:
                b = C.bank()
                for kc in range(KCn):
                    P.op("pe", lambda e, b=b, s=s, kc=kc, j0=j0, cw=cw, t0=t0, tn=tn: e.matmul(
                        C.ps[b][0:cw, 0:tn], lhsT=wbufs[s][:, kc, j0:j0 + cw], rhs=act[:, kc, t0:t0 + tn],
                        start=(kc == 0), stop=(kc == KCn - 1)),
                        r=[("wbuf", s), actkey], w=[("ps", b)])
                epi(b, g0 + j0, cw, t0, tn)


def gemm_tm(C, act, KCn, Wv, col0, ncols, tts, epi, tag, wbufs, actkey):
    P = C.P
    for g0 in range(col0, col0 + ncols, 256):
        gw = min(256, col0 + ncols - g0)
        s = C.wi % len(wbufs)
        C.wi += 1
        P.dma("pool", wbufs[s][:, 0:KCn, 0:gw], Wv[:, :, g0:g0 + gw], r=["dram_w" + tag], w=[("wbuf", s)])
        for tt in tts:
            b = C.bank()
            for kc in range(KCn):
                P.op("pe", lambda e, b=b, s=s, kc=kc, gw=gw, tt=tt: e.matmul(
                    C.ps[b][:, 0:gw], lhsT=act[:, kc, tt * 128:(tt + 1) * 128], rhs=wbufs[s][:, kc, 0:gw],
                    start=(kc == 0), stop=(kc == KCn - 1)),
                    r=[("wbuf", s), actkey], w=[("ps", b)])
            epi(b, tt, g0, gw)


def make_hmix(C, srcT, hm, modq, sc_col, sh_col, xb, key):
    P = C.P
    for kc in range(KC):
        s = kc % len(xb)
        P.dma("sp", xb[s], srcT[kc * 128:(kc + 1) * 128, :], r=["dram_src" + str(key)], w=[("xb", s)])
        P.op("dve", lambda e, s=s, kc=kc: e.tensor_scalar(out=hm[:, kc, :], in0=xb[s], scalar1=modq[:, sc_col + kc:sc_col + kc + 1],
                                                          scalar2=modq[:, sh_col + kc:sh_col + kc + 1], op0=ALU.mult, op1=ALU.add),
             r=[("xb", s), "c_modq"], w=[key])


O_XBC = 4096
O_DT = 4096 + 6144
O_U = O_DT + 64


def stage_inproj(C, srcT, modq, Win, xbcT, dt_tok, z_tok, uT, reg):
    P = C.P
    stage_begin(C)
    C.wi = 0
    Wv = Win.rearrange("(kc p) n -> p kc n", p=128)
    hm = P.alloc([128, KC, NT], BF16)
    xb = [P.alloc([128, NT], F32) for _ in range(2)]
    wbufs = [P.alloc([128, KC, 256], BF16) for _ in range(3)]
    ev = [P.alloc([128, 512], F32) for _ in range(4)]
    evi = [0]

    def evac_store(b, rows, cols, dst):
        o = evi[0] % 4
        evi[0] += 1
        P.op("act", lambda e, b=b, o=o: e.activation(out=ev[o][0:rows, 0:cols], in_=C.ps[b][0:rows, 0:cols], func=AF.Copy),
             r=[("ps", b)], w=[("ev", o)])
        P.dma("sp", dst, ev[o][0:rows, 0:cols], r=[("ev", o)], w=["dram_s1"], semkey=("evst", o))

    key = "hm"
    make_hmix(C, srcT, hm, modq, 1 * 32, 0 * 32, xb, key)
    toks = [(0, 512), (512, 512)]
    r0 = reg * NT
    gemm_fm(C, hm, KC, Wv, O_XBC, 6144, toks,
            lambda b, col, cw, t0, tn: evac_store(b, cw, tn, xbcT[col - O_XBC:col - O_XBC + cw, r0 + t0:r0 + t0 + tn]),
            "in", wbufs, key)
    gemm_tm(C, hm, KC, Wv, O_DT, 64, list(range(8)),
            lambda b, tt, col, gw: evac_store(b, 128, gw, dt_tok[r0 + tt * 128:r0 + (tt + 1) * 128, :]),
            "in", wbufs, key)
    gemm_fm(C, hm, KC, Wv, O_U, 4096, toks,
            lambda b, col, cw, t0, tn: evac_store(b, cw, tn, uT[col - O_U:col - O_U + cw, 16 + t0:16 + t0 + tn]),
            "in", wbufs, key)
    gemm_tm(C, hm, KC, Wv, 0, 4096, list(range(8)),
            lambda b, tt, col, gw: evac_store(b, 128, gw, z_tok[tt * 128:(tt + 1) * 128, col:col + gw]),
            "in", wbufs, key)
    stage_end(C)


def stage_conv(C, xbcT, xactT, convw, convb, reg, flag):
    P = C.P
    stage_begin(C)
    NCH = 6144 // 128
    r0 = reg * NT
    ib = [P.alloc([128, 3 + NT], F32) for _ in range(2)]
    ac = [P.alloc([128, NT], F32) for _ in range(2)]
    for c in range(NCH):
        s = c % 2
        if reg == 0:
            P.op("dve", lambda e, s=s: e.memset(ib[s][:, 0:3], 0.0), r=[], w=[("ibh", s)])
        else:
            P.dma("sp", ib[s][:, 0:3], xbcT[c * 128:(c + 1) * 128, NT - 3:NT], r=["dram_xbc"], w=[("ibh", s)])
            P.op("dve", lambda e, s=s: e.tensor_scalar(out=ib[s][:, 0:3], in0=ib[s][:, 0:3], scalar1=flag[:, 0:1], scalar2=None, op0=ALU.mult),
                 r=[("ibh", s), "c_flag"], w=[("ibh", s)])
        P.dma("sp", ib[s][:, 3:3 + NT], xbcT[c * 128:(c + 1) * 128, r0:r0 + NT], r=["dram_xbc"], w=[("ib", s)])
        P.op("dve", lambda e, s=s, c=c: e.tensor_scalar(out=ac[s], in0=ib[s][:, 0:NT], scalar1=convw[:, c * 4:c * 4 + 1], scalar2=None, op0=ALU.mult),
             r=[("ib", s), ("ibh", s), "c_convw"], w=[("ac", s)])
        for k in range(1, 4):
            P.op("dve", lambda e, s=s, c=c, k=k: e.scalar_tensor_tensor(out=ac[s], in0=ib[s][:, k:k + NT], scalar=convw[:, c * 4 + k:c * 4 + k + 1], in1=ac[s],
                                                                        op0=ALU.mult, op1=ALU.add),
                 r=[("ib", s), ("ibh", s), ("ac", s)], w=[("ac", s)])
        P.op("act", lambda e, s=s, c=c: e.activation(out=ac[s], in_=ac[s], func=AF.Silu, bias=convb[:, c:c + 1], scale=1.0),
             r=[("ac", s), "c_convb"], w=[("ac", s)])
        P.dma("sp", xactT[c * 128:(c + 1) * 128, r0:r0 + NT], ac[s], r=[("ac", s)], w=["dram_xact"], semkey=("acst", s))
    stage_end(C)


def stage_ssd(C, xactT, dt_tok, z_tok, catT, K_, reg, hstate):
    P = C.P
    stage_begin(C)
    ones = C.consts["ones_f"]
    ident = C.consts["ident"]
    U = K_["U"]
    maskneg = K_["maskneg"]
    aneg = K_["aneg"]
    dtb = K_["dtb"]
    Drow = P.alloc([128, D], F32)
    P.dma("sp", Drow, K_["Drow_d"], r=["dram_drow"], w=["c_Drow"])
    flag = K_["flag"]
    nw = K_["ssdnw"]
    xv = xactT[0:4096, :].rearrange("(kc p) t -> p kc t", p=128)
    bv = xactT[4096:5120, :].rearrange("(g p) t -> p g t", p=128)
    cv = xactT[5120:6144, :].rearrange("(g p) t -> p g t", p=128)
    hS = P.alloc([128, D], F32)
    hB = P.alloc([128, D], BF16)
    if reg == 0:
        P.op("dve", lambda e: e.memset(hS, 0.0), r=[], w=["hS"])
    else:
        P.dma("sp", hS, hstate, r=["dram_hstate"], w=["hS"])
        P.op("dve", lambda e: e.tensor_scalar(out=hS, in0=hS, scalar1=K_["flag"][:, 0:1], scalar2=None, op0=ALU.mult), r=["hS", "c_flag"], w=["hS"])
    P.op("act", lambda e: e.activation(out=hB, in_=hS, func=AF.Copy), r=["hS"], w=["hB"])
    xTc = [P.alloc([128, KC, 128], F32) for _ in range(1)]
    bTf = P.alloc([128, 8, 128], F32)
    bTb = P.alloc([128, 8, 128], BF16)
    cTb = P.alloc([128, 8, 128], BF16)
    dtr = P.alloc([128, 64], F32)
    sA = P.alloc([128, 64], F32)
    sB = P.alloc([128, 64], F32)
    dts = P.alloc([128, 64], F32)
    la = P.alloc([128, 64], F32)
    cum = P.alloc([128, 64], F32)
    ecum = P.alloc([128, 64], F32)
    toend = P.alloc([128, 64], F32)
    cdec = P.alloc([128, 64], F32)
    xtok = P.alloc([128, D], F32)
    xdt = P.alloc([128, D], BF16)
    xdte = P.alloc([128, D], BF16)
    btok = P.alloc([128, 8, 128], BF16)
    zc = P.alloc([128, D], F32)
    y = P.alloc([128, D], F32)
    cbT = P.alloc([128, 8, 128], F32)
    larep = [P.alloc([128, 4, 128], F32) for _ in range(2)]
    tmp = [P.alloc([128, 4, 128], F32) for _ in range(2)]
    MT = [P.alloc([128, 4, 128], BF16) for _ in range(2)]
    ms = P.alloc([128, 8], F32)
    cat = P.alloc([128, KC, 128], BF16)
    v3 = lambda t: t.rearrange("p (h q) -> p h q", q=64)
    bc3 = lambda t: t[:, 0:64].unsqueeze(2).to_broadcast([128, 64, 64])
    for c in range(reg * 8, reg * 8 + 8):
        own = True
        t0 = c * 128
        tl = t0 - reg * NT
        xs = 0
        P.dma("sp", xTc[xs], xv[:, :, t0:t0 + 128], r=["dram_xact"], w=[("xTc", xs)])
        P.dma("sp", bTf, bv[:, :, t0:t0 + 128], r=["dram_xact"], w=["bTf"])
        P.dma("sp", dtr, dt_tok[t0:t0 + 128, :], r=["dram_dt"], w=["dtr"])
        if own:
            P.dma("pool", bTb, bv[:, :, t0:t0 + 128], r=["dram_xact"], w=["bTb"])
            P.dma("pool", cTb, cv[:, :, t0:t0 + 128], r=["dram_xact"], w=["cTb"])
            P.dma("sp", zc, z_tok[tl:tl + 128, :], r=["dram_z"], w=["zc"])
        P.op("dve", lambda e: e.tensor_tensor(out=dtr, in0=dtr, in1=dtb, op=ALU.add), r=["dtr", "c_dtb"], w=["dtr"])
        P.op("act", lambda e: e.activation(out=sA, in_=dtr, func=AF.Abs), r=["dtr"], w=["sA"])
        P.op("act", lambda e: e.activation(out=sA, in_=sA, func=AF.Exp, scale=-1.0), r=["sA"], w=["sA"])
        P.op("act", lambda e: e.activation(out=sA, in_=sA, func=AF.Ln, bias=1.0, scale=1.0), r=["sA"], w=["sA"])
        P.op("dve", lambda e: e.tensor_scalar(out=sB, in0=dtr, scalar1=0.0, scalar2=None, op0=ALU.max), r=["dtr"], w=["sB"])
        P.op("dve", lambda e: e.tensor_tensor(out=dts, in0=sA, in1=sB, op=ALU.add), r=["sA", "sB"], w=["dts"])
        P.op("dve", lambda e: e.tensor_tensor(out=la, in0=dts, in1=aneg, op=ALU.mult), r=["dts", "c_aneg"], w=["la"])
        b1 = C.bank()
        P.op("pe", lambda e, b=b1: e.matmul(C.ps[b][:, 0:64], lhsT=U, rhs=la, start=True, stop=True), r=["la", "c_U"], w=[("ps", b1)])
        b2 = C.bank()
        P.op("pe", lambda e, b=b2: e.matmul(C.ps[b][:, 0:64], lhsT=ones, rhs=la, start=True, stop=True), r=["la", "c_ones_f"], w=[("ps", b2)])
        P.op("act", lambda e, b=b1: e.activation(out=cum, in_=C.ps[b][:, 0:64], func=AF.Copy), r=[("ps", b1)], w=["cum"])
        P.op("act", lambda e, b=b1: e.activation(out=ecum, in_=C.ps[b][:, 0:64], func=AF.Exp), r=[("ps", b1)], w=["ecum"])
        P.op("dve", lambda e, b=b2: e.tensor_tensor(out=toend, in0=C.ps[b][:, 0:64], in1=cum, op=ALU.subtract), r=[("ps", b2), "cum"], w=["toend"])
        P.op("act", lambda e: e.activation(out=toend, in_=toend, func=AF.Exp), r=["toend"], w=["toend"])
        P.op("act", lambda e, b=b2: e.activation(out=cdec, in_=C.ps[b][:, 0:64], func=AF.Exp), r=[("ps", b2)], w=["cdec"])
        for q in range(8):
            b = C.bank()
            for j in range(4):
                kc = q * 4 + j
                P.op("pe", lambda e, b=b, j=j, kc=kc, xs=xs: e.transpose(C.ps[b][:, j * 128:(j + 1) * 128], xTc[xs][:, kc, :], ident),
                     r=[("xTc", xs), "c_ident"], w=[("ps", b)])
            P.op("act", lambda e, b=b, q=q: e.activation(out=xtok[:, q * 512:(q + 1) * 512], in_=C.ps[b][:, :], func=AF.Copy),
                 r=[("ps", b)], w=[("xtok", q)])
        xk = [("xtok", q) for q in range(8)]
        P.op("dve", lambda e: e.tensor_tensor(out=v3(xdt), in0=v3(xtok), in1=bc3(dts), op=ALU.mult), r=xk + ["dts"], w=["xdt"])
        P.op("pool", lambda e: e.tensor_tensor(out=v3(xdte), in0=v3(xdt), in1=bc3(toend), op=ALU.mult), r=["xdt", "toend"], w=["xdte"])
        for q in range(2):
            b = C.bank()
            for j in range(4):
                g = q * 4 + j
                P.op("pe", lambda e, b=b, j=j, g=g: e.transpose(C.ps[b][:, j * 128:(j + 1) * 128], bTf[:, g, :], ident),
                     r=["bTf", "c_ident"], w=[("ps", b)])
            P.op("act", lambda e, b=b, q=q: e.activation(out=btok[:, q * 4:(q + 1) * 4, :].rearrange("p a b -> p (a b)"), in_=C.ps[b][:, :], func=AF.Copy),
                 r=[("ps", b)], w=[("btok", q)])
        if own:
            for g in range(8):
                b = C.bank()
                P.op("pe", lambda e, b=b, g=g: e.matmul(C.ps[b][:, :], lhsT=cTb[:, g, :], rhs=hB[:, g * 512:(g + 1) * 512], start=True, stop=True),
                     r=["cTb", "hB"], w=[("ps", b)])
                P.op("dve", lambda e, b=b, g=g: e.tensor_tensor(out=y[:, g * 512:(g + 1) * 512].rearrange("p (h q) -> p h q", q=64),
                                                                in0=C.ps[b][:, :].rearrange("p (h q) -> p h q", q=64),
                                                                in1=ecum[:, g * 8:(g + 1) * 8].unsqueeze(2).to_broadcast([128, 8, 64]), op=ALU.mult),
                     r=[("ps", b), "ecum"], w=[("y", g)])
            for q in range(2):
                b = C.bank()
                for j in range(4):
                    g = q * 4 + j
                    P.op("pe", lambda e, b=b, j=j, g=g: e.matmul(C.ps[b][:, j * 128:(j + 1) * 128], lhsT=bTb[:, g, :], rhs=cTb[:, g, :], start=True, stop=True),
                         r=["bTb", "cTb"], w=[("ps", b)])
                P.op("act", lambda e, b=b, q=q: e.activation(out=cbT[:, q * 4:(q + 1) * 4, :].rearrange("p a b -> p (a b)"), in_=C.ps[b][:, :], func=AF.Copy),
                     r=[("ps", b)], w=[("cbT", q)])
            yb = None
            for q in range(16):
                g = q // 2
                u = q % 2
                P.op("dve", lambda e, u=u, q=q: e.tensor_copy(out=larep[u], in_=la[:, q * 4:(q + 1) * 4].unsqueeze(2).to_broadcast([128, 4, 128])),
                     r=["la"], w=[("larep", u)])
                b = C.bank()
                for j in range(4):
                    P.op("pe", lambda e, b=b, j=j, u=u: e.matmul(C.ps[b][:, j * 128:(j + 1) * 128], lhsT=larep[u][:, j, :], rhs=U, start=True, stop=True),
                         r=[("larep", u), "c_U"], w=[("ps", b)])
                for j in range(4):
                    h = q * 4 + j
                    P.op("dve", lambda e, b=b, j=j, u=u, h=h: e.scalar_tensor_tensor(out=tmp[u][:, j, :], in0=C.ps[b][:, j * 128:(j + 1) * 128], scalar=cum[:, h:h + 1],
                                                                                    in1=maskneg, op0=ALU.subtract, op1=ALU.add),
                         r=[("ps", b), "cum", "c_maskneg"], w=[("tmp", u)])
                P.op("act", lambda e, u=u: e.activation(out=tmp[u], in_=tmp[u], func=AF.Exp), r=[("tmp", u)], w=[("tmp", u)])
                P.op("pool", lambda e, u=u, g=g: e.tensor_tensor(out=MT[u], in0=tmp[u], in1=cbT[:, g, :].unsqueeze(1).to_broadcast([128, 4, 128]), op=ALU.mult),
                     r=[("tmp", u), ("cbT", g // 4)], w=[("MT", u)])
                if q % 2 == 0:
                    yb = C.bank()
                for j in range(4):
                    h = q * 4 + j
                    hh = h % 8
                    P.op("pe", lambda e, yb=yb, j=j, u=u, h=h, hh=hh: e.matmul(C.ps[yb][:, hh * 64:(hh + 1) * 64], lhsT=MT[u][:, j, :], rhs=xdt[:, h * 64:(h + 1) * 64],
                                                                              start=True, stop=True),
                         r=[("MT", u), "xdt"], w=[("ps", yb)])
                if q % 2 == 1:
                    P.op("dve", lambda e, yb=yb, g=g: e.tensor_tensor(out=y[:, g * 512:(g + 1) * 512], in0=C.ps[yb][:, :], in1=y[:, g * 512:(g + 1) * 512], op=ALU.add),
                         r=[("ps", yb), ("y", g)], w=[("y", g)])
            yk = [("y", g) for g in range(8)]
            P.op("pool", lambda e: e.tensor_tensor(out=xtok, in0=xtok, in1=Drow, op=ALU.mult), r=xk + ["c_Drow", "xdt"], w=xk)
            P.op("dve", lambda e: e.tensor_tensor(out=y, in0=y, in1=xtok, op=ALU.add), r=yk + xk, w=yk)
            P.op("act", lambda e: e.activation(out=zc, in_=zc, func=AF.Silu), r=["zc"], w=["zc"])
            P.op("dve", lambda e: e.tensor_tensor(out=y, in0=y, in1=zc, op=ALU.mult), r=yk + ["zc"], w=yk)
            P.op("dve", lambda e: e.memset(ms, 0.0), r=[], w=["msr"])
            for g in range(8):
                P.op("act", lambda e, g=g: e.activation(out=zc[:, g * 512:(g + 1) * 512], in_=y[:, g * 512:(g + 1) * 512], func=AF.Square, accum_out=ms[:, g:g + 1]),
                     r=yk + ["zc", "msr"], w=["zc", "msr"])
            P.op("dve", lambda e: e.tensor_scalar(out=ms, in0=ms, scalar1=1.0 / 512, scalar2=LN_EPS, op0=ALU.mult, op1=ALU.add), r=["msr"], w=["msr"])
            P.op("act", lambda e: e.activation(out=ms, in_=ms, func=AF.Sqrt), r=["msr"], w=["msr"])
            P.op("dve", lambda e: e.reciprocal(out=ms, in_=ms), r=["msr"], w=["msr"])
            P.op("dve", lambda e: e.tensor_tensor(out=y.rearrange("p (g q) -> p g q", q=512), in0=y.rearrange("p (g q) -> p g q", q=512),
                                                  in1=ms[:, 0:8].unsqueeze(2).to_broadcast([128, 8, 512]), op=ALU.mult), r=yk + ["msr"], w=yk)
            for q in range(8):
                b = C.bank()
                for j in range(4):
                    kc = q * 4 + j
                    P.op("pe", lambda e, b=b, j=j, kc=kc: e.transpose(C.ps[b][:, j * 128:(j + 1) * 128], y[:, kc * 128:(kc + 1) * 128], ident),
                         r=yk + ["c_ident"], w=[("ps", b)])
                for j in range(4):
                    kc = q * 4 + j
                    P.op("act", lambda e, b=b, j=j, kc=kc: e.activation(out=cat[:, kc, :], in_=C.ps[b][:, j * 128:(j + 1) * 128], func=AF.Copy, scale=nw[:, kc:kc + 1]),
                         r=[("ps", b), "c_ssdnw"], w=["cat"])
            P.dma("sp", catT[0:4096, tl:tl + 128].rearrange("(kc p) t -> p kc t", p=128), cat, r=["cat"], w=["dram_cat"], semkey="catst")
        P.op("dve", lambda e: e.tensor_tensor(out=v3(hS), in0=v3(hS), in1=bc3(cdec), op=ALU.mult), r=["hS", "cdec"], w=["hS"])
        for g in range(8):
            b = C.bank()
            P.op("pe", lambda e, b=b, g=g: e.matmul(C.ps[b][:, :], lhsT=btok[:, g, :], rhs=xdte[:, g * 512:(g + 1) * 512], start=True, stop=True),
                 r=[("btok", g // 4), "xdte"], w=[("ps", b)])
            P.op("dve", lambda e, b=b, g=g: e.tensor_tensor(out=hS[:, g * 512:(g + 1) * 512], in0=hS[:, g * 512:(g + 1) * 512], in1=C.ps[b][:, :], op=ALU.add),
                 r=[("ps", b), "hS"], w=["hS"])
        P.op("act", lambda e: e.activation(out=hB, in_=hS, func=AF.Copy), r=["hS"], w=["hB"])
    if reg == 0:
        P.dma("sp", hstate, hS, r=["hS"], w=["dram_hstate"], semkey="hsst")
    stage_end(C)


def stage_pool(C, uT, catT, poolw, K_, reg, uT_prev):
    P = C.P
    stage_begin(C)
    C.wi = 0
    pscale = K_["pscale"]
    invc = K_["invc"]
    diff = P.alloc([128, KC, NT], BF16)
    ub = [P.alloc([128, 16 + NT], F32) for _ in range(2)]
    pa = P.alloc([128, 16 + NT], F32)
    pb = P.alloc([128, 16 + NT], F32)
    ic = P.alloc([128, NT], F32)
    W_ = 16 + NT
    for kc in range(KC):
        g = kc // 8
        s = kc % 2
        if kc % 8 == 0:
            P.dma("sp", ic, invc[g], r=["dram_invc"], w=["ic"])
        P.dma("sp", ub[s][:, 16:W_], uT[kc * 128:(kc + 1) * 128, 16:W_], r=["dram_u"], w=[("ub", s)])
        if reg == 0:
            P.op("dve", lambda e, s=s: e.memset(ub[s][:, 0:16], 0.0), r=[("ub", s)], w=[("ub", s)])
        else:
            P.dma("sp", ub[s][:, 0:16], uT_prev[kc * 128:(kc + 1) * 128, NT:NT + 16], r=["dram_u"], w=[("ubh", s)])
            P.op("dve", lambda e, s=s: e.tensor_scalar(out=ub[s][:, 0:16], in0=ub[s][:, 0:16], scalar1=K_["flag"][:, 0:1], scalar2=None, op0=ALU.mult),
                 r=[("ubh", s), ("ub", s), "c_flag"], w=[("ub", s)])
        src = ub[s]
        srck = ("ub", s)
        dsts = [(pa, "pa"), (pb, "pb")]
        for st in range(g + 1):
            sh = 1 << st
            dst, dk = dsts[st % 2]
            P.op("dve", lambda e, dst=dst, src=src, sh=sh: e.tensor_tensor(out=dst[:, sh:W_], in0=src[:, sh:W_], in1=src[:, 0:W_ - sh], op=ALU.add),
                 r=[srck], w=[dk])
            src, srck = dst, dk
        P.op("pool", lambda e, src=src: e.tensor_tensor(out=src[:, 16:W_], in0=src[:, 16:W_], in1=ic, op=ALU.mult), r=[srck, "ic"], w=[srck])
        P.op("dve", lambda e, src=src, s=s, kc=kc: e.tensor_tensor(out=diff[:, kc, :], in0=src[:, 16:W_], in1=ub[s][:, 16:W_], op=ALU.subtract),
             r=[srck, ("ub", s)], w=["diff"])
    wbufs = [P.alloc([128, 8, 256], BF16) for _ in range(3)]
    ev = [P.alloc([128, 512], BF16) for _ in range(4)]
    evi = [0]

    def epi(b, col, cw, t0, tn, g):
        o = evi[0] % 4
        evi[0] += 1
        d0 = g * 1024 + col
        kcx = d0 // 128
        P.op("act", lambda e, b=b, o=o: e.activation(out=ev[o][:, 0:tn], in_=C.ps[b][:, 0:tn], func=AF.Copy, scale=pscale[:, kcx:kcx + 1]),
             r=[("ps", b), "c_pscale"], w=[("ev", o)])
        P.dma("sp", catT[4096 + d0:4096 + d0 + 128, t0:t0 + tn], ev[o][:, 0:tn], r=[("ev", o)], w=["dram_cat"], semkey=("pevst", o))

    for g in range(4):
        Wv = poolw[g].rearrange("(kc p) n -> p kc n", p=128)
        gemm_fm(C, diff[:, g * 8:(g + 1) * 8, :], 8, Wv, 0, 1024, [(0, 512), (512, 512)],
                lambda b, col, cw, t0, tn, g=g: epi(b, col, cw, t0, tn, g), "pool", wbufs, "diff")
    stage_end(C)


def stage_proj_res(C, actT, KCn, Wd, resT, vT, modq, gcol, tag):
    P = C.P
    stage_begin(C)
    C.wi = 0
    Wv = Wd.rearrange("(kc p) n -> p kc n", p=128)
    act = P.alloc([128, KCn, 512], BF16)
    wbufs = [P.alloc([128, KCn, 256], BF16) for _ in range(3)]
    xr = [P.alloc([128, 512], F32) for _ in range(4)]
    xi = [0]
    for hb in range(2):
        P.dma("sp", act, actT[:, hb * 512:(hb + 1) * 512].rearrange("(kc p) t -> p kc t", p=128), r=["dram_" + tag + "a"], w=["pact"])

        def epi(b, col, cw, t0, tn, hb=hb):
            x = xi[0] % 4
            xi[0] += 1
            kcx = col // 128
            P.dma("sp", xr[x], resT[col:col + 128, hb * 512:(hb + 1) * 512], r=["dram_" + tag + "r"], w=[("xr", x)])
            P.op("dve", lambda e, b=b, x=x: e.scalar_tensor_tensor(out=xr[x], in0=C.ps[b][:, :], scalar=modq[:, gcol + kcx:gcol + kcx + 1], in1=xr[x],
                                                                   op0=ALU.mult, op1=ALU.add),
                 r=[("ps", b), ("xr", x), "c_modq"], w=[("xr", x)])
            P.dma("sp", vT[col:col + 128, hb * 512:(hb + 1) * 512], xr[x], r=[("xr", x)], w=["dram_" + tag + "v"], semkey=("prst", x))

        gemm_fm(C, act, KCn, Wv, 0, D, [(0, 512)], epi, tag, wbufs, "pact")
    stage_end(C)


def load_l0_consts(C, sfx=""):
    P = C.P
    K_ = {}
    K_["U"] = C.const("U", C.dram("c_U", [128, 128]), [128, 128])
    K_["maskneg"] = C.const("maskneg", C.dram("c_maskneg", [128, 128]), [128, 128])
    K_["aneg"] = C.const("aneg", C.dram("alog_rep", [128, 64]), [128, 64])
    K_["dtb"] = C.const("dtb", C.dram("dtb_rep", [128, 64]), [128, 64])
    K_["Drow_d"] = C.dram("drow_rep", [128, D])
    K_["flag"] = C.const("flag", C.dram("flag" + sfx, [128, 1]), [128, 1])
    K_["ssdnw"] = C.const("ssdnw", C.dram("ssdnw_p", [128, KC]), [128, KC])
    K_["pscale"] = C.const("pscale", C.dram("pscale_p", [128, KC]), [128, KC])
    K_["convw"] = C.const("convw", C.dram("convw_p", [128, 192]), [128, 192])
    K_["convb"] = C.const("convb", C.dram("convb_p", [128, 48]), [128, 48])
    K_["invc"] = C.dram("invc_rep" + sfx, [4, 128, NT])
    a = K_["aneg"]
    P.op("act", lambda e: e.activation(out=a, in_=a, func=AF.Exp), r=["c_aneg"], w=["c_aneg"])
    P.op("dve", lambda e: e.tensor_scalar(out=a, in0=a, scalar1=-1.0, scalar2=None, op0=ALU.mult), r=["c_aneg"], w=["c_aneg"])
    P.barrier()
    return K_


def host_l0_consts(half, conv_w, conv_b, dt_bias, a_log, d_skip, ssd_norm_w, pool_scale):
    m = {}
    m["c_U"] = np.triu(np.ones((128, 128), np.float32))
    m["c_maskneg"] = np.where(np.arange(128)[None, :] >= np.arange(128)[:, None], 0.0, -1e30).astype(np.float32)
    rep = lambda v: np.ascontiguousarray(np.broadcast_to(v[None, :], (128, v.shape[0])))
    m["alog_rep"] = rep(a_log[0])
    m["dtb_rep"] = rep(dt_bias[0])
    m["drow_rep"] = rep(np.repeat(d_skip[0], 64))
    m["flag"] = np.full((128, 1), float(half), np.float32)
    m["ssdnw_p"] = pp(ssd_norm_w[0])
    m["pscale_p"] = pp(pool_scale[0])
    m["convw_p"] = np.ascontiguousarray(conv_w[0].reshape(4, 48, 128).transpose(2, 1, 0).reshape(128, 192))
    m["convb_p"] = pp(conv_b[0])
    tg = half * NT + np.arange(NT) + 1
    ic = np.stack([1.0 / np.minimum(tg, w).astype(np.float32) for w in (2, 4, 8, 16)], 0)
    m["invc_rep"] = np.ascontiguousarray(np.broadcast_to(ic[:, None, :], (4, 128, NT))).astype(np.float32)
    return m


def layer0_mixer(C, modq, srcT, x1T, reg, sfx=""):
    K_ = load_l0_consts(C, sfx)
    Win = C.dram("in_proj", [D, 14400])
    poolw = C.dram("pool_w", [4, 1024, 1024])
    Wout = C.dram("out_proj", [2 * D, D])
    xbcT = C.dram("s_xbcT", [6144, 2 * NT])
    xactT = C.dram("s_xactT", [6144, 2 * NT])
    dt_tok = C.dram("s_dt", [2 * NT, 64])
    z_tok = C.dram("s_z", [NT, D])
    uT = C.dram("s_uT%d" % reg, [D, 16 + NT])
    uT_prev = C.dram("s_uT0", [D, 16 + NT])
    hstate = C.dram("s_hstate", [128, D])
    catT = C.dram("s_catT", [2 * D, NT], BF16)
    v1T = C.dram("s_v1T", [D, NT])
    stage_inproj(C, srcT, modq, Win, xbcT, dt_tok, z_tok, uT, reg)
    stage_conv(C, xbcT, xactT, K_["convw"], K_["convb"], reg, K_["flag"])
    stage_ssd(C, xactT, dt_tok, z_tok, catT, K_, reg, hstate)
    stage_pool(C, uT, catT, poolw, K_, reg, uT_prev)
    stage_proj_res(C, catT, 64, Wout, srcT, v1T, modq, 0 * 192 + 2 * 32, "op")
    lw = C.consts["lnw"]
    lb = C.consts["lnb"]
    stage_ln(C, v1T, x1T, lw[:, 0:32], lb[:, 0:32], LN_EPS / DN_ALPHA ** 2, "ln1")


QK_SCALE = 192 ** -0.5


def rms_feat(C, src, nch, wp, dstT, nfeat, tagk):
    P = C.P
    ones = C.consts["ones_f"]
    sq = [P.alloc([128, NT], F32) for _ in range(2)]
    rstd = P.alloc([128, NT], F32)
    ob = [P.alloc([128, NT], BF16) for _ in range(2)]
    for c in range(nch):
        q = c % 2
        P.op("act", lambda e, c=c, q=q: e.activation(out=sq[q], in_=src[:, c, :], func=AF.Square), r=[tagk], w=[("rsq", q)])
        for hb in range(2):
            P.op("pe", lambda e, q=q, hb=hb, c=c: e.matmul(C.ps[hb][:, :], lhsT=ones, rhs=sq[q][:, hb * 512:(hb + 1) * 512], start=(c == 0), stop=(c == nch - 1)),
                 r=[("rsq", q), "c_ones_f"], w=[("ps", hb)])
    for hb in range(2):
        sl = slice(hb * 512, (hb + 1) * 512)
        P.op("dve", lambda e, hb=hb, sl=sl: e.tensor_scalar(out=rstd[:, sl], in0=C.ps[hb][:, :], scalar1=1.0 / nfeat, scalar2=RMS_EPS, op0=ALU.mult, op1=ALU.add),
             r=[("ps", hb)], w=["rrstd"])
    P.op("act", lambda e: e.activation(out=rstd, in_=rstd, func=AF.Sqrt), r=["rrstd"], w=["rrstd"])
    P.op("dve", lambda e: e.reciprocal(out=rstd, in_=rstd), r=["rrstd"], w=["rrstd"])
    for c in range(nch):
        q = c % 2
        P.op("dve", lambda e, c=c, q=q: e.tensor_tensor(out=sq[q], in0=src[:, c, :], in1=rstd, op=ALU.mult), r=[tagk, "rrstd"], w=[("rsq", q)])
        P.op("act", lambda e, c=c, q=q: e.activation(out=ob[q], in_=sq[q], func=AF.Copy, scale=wp[:, c:c + 1]), r=[("rsq", q)], w=[("rob", q)])
        P.dma("sp", dstT[c * 128:(c + 1) * 128, :], ob[q], r=[("rob", q)], w=["dram_rms" + str(tagk)], semkey=("robst", q))


def rotary_fm(C, src, dst_bf, cosT, sinT, rotm, scale, key_in, key_out, t1, t2):
    P = C.P
    for hb in range(2):
        sl = slice(hb * 512, (hb + 1) * 512)
        b = 2 + hb
        P.op("pe", lambda e, b=b, sl=sl: e.matmul(C.ps[b][0:64, :], lhsT=rotm, rhs=src[:, sl], start=True, stop=True), r=[key_in, "c_rotm"], w=[("ps", b)])
        P.op("dve", lambda e, b=b, sl=sl: e.tensor_tensor(out=t1[:, sl], in0=C.ps[b][0:64, :], in1=sinT[:, sl], op=ALU.mult), r=[("ps", b), "c_sinT"], w=[("rt1", hb)])
        P.op("pool", lambda e, sl=sl: e.tensor_tensor(out=t2[:, sl], in0=src[:, sl], in1=cosT[:, sl], op=ALU.mult), r=[key_in, "c_cosT"], w=[("rt2", hb)])
        P.op("dve", lambda e, sl=sl: e.tensor_tensor(out=t1[:, sl], in0=t1[:, sl], in1=t2[:, sl], op=ALU.add), r=[("rt1", hb), ("rt2", hb)], w=[("rt1", hb)])
        P.op("act", lambda e, sl=sl: e.activation(out=dst_bf[:, sl], in_=t1[:, sl], func=AF.Copy, scale=float(scale)), r=[("rt1", hb)], w=[key_out])


def stage_mla_proj(C, x2T, modq, Wqa, Wkva, qnT, kvnT, kpeT, L1, do_q=True):
    P = C.P
    stage_begin(C)
    C.wi = 0
    hm = P.alloc([128, KC, NT], BF16)
    xb = [P.alloc([128, NT], F32) for _ in range(2)]
    wbufs = [P.alloc([128, KC, 256], BF16) for _ in range(2)]
    make_hmix(C, x2T, hm, modq, 192 + 1 * 32, 192 + 0 * 32, xb, "hm1")
    toks = [(0, 512), (512, 512)]
    P.mark()
    if do_q:
        qa = P.alloc([128, 8, NT], F32)
        gemm_fm(C, hm, KC, Wqa.rearrange("(kc p) n -> p kc n", p=128), 0, 1024, toks,
                lambda b, col, cw, t0, tn: P.op("act", lambda e, b=b: e.activation(out=qa[:, col // 128, t0:t0 + tn], in_=C.ps[b][:, 0:tn], func=AF.Copy),
                                                r=[("ps", b)], w=["qa"]),
                "qa", wbufs, "hm1")
        P.barrier()
        rms_feat(C, qa, 8, L1["qnw"], qnT, 1024, "qa")
    P.barrier()
    P.release()
    kvc = P.alloc([128, 5, NT], F32)
    gemm_fm(C, hm, KC, Wkva.rearrange("(kc p) n -> p kc n", p=128), 0, 576, toks,
            lambda b, col, cw, t0, tn: P.op("act", lambda e, b=b: e.activation(out=kvc[0:cw, col // 128, t0:t0 + tn], in_=C.ps[b][0:cw, 0:tn], func=AF.Copy),
                                            r=[("ps", b)], w=["kvc"]),
            "kva", wbufs, "hm1")
    P.barrier()
    rms_feat(C, kvc, 4, L1["kvnw"], kvnT, 512, "kvc")
    kpb = P.alloc([64, NT], BF16)
    rt1 = P.alloc([64, NT], F32)
    rt2 = P.alloc([64, NT], F32)
    rotary_fm(C, kvc[0:64, 4, :], kpb, L1["cosT"], L1["sinT"], L1["rotm"], 1.0, "kvc", "kpb", rt1, rt2)
    P.dma("sp", kpeT, kpb, r=["kpb"], w=["dram_kpe"], semkey="kpbst")
    stage_end(C)


def stage_attn(C, qnT, kvnT_prev, kpeT_prev, kvnT, kpeT, Wqb, Wkvb, oT, L1):
    P = C.P
    stage_begin(C)
    onesb = L1["ones_b"]
    qn = P.alloc([128, 8, NT], BF16)
    kvn = P.alloc([128, 4, 2 * NT], BF16)
    kpe = P.alloc([64, 2 * NT], BF16)
    P.dma("sp", qn, qnT.rearrange("(kc p) t -> p kc t", p=128), r=["dram_qn"], w=["qn"])
    P.dma("sp", kvn[:, :, 0:NT], kvnT_prev.rearrange("(kc p) t -> p kc t", p=128), r=["dram_kvp"], w=["kvn0"])
    P.dma("sp", kvn[:, :, NT:2 * NT], kvnT.rearrange("(kc p) t -> p kc t", p=128), r=["dram_kv"], w=["kvn1"])
    P.dma("sp", kpe[:, 0:NT], kpeT_prev, r=["dram_kpp"], w=["kpe0"])
    P.dma("sp", kpe[:, NT:2 * NT], kpeT, r=["dram_kp"], w=["kpe1"])
    kin = ["kvn0", "kvn1"]
    wq = [P.alloc([128, 8, 192], BF16) for _ in range(2)]
    wk = [P.alloc([128, 4, 256], BF16) for _ in range(2)]
    qno = P.alloc([128, NT], BF16)
    qpf = P.alloc([64, NT], F32)
    qpe = P.alloc([64, NT], BF16)
    rt1 = P.alloc([64, NT], F32)
    rt2 = P.alloc([64, NT], F32)
    kno = P.alloc([128, 2 * NT], BF16)
    vtok = P.alloc([128, 16, 128], BF16)
    pt = [P.alloc([128, 512], BF16) for _ in range(3)]
    rinv = P.alloc([128, 512], F32)
    ob = [P.alloc([128, 512], BF16) for _ in range(2)]
    Wqv = Wqb.rearrange("(kc p) n -> p kc n", p=128)
    Wkv = Wkvb.rearrange("(kc p) n -> p kc n", p=128)
    rot = [0]

    def rb():
        b = rot[0] % 6
        rot[0] += 1
        return b
    pti = 0
    obi = 0
    for h in range(32):
        s = h % 2
        P.dma("pool", wq[s], Wqv[:, :, h * 192:(h + 1) * 192], r=["dram_wqb"], w=[("wq", s)])
        P.dma("pool", wk[s], Wkv[:, :, h * 256:(h + 1) * 256], r=["dram_wkvb"], w=[("wk", s)])
        for hb in range(2):
            sl = slice(hb * 512, (hb + 1) * 512)
            b = rb()
            for kc in range(8):
                P.op("pe", lambda e, b=b, s=s, kc=kc, sl=sl: e.matmul(C.ps[b][:, :], lhsT=wq[s][:, kc, 0:128], rhs=qn[:, kc, sl], start=(kc == 0), stop=(kc == 7)),
                     r=[("wq", s), "qn"], w=[("ps", b)])
            P.op("act", lambda e, b=b, sl=sl: e.activation(out=qno[:, sl], in_=C.ps[b][:, :], func=AF.Copy, scale=float(QK_SCALE)), r=[("ps", b)], w=["qno"])
            b = rb()
            for kc in range(8):
                P.op("pe", lambda e, b=b, s=s, kc=kc, sl=sl: e.matmul(C.ps[b][0:64, :], lhsT=wq[s][:, kc, 128:192], rhs=qn[:, kc, sl], start=(kc == 0), stop=(kc == 7)),
                     r=[("wq", s), "qn"], w=[("ps", b)])
            P.op("act", lambda e, b=b, sl=sl: e.activation(out=qpf[:, sl], in_=C.ps[b][0:64, :], func=AF.Copy), r=[("ps", b)], w=["qpf"])
        rotary_fm(C, qpf, qpe, L1["cosT"], L1["sinT"], L1["rotm"], QK_SCALE, "qpf", "qpe", rt1, rt2)
        for kb in range(4):
            sl = slice(kb * 512, (kb + 1) * 512)
            b = rb()
            for kc in range(4):
                P.op("pe", lambda e, b=b, s=s, kc=kc, sl=sl: e.matmul(C.ps[b][:, :], lhsT=wk[s][:, kc, 0:128], rhs=kvn[:, kc, sl], start=(kc == 0), stop=(kc == 3)),
                     r=[("wk", s)] + kin, w=[("ps", b)])
            P.op("act", lambda e, b=b, sl=sl: e.activation(out=kno[:, sl], in_=C.ps[b][:, :], func=AF.Copy), r=[("ps", b)], w=["kno"])
        for q4 in range(4):
            b = rb()
            for j in range(4):
                kt = q4 * 4 + j
                for kc in range(4):
                    P.op("pe", lambda e, b=b, s=s, kc=kc, kt=kt, j=j: e.matmul(C.ps[b][:, j * 128:(j + 1) * 128], lhsT=kvn[:, kc, kt * 128:(kt + 1) * 128], rhs=wk[s][:, kc, 128:256],
                                                                              start=(kc == 0), stop=(kc == 3)),
                         r=[("wk", s)] + kin, w=[("ps", b)])
            P.op("dve", lambda e, b=b, q4=q4: e.tensor_copy(out=vtok[:, q4 * 4:(q4 + 1) * 4, :].rearrange("p a b -> p (a b)"), in_=C.ps[b][:, :]), r=[("ps", b)], w=["vtok"])
        for qb in range(2):
            qsl = slice(qb * 512, (qb + 1) * 512)
            kts = list(range(8)) + [8 + j for j in range(4 * qb + 4)]
            for i_, kt in enumerate(kts):
                ksl = slice(kt * 128, (kt + 1) * 128)
                b = rb()
                P.op("pe", lambda e, b=b, ksl=ksl, qsl=qsl: e.matmul(C.ps[b][:, :], lhsT=kno[:, ksl], rhs=qno[:, qsl], start=True, stop=False),
                     r=["kno", "qno"], w=[("ps", b)])
                P.op("pe", lambda e, b=b, ksl=ksl, qsl=qsl: e.matmul(C.ps[b][:, :], lhsT=kpe[:, ksl], rhs=qpe[:, qsl], start=False, stop=True),
                     r=["kpe0", "kpe1", "qpe"], w=[("ps", b)])
                p_ = pti % 3
                pti += 1
                if kt < 8:
                    P.op("act", lambda e, b=b, p_=p_: e.activation(out=pt[p_], in_=C.ps[b][:, :], func=AF.Exp, bias=L1["kbias"][:, 0:1], scale=1.0),
                         r=[("ps", b), "c_kbias"], w=[("pt", p_)])
                else:
                    P.op("act", lambda e, b=b, p_=p_: e.activation(out=pt[p_], in_=C.ps[b][:, :], func=AF.Exp), r=[("ps", b)], w=[("pt", p_)])
                    j = kt - 8 - 4 * qb
                    if j >= 0:
                        P.op("pool", lambda e, p_=p_, j=j: e.tensor_tensor(out=pt[p_], in0=pt[p_], in1=L1["mask01"][:, j * 512:(j + 1) * 512], op=ALU.mult),
                             r=[("pt", p_), "c_mask01"], w=[("pt", p_)])
                first = (i_ == 0)
                last = (i_ == len(kts) - 1)
                P.op("pe", lambda e, p_=p_, kt=kt, first=first, last=last: e.matmul(C.ps[6][:, :], lhsT=vtok[:, kt, :], rhs=pt[p_], start=first, stop=last),
                     r=["vtok", ("pt", p_)], w=[("ps", 6)])
                P.op("pe", lambda e, p_=p_, first=first, last=last: e.matmul(C.ps[7][:, :], lhsT=onesb, rhs=pt[p_], start=first, stop=last),
                     r=["c_ones_b", ("pt", p_)], w=[("ps", 7)])
            P.op("dve", lambda e: e.reciprocal(out=rinv, in_=C.ps[7][:, :]), r=[("ps", 7)], w=["rinv"])
            o = obi % 2
            obi += 1
            P.op("dve", lambda e, o=o: e.tensor_tensor(out=ob[o], in0=C.ps[6][:, :], in1=rinv, op=ALU.mult), r=[("ps", 6), "rinv"], w=[("aob", o)])
            P.dma("sp", oT[h * 128:(h + 1) * 128, qsl], ob[o], r=[("aob", o)], w=["dram_oT"], semkey=("aobst", o))
    stage_end(C)


def load_l1_consts(C, attn, sfx=""):
    L1 = {}
    L1["qnw"] = C.const("qnw", C.dram_once("qnw_p", [128, 8]), [128, 8])
    L1["kvnw"] = C.const("kvnw", C.dram_once("kvnw_p", [128, 4]), [128, 4])
    L1["cosT"] = C.const("cosT", C.dram_once("cosT" + sfx, [64, NT]), [64, NT])
    L1["sinT"] = C.const("sinT", C.dram_once("sinT" + sfx, [64, NT]), [64, NT])
    L1["rotm"] = C.const("rotm", C.dram_once("rotm", [64, 64]), [64, 64])
    if attn:
        L1["ones_b"] = C.const("ones_b", C.dram_once("c_ones_b", [128, 128], BF16), [128, 128], BF16)
        L1["kbias"] = C.const("kbias", C.dram_once("kbias", [128, 1]), [128, 1])
        L1["mask01"] = C.const("mask01", C.dram_once("c_mask01", [128, 4 * 512], BF16), [128, 4 * 512], BF16)
    C.P.barrier()
    return L1


def host_l1_consts(half, q_norm_w, kv_norm_w):
    import ml_dtypes
    m = {}
    m["qnw_p"] = pp(q_norm_w[0])
    m["kvnw_p"] = pp(kv_norm_w[0])
    pos = (half * NT + np.arange(NT)).astype(np.float32)
    inv = (10000.0 ** (-np.arange(32, dtype=np.float32) * 2.0 / 64)).astype(np.float32)
    ang = pos[None, :] * inv[:, None]
    m["cosT"] = np.repeat(np.cos(ang), 2, axis=0).astype(np.float32)
    m["sinT"] = np.repeat(np.sin(ang), 2, axis=0).astype(np.float32)
    rot = np.zeros((64, 64), np.float32)
    for i in range(32):
        rot[2 * i + 1, 2 * i] = -1.0
        rot[2 * i, 2 * i + 1] = 1.0
    m["rotm"] = rot
    m["c_ones_b"] = np.ones((128, 128), ml_dtypes.bfloat16)
    m["kbias"] = np.full((128, 1), 0.0 if half == 1 else -1e30, np.float32)
    k = np.arange(128)[:, None]
    q = np.arange(512)[None, :]
    m["c_mask01"] = np.concatenate([(q >= j * 128 + k) for j in range(4)], axis=1).astype(ml_dtypes.bfloat16)
    return m


ADA_COLS = 6 * D // 8
ADA_CC = ADA_COLS // 128


def stage_ada(C, cT, adaw, adab_p, modo):
    P = C.P
    stage_begin(C)
    cs = P.alloc([128, KC, 4], F32)
    P.dma("sp", cs, cT.rearrange("(kc p) b -> p kc b", p=128), r=["dram_c"], w=["cs"])
    P.op("act", lambda e: e.activation(out=cs, in_=cs, func=AF.Silu), r=["cs"], w=["cs"])
    ab = P.alloc([128, DEPTH * ADA_CC], F32)
    P.dma("sp", ab, adab_p, r=["dram_ab"], w=["ab"])
    ob = P.alloc([128, DEPTH * ADA_CC * 4], F32)
    wb = [P.alloc([128, KC, 128], F32) for _ in range(3)]
    n = 0
    for i in range(DEPTH):
        Wv = adaw[i].rearrange("(kc p) n -> p kc n", p=128)
        for cc in range(ADA_CC):
            s = n % 3
            P.dma("sp", wb[s], Wv[:, :, cc * 128:(cc + 1) * 128], r=["dram_adaw"], w=[("awb", s)])
            b = C.bank()
            for kc in range(KC):
                P.op("pe", lambda e, b=b, s=s, kc=kc: e.matmul(C.ps[b][:, 0:4], lhsT=wb[s][:, kc, :], rhs=cs[:, kc, :], start=(kc == 0), stop=(kc == KC - 1)),
                     r=[("awb", s), "cs"], w=[("ps", b)])
            P.op("dve", lambda e, b=b, n=n: e.tensor_scalar(out=ob[:, n * 4:(n + 1) * 4], in0=C.ps[b][:, 0:4], scalar1=ab[:, n:n + 1], scalar2=None, op0=ALU.add),
                 r=[("ps", b), "ab"], w=["aob"])
            n += 1
    P.dma("sp", modo, ob, r=["aob"], w=["dram_modo"], semkey="aobst")
    stage_end(C)


def _finish(C):
    C.P.final_wait("sp")
    C.P.emit()


def build_ada():
    nc = bass.Bass("TRN2", target_bir_lowering=False)
    with contextlib.ExitStack() as stack:
        C = Ctx(nc, stack, {"cT": "in", "adaw": "in", "adab_p": "in", "modo": "out"})
        stage_ada(C, C.dram("cT", [D, 4]), C.dram("adaw", [DEPTH, D, ADA_COLS]), C.dram("adab_p", [128, DEPTH * ADA_CC]),
                  C.dram("modo", [128, DEPTH * ADA_CC * 4]))
        _finish(C)
    return nc


class _AllIn(dict):
    def __init__(self, outs, scratch_prefix="s_"):
        super().__init__()
        self.outs = set(outs)
        self.sp = scratch_prefix

    def get(self, k, default=None):
        if k in self.outs:
            return "out"
        if k.startswith(self.sp):
            return None
        return "in"


def load_consts_fused(C):
    C.const("ones_f", C.dram("c_ones", [128, 128]), [128, 128])
    C.const("ident", C.dram("c_ident", [128, 128]), [128, 128])
    C.const("lnw", C.dram("lnw_p", [128, DEPTH * 2 * KC]), [128, DEPTH * 2 * KC])
    C.const("lnb", C.dram("lnb_p", [128, DEPTH * 2 * KC]), [128, DEPTH * 2 * KC])
    modq = C.P.alloc([128, DEPTH * 6 * KC], F32)
    C.consts["modq"] = modq
    return modq


def stage_ada_own(C, modq):
    P = C.P
    stage_begin(C)
    cs = P.alloc([128, KC], F32)
    P.dma("sp", cs, C.dram("c_own_p", [128, KC]), r=["dram_c"], w=["cs"])
    P.op("act", lambda e: e.activation(out=cs, in_=cs, func=AF.Silu), r=["cs"], w=["cs"])
    cb = P.alloc([128, KC], BF16)
    P.op("dve", lambda e: e.tensor_copy(out=cb, in_=cs), r=["cs"], w=["cb"])
    ab = P.alloc([128, DEPTH * 192], F32)
    P.dma("sp", ab, C.dram("adab_full_p", [128, DEPTH * 192]), r=["dram_ab"], w=["ab"])
    wb = [P.alloc([128, KC, 512], BF16) for _ in range(3)]
    adaw = C.dram("ada_w", [DEPTH, D, 6 * D])
    n = 0
    for i in range(DEPTH):
        Wv = adaw[i].rearrange("(kc p) n -> p kc n", p=128)
        for c4 in range(48):
            s = n % 3
            n += 1
            P.dma("pool", wb[s], Wv[:, :, c4 * 512:(c4 + 1) * 512], r=["dram_adaw"], w=[("awb", s)])
            for j in range(4):
                b = C.bank()
                col = i * 192 + c4 * 4 + j
                for kc in range(KC):
                    P.op("pe", lambda e, b=b, s=s, kc=kc, j=j: e.matmul(C.ps[b][:, 0:1], lhsT=wb[s][:, kc, j * 128:(j + 1) * 128], rhs=cb[:, kc:kc + 1],
                                                                       start=(kc == 0), stop=(kc == KC - 1)),
                         r=[("awb", s), "cb"], w=[("ps", b)])
                P.op("dve", lambda e, b=b, col=col: e.tensor_tensor(out=modq[:, col:col + 1], in0=C.ps[b][:, 0:1], in1=ab[:, col:col + 1], op=ALU.add),
                     r=[("ps", b), "ab"], w=["c_modq"])
    for i in range(DEPTH):
        for s_ in (1, 4):
            sl = slice(i * 192 + s_ * 32, i * 192 + s_ * 32 + 32)
            P.op("dve", lambda e, sl=sl: e.tensor_scalar(out=modq[:, sl], in0=modq[:, sl], scalar1=1.0, scalar2=None, op0=ALU.add),
                 r=["c_modq"], w=["c_modq"])
        for s_ in (2, 5):
            sl = slice(i * 192 + s_ * 32, i * 192 + s_ * 32 + 32)
            P.op("dve", lambda e, sl=sl: e.tensor_scalar(out=modq[:, sl], in0=modq[:, sl], scalar1=1.0, scalar2=1.0 / DN_ALPHA, op0=ALU.add, op1=ALU.mult),
                 r=["c_modq"], w=["c_modq"])
    stage_end(C)


def build_fused(upto=None, outs=("outT",)):
    nc = bass.Bass("TRN2", target_bir_lowering=False)
    with contextlib.ExitStack() as stack:
        C = Ctx(nc, stack, _AllIn(list(outs)))
        P = C.P
        modq = load_consts_fused(C)
        stage_ada_own(C, modq)
        xT = C.dram("xT", [D, NT])
        xpT = C.dram("xpT", [D, NT])
        x1T = C.dram("s_x1T", [D, NT])
        v2T = C.dram("s_v2T", [D, NT])
        HT = C.dram("s_HT", [NE * 512, NT], BF16)
        x2T = C.dram("s_x2T", [D, NT])
        qnT = C.dram("s_qnT", [1024, NT], BF16)
        lw = C.consts["lnw"]
        lb = C.consts["lnb"]
        eps2 = LN_EPS / DN_ALPHA ** 2
        for ps_ in ("P", "O"):
            src = xpT if ps_ == "P" else xT
            P.mark()
            layer0_mixer(C, modq, src, x1T, 0 if ps_ == "P" else 1, "_" + ps_)
            P.barrier()
            P.release()
            stage_moe(C, 0, x1T, v2T, HT, modq, moe_weight_aps(C, "0"))
            stage_ln(C, v2T, x2T, lw[:, 32:64], lb[:, 32:64], eps2, "ln2")
            P.mark()
            L1 = load_l1_consts(C, False, "_" + ps_)
            stage_mla_proj(C, x2T, modq, C.dram("wq_a", [D, 1024]), C.dram("wkv_a", [D, 576]),
                           qnT, C.dram("s_kvnT_" + ps_, [512, NT], BF16), C.dram("s_kpeT_" + ps_, [64, NT], BF16), L1, do_q=(ps_ == "O"))
            P.barrier()
            P.release()
            if upto == ps_:
                _finish(C)
                return nc
        P.mark()
        L1 = load_l1_consts(C, True, "_O")
        oT = C.dram("s_oT", [D, NT], BF16)
        stage_attn(C, qnT, C.dram("s_kvnT_P", [512, NT], BF16), C.dram("s_kpeT_P", [64, NT], BF16),
                   C.dram("s_kvnT_O", [512, NT], BF16), C.dram("s_kpeT_O", [64, NT], BF16),
                   C.dram("wq_b", [1024, 6144]), C.dram("wkv_b", [512, 8192]), oT, L1)
        P.barrier()
        P.release()
        v3T = C.dram("s_v3T", [D, NT])
        x3T = C.dram("s_x3T", [D, NT])
        stage_proj_res(C, oT, 32, C.dram("wo", [D, D]), x2T, v3T, modq, 192 + 2 * 32, "wo")
        stage_ln(C, v3T, x3T, lw[:, 64:96], lb[:, 64:96], eps2, "ln3")
        v4T = C.dram("s_v4T", [D, NT])
        outT = C.dram("outT", [D, NT])
        stage_moe(C, 1, x3T, v4T, HT, modq, moe_weight_aps(C, "1"))
        stage_ln(C, v4T, outT, lw[:, 96:128], lb[:, 96:128], eps2, "ln4")
        _finish(C)
    return nc


def kernel(x, c, ada_w, ada_b, ln_w, ln_b, in_proj, conv_w, conv_b, dt_bias, a_log, d_skip,
           ssd_norm_w, pool_w, pool_scale, out_proj, wq_a, q_norm_w, wq_b, wkv_a, kv_norm_w,
           wkv_b, wo, router_w, router_b, w_gu, b_gu, w_down, b_down):
    A = lambda a: np.asarray(a)
    x, c, ada_w, ada_b, ln_w, ln_b = map(A, (x, c, ada_w, ada_b, ln_w, ln_b))
    cores = list(range(8))
    nc = build_fused()
    W = dict(host_consts())
    del W["c_sel"]
    W["c_sel"] = host_consts()["c_sel"]
    W["lnw_p"] = np.concatenate([pp(ln_w[i, j]) for i in range(2) for j in range(2)], axis=1)
    W["lnb_p"] = np.concatenate([pp(ln_b[i, j]) for i in range(2) for j in range(2)], axis=1)
    W["ada_w"] = ada_w
    W["adab_full_p"] = np.concatenate([pp(ada_b[i]) for i in range(DEPTH)], axis=1)
    for li in range(2):
        W.update(host_moe_weights(li, A(router_w), A(router_b), A(w_gu), A(b_gu), A(w_down), A(b_down), str(li)))
    W["in_proj"] = A(in_proj)[0]
    W["pool_w"] = A(pool_w)[0]
    W["out_proj"] = A(out_proj)[0]
    W["wq_a"] = A(wq_a)[0]
    W["wkv_a"] = A(wkv_a)[0]
    W["wq_b"] = A(wq_b)[0]
    W["wkv_b"] = A(wkv_b)[0]
    W["wo"] = A(wo)[0]
    l0 = [host_l0_consts(h, A(conv_w), A(conv_b), A(dt_bias), A(a_log), A(d_skip), A(ssd_norm_w), A(pool_scale)) for h in range(2)]
    l1 = [host_l1_consts(h, A(q_norm_w), A(kv_norm_w)) for h in range(2)]
    for k_, v_ in l0[0].items():
        if k_ not in ("flag", "invc_rep"):
            W[k_] = v_
    for k_, v_ in l1[0].items():
        if k_ not in ("cosT", "sinT", "kbias"):
            W[k_] = v_
    ins = []
    for k in cores:
        b, half = k // 2, k % 2
        m = dict(W)
        m["flag_P"] = l0[0]["flag"]
        m["invc_rep_P"] = l0[0]["invc_rep"]
        m["flag_O"] = l0[half]["flag"]
        m["invc_rep_O"] = l0[half]["invc_rep"]
        m["cosT_P"] = l1[0]["cosT"]
        m["sinT_P"] = l1[0]["sinT"]
        m["cosT_O"] = l1[half]["cosT"]
        m["sinT_O"] = l1[half]["sinT"]
        m["kbias"] = l1[half]["kbias"]
        m["c_own_p"] = pp(c[b])
        m["xT"] = np.ascontiguousarray(x[b, half * NT:(half + 1) * NT].T)
        m["xpT"] = np.ascontiguousarray(x[b, 0:NT].T)
        ins.append(m)
    r = run_bass_kernel_spmd(nc, ins, core_ids=cores)
    out = np.zeros((4, 2 * NT, D), np.float32)
    for k in cores:
        b, half = k // 2, k % 2
        out[b, half * NT:(half + 1) * NT, :] = r.results[k]["outT"].T
    return out
```

```python
import numpy as np, contextlib, time
import concourse.bass as bass
import concourse.mybir as mybir
from concourse.bass_utils import run_bass_kernel_spmd

F32 = mybir.dt.float32
BF16 = mybir.dt.bfloat16
I32 = mybir.dt.int32
AF = mybir.ActivationFunctionType
ALU = mybir.AluOpType
AX = mybir.AxisListType


class Tok:
    __slots__ = ("sem", "val", "eng")

    def __init__(self, sem, val, eng):
        self.sem = sem
        self.val = val
        self.eng = eng


class Prog:
    ENGS = ("pe", "act", "dve", "pool", "sp")
    LIMIT = 30000

    def __init__(self, nc, stack):
        self.nc = nc
        self.stack = stack
        self.q = {e: [] for e in self.ENGS}
        self.cur = {}
        self.cnt = {}
        self.nsem = 0
        for e in self.ENGS:
            self._new_eng_sem(e)
        self.lastw = {}
        self.readers = {}
        self.seen = {e: {} for e in self.ENGS}
        self.dsem = {}
        self.dcnt = {}
        self.all_tokens = {}
        self.arena = None
        self.arena_off = 0
        self.arena_marks = []

    def _sem(self, name):
        self.nsem += 1
        return self.stack.enter_context(self.nc.semaphore(name))

    def _new_eng_sem(self, e):
        self.cur[e] = self._sem("s_%s_%d" % (e, self.nsem))
        self.cnt[e] = 0

    def _deps(self, eng, r, w, is_dma):
        deps = []
        for k in r:
            t = self.lastw.get(k)
            if t is not None:
                deps.append(t)
        for k in w:
            t = self.lastw.get(k)
            if t is not None and (t.eng != eng or is_dma or t.eng is None):
                deps.append(t)
            for t in self.readers.get(k, ()):
                if t.eng != eng or is_dma or t.eng is None:
                    deps.append(t)
        out = []
        seen = self.seen[eng]
        for t in deps:
            sid = id(t.sem)
            if seen.get(sid, 0) >= t.val:
                continue
            seen[sid] = t.val
            out.append((t.sem, t.val))
        best = {}
        for s, v in out:
            if id(s) not in best or best[id(s)][1] < v:
                best[id(s)] = (s, v)
        return list(best.values())

    def _commit(self, tok, r, w):
        for k in w:
            self.lastw[k] = tok
            self.readers[k] = []
        for k in r:
            self.readers.setdefault(k, []).append(tok)
        self.all_tokens[id(tok.sem)] = tok

    def op(self, eng, fn, r=(), w=()):
        waits = self._deps(eng, r, w, False)
        if self.cnt[eng] >= self.LIMIT:
            self._new_eng_sem(eng)
        self.cnt[eng] += 1
        tok = Tok(self.cur[eng], self.cnt[eng], eng)
        self.q[eng].append((waits, fn, tok.sem, 1))
        self._commit(tok, r, w)
        return tok

    def _dma_sem(self, semkey, queue):
        if not hasattr(self, "dpool"):
            self.dpool = {}
            self.dfree = {}
        semkey = (queue, semkey)
        pool = self.dpool.setdefault(queue, [])
        free = self.dfree.setdefault(queue, [])
        ent = self.dsem.get(semkey)
        if ent is None or ent[1] >= self.LIMIT:
            ent = None
            while free:
                cand = free.pop()
                if cand[1] < self.LIMIT:
                    ent = cand
                    break
            if ent is None:
                ent = [self._sem("d%d" % self.nsem), 0]
                pool.append(ent)
            self.dsem[semkey] = ent
        ent[1] += 16
        return ent[0], ent[1]

    def dma(self, queue, out, in_, r=(), w=(), semkey=None, **kw):
        waits = self._deps(queue, r, w, True)
        if semkey is None:
            semkey = w[0] if (w and not str(w[0]).startswith("dram")) else r[0]
        sem, val = self._dma_sem(semkey, queue)
        tok = Tok(sem, val, None)
        self.q[queue].append((waits, lambda e: e.dma_start(out=out, in_=in_, **kw), tok.sem, 16))
        self._commit(tok, r, w)
        return tok

    def coll(self, fn, r=(), w=(), semkey=None):
        waits = self._deps("pool", r, w, True)
        sem, val = self._dma_sem(semkey, "pool")
        tok = Tok(sem, val, None)
        self.q["pool"].append((waits, fn, tok.sem, 16))
        self._commit(tok, r, w)
        return tok

    def barrier(self):
        toks = list(self.all_tokens.values())
        for e in self.ENGS:
            seen = self.seen[e]
            waits = []
            for t in toks:
                if t.eng == e:
                    continue
                if seen.get(id(t.sem), 0) >= t.val:
                    continue
                seen[id(t.sem)] = t.val
                waits.append((t.sem, t.val))
            if waits:
                self.q[e].append((waits, None, None, 0))
        self.lastw.clear()
        self.readers.clear()
        if hasattr(self, "dpool"):
            self.dsem.clear()
            self.dfree = {q: [e for e in lst if e[1] < self.LIMIT] for q, lst in self.dpool.items()}

    def final_wait(self, eng="sp"):
        toks = list(self.all_tokens.values())
        waits = [(t.sem, t.val) for t in toks]
        self.q[eng].append((waits, None, None, 0))

    def emit(self):
        nc = self.nc
        q = self.q

        def run(e, ename):
            for waits, fn, sem, inc in q[ename]:
                for s, v in waits:
                    e.wait_ge(s, v)
                if fn is not None:
                    ins = fn(e)
                    ins.then_inc(sem, inc)

        with nc.Block() as block:
            @block.tensor
            def _(e):
                run(e, "pe")

            @block.scalar
            def _(e):
                run(e, "act")

            @block.vector
            def _(e):
                run(e, "dve")

            @block.gpsimd
            def _(e):
                run(e, "pool")

            @block.sync
            def _(e):
                run(e, "sp")

    def init_arena(self, nbytes):
        self.arena = self.stack.enter_context(self.nc.sbuf_tensor("arena", [128, nbytes // 4], F32))
        self.arena_bytes = nbytes
        self.arena_off = 0

    def mark(self):
        self.arena_marks.append(self.arena_off)

    def release(self):
        self.arena_off = self.arena_marks.pop()

    def alloc(self, shape, dt):
        n = 1
        for s in shape[1:]:
            n *= s
        esz = 2 if dt == BF16 else 4
        nb = (n * esz + 31) // 32 * 32
        assert self.arena_off + nb <= self.arena_bytes, ("SBUF arena overflow", self.arena_off, nb)
        a = self.arena[:, self.arena_off // 4:(self.arena_off + nb) // 4]
        self.arena_off += nb
        if dt != F32:
            a = a.bitcast(dt)
        a = a[:, 0:n]
        if len(shape) == 3:
            a = a.rearrange("p (a b) -> p a b", a=shape[1])
        elif len(shape) == 4:
            a = a.rearrange("p (a b c) -> p a b c", a=shape[1], b=shape[2])
        if shape[0] != 128:
            a = a[0:shape[0]]
        return a


D = 4096
KC = 32
NT = 1024
NE = 32
DEPTH = 2
DN_ALPHA = (2 * DEPTH) ** 0.25
LN_EPS = 1e-5
RMS_EPS = 1e-6


class Ctx:
    def __init__(self, nc, stack, ext):
        self.nc = nc
        self.stack = stack
        self.ext = ext
        self.P = Prog(nc, stack)
        self.P.init_arena(190 * 1024)
        self.ps = [stack.enter_context(nc.psum_tensor("ps%d" % i, [128, 512], F32)) for i in range(8)]
        self.pi = 0
        self.consts = {}

    def dram(self, name, shape, dt=F32):
        if not hasattr(self, "_drams"):
            self._drams = {}
        if name in self._drams:
            return self._drams[name]
        k = self.ext.get(name)
        kind = {"in": "ExternalInput", "out": "ExternalOutput", None: "Internal"}[k]
        ap = self.nc.dram_tensor(name, list(shape), dt, kind=kind).ap()
        self._drams[name] = ap
        return ap

    def dram_once(self, name, shape, dt=F32):
        return self.dram(name, shape, dt)

    def bank(self):
        b = self.pi % 8
        self.pi += 1
        return b

    def const(self, name, dram_ap, shape, dt=F32, queue="sp"):
        t = self.P.alloc(shape, dt)
        self.P.dma(queue, t, dram_ap, r=["dram_c_" + name], w=["c_" + name])
        self.consts[name] = t
        return t


def stage_begin(C):
    C.P.barrier()
    C.P.mark()


def stage_end(C):
    C.P.barrier()
    C.P.release()


def stage_ln(C, vT, outT, w_p, b_p, eps, tag):
    P = C.P
    stage_begin(C)
    ones = C.consts["ones_f"]
    zb = [P.alloc([128, NT], F32) for _ in range(3)]
    sq = [P.alloc([128, NT], F32) for _ in range(2)]
    for kc in range(KC):
        s = kc % 3
        q = kc % 2
        P.dma("sp", zb[s], vT[kc * 128:(kc + 1) * 128, :], r=["dram_" + tag + "v"], w=[("zb", s)])
        P.op("act", lambda e, s=s, q=q: e.activation(out=sq[q], in_=zb[s], func=AF.Square), r=[("zb", s)], w=[("sq", q)])
        for hb in range(2):
            P.op("pe", lambda e, s=s, hb=hb, kc=kc: e.matmul(C.ps[hb][:, :], lhsT=ones, rhs=zb[s][:, hb * 512:(hb + 1) * 512],
                                                             start=(kc == 0), stop=(kc == KC - 1)),
                 r=[("zb", s), "c_ones_f"], w=[("ps", hb)])
            P.op("pe", lambda e, q=q, hb=hb, kc=kc: e.matmul(C.ps[2 + hb][:, :], lhsT=ones, rhs=sq[q][:, hb * 512:(hb + 1) * 512],
                                                             start=(kc == 0), stop=(kc == KC - 1)),
                 r=[("sq", q), "c_ones_f"], w=[("ps", 2 + hb)])
    mean = P.alloc([128, NT], F32)
    rstd = P.alloc([128, NT], F32)
    tmp = P.alloc([128, NT], F32)
    for hb in range(2):
        sl = slice(hb * 512, (hb + 1) * 512)
        P.op("dve", lambda e, hb=hb, sl=sl: e.tensor_scalar(out=mean[:, sl], in0=C.ps[hb][:, :], scalar1=1.0 / D, scalar2=None, op0=ALU.mult),
             r=[("ps", hb)], w=[("mean", hb)])
        P.op("dve", lambda e, sl=sl: e.tensor_tensor(out=tmp[:, sl], in0=mean[:, sl], in1=mean[:, sl], op=ALU.mult),
             r=[("mean", hb)], w=[("tmp", hb)])
        P.op("dve", lambda e, hb=hb, sl=sl: e.scalar_tensor_tensor(out=rstd[:, sl], in0=C.ps[2 + hb][:, :], scalar=1.0 / D, in1=tmp[:, sl],
                                                                   op0=ALU.mult, op1=ALU.subtract),
             r=[("ps", 2 + hb), ("tmp", hb)], w=[("rstd", hb)])
        P.op("dve", lambda e, sl=sl: e.tensor_scalar(out=rstd[:, sl], in0=rstd[:, sl], scalar1=float(eps), scalar2=None, op0=ALU.add),
             r=[("rstd", hb)], w=[("rstd", hb)])
        P.op("act", lambda e, sl=sl: e.activation(out=rstd[:, sl], in_=rstd[:, sl], func=AF.Sqrt), r=[("rstd", hb)], w=[("rstd", hb)])
        P.op("dve", lambda e, sl=sl: e.reciprocal(out=rstd[:, sl], in_=rstd[:, sl]), r=[("rstd", hb)], w=[("rstd", hb)])
    for kc in range(KC):
        s = kc % 3
        q = kc % 2
        P.dma("sp", zb[s], vT[kc * 128:(kc + 1) * 128, :], r=["dram_" + tag + "v"], w=[("zb", s)])
        P.op("dve", lambda e, s=s: e.tensor_tensor(out=zb[s], in0=zb[s], in1=mean, op=ALU.subtract),
             r=[("zb", s), ("mean", 0), ("mean", 1)], w=[("zb", s)])
        P.op("pool", lambda e, s=s: e.tensor_tensor(out=zb[s], in0=zb[s], in1=rstd, op=ALU.mult),
             r=[("zb", s), ("rstd", 0), ("rstd", 1)], w=[("zb", s)])
        P.op("act", lambda e, s=s, q=q, kc=kc: e.activation(out=sq[q], in_=zb[s], func=AF.Identity, bias=b_p[:, kc:kc + 1], scale=w_p[:, kc:kc + 1]),
             r=[("zb", s)], w=[("sq", q)])
        P.dma("sp", outT[kc * 128:(kc + 1) * 128, :], sq[q], r=[("sq", q)], w=["dram_" + tag + "o"], semkey=("lnst", q))
    stage_end(C)


def stage_moe(C, li, x1T, vT, HT, modq, W):
    P = C.P
    ones = C.consts["ones_f"]
    ident = C.consts["ident"]
    base = li * 192
    stage_begin(C)
    hff = P.alloc([128, KC, NT], BF16)
    P.mark()
    rw = P.alloc([128, KC, NE], F32)
    P.dma("sp", rw, W["router_w"].rearrange("(kc p) n -> p kc n", p=128), r=["dram_rw"], w=["rw"])
    rb = P.alloc([128, NE], F32)
    P.dma("sp", rb, W["router_b_rep"], r=["dram_rb"], w=["rb"])
    xb = [P.alloc([128, NT], F32) for _ in range(3)]
    for kc in range(KC):
        s = kc % 3
        P.dma("sp", xb[s], x1T[kc * 128:(kc + 1) * 128, :], r=["dram_x1"], w=[("xb", s)])
        P.op("dve", lambda e, s=s, kc=kc: e.tensor_scalar(out=xb[s], in0=xb[s], scalar1=modq[:, base + 4 * 32 + kc:base + 4 * 32 + kc + 1],
                                                          scalar2=modq[:, base + 3 * 32 + kc:base + 3 * 32 + kc + 1], op0=ALU.mult, op1=ALU.add),
             r=[("xb", s), "modq"], w=[("xb", s)])
        P.op("act", lambda e, s=s, kc=kc: e.activation(out=hff[:, kc, :], in_=xb[s], func=AF.Copy), r=[("xb", s)], w=[("hff", kc)])
        for tt in range(8):
            P.op("pe", lambda e, s=s, kc=kc, tt=tt: e.matmul(C.ps[tt][:, 0:NE], lhsT=xb[s][:, tt * 128:(tt + 1) * 128], rhs=rw[:, kc, :],
                                                             start=(kc == 0), stop=(kc == KC - 1)),
                 r=[("xb", s), "rw"], w=[("ps", tt)])
    PT = P.alloc([128, NT], F32)
    lg = [P.alloc([128, NE], F32) for _ in range(2)]
    t8 = [P.alloc([128, 8], F32) for _ in range(2)]
    mk = [P.alloc([128, NE], F32) for _ in range(2)]
    ex = [P.alloc([128, NE], F32) for _ in range(2)]
    sm = [P.alloc([128, 2], F32) for _ in range(2)]
    for tt in range(8):
        s = tt % 2
        K = lambda n, s=s: (n, s)
        P.op("dve", lambda e, s=s, tt=tt: e.tensor_tensor(out=lg[s], in0=C.ps[tt][:, 0:NE], in1=rb, op=ALU.add),
             r=[("ps", tt), "rb"], w=[K("lg")])
        P.op("dve", lambda e, s=s: e.max(out=t8[s], in_=lg[s]), r=[K("lg")], w=[K("t8")])
        P.op("dve", lambda e, s=s: e.tensor_scalar(out=mk[s], in0=lg[s], scalar1=t8[s][:, 3:4], scalar2=None, op0=ALU.is_ge),
             r=[K("lg"), K("t8")], w=[K("mk")])
        P.op("dve", lambda e, s=s: e.tensor_scalar(out=sm[s][:, 0:1], in0=t8[s][:, 0:1], scalar1=-1.0, scalar2=None, op0=ALU.mult),
             r=[K("t8")], w=[K("sm0")])
        P.op("act", lambda e, s=s: e.activation(out=ex[s], in_=lg[s], func=AF.Exp, bias=sm[s][:, 0:1], scale=1.0),
             r=[K("lg"), K("sm0")], w=[K("ex")])
        P.op("dve", lambda e, s=s: e.tensor_tensor(out=ex[s], in0=ex[s], in1=mk[s], op=ALU.mult), r=[K("ex"), K("mk")], w=[K("ex")])
        P.op("dve", lambda e, s=s: e.reduce_sum(out=sm[s][:, 1:2], in_=ex[s], axis=AX.X), r=[K("ex")], w=[K("sm1")])
        P.op("dve", lambda e, s=s: e.reciprocal(out=sm[s][:, 1:2], in_=sm[s][:, 1:2]), r=[K("sm1")], w=[K("sm1")])
        P.op("dve", lambda e, s=s: e.tensor_scalar(out=ex[s], in0=ex[s], scalar1=sm[s][:, 1:2], scalar2=None, op0=ALU.mult),
             r=[K("ex"), K("sm1")], w=[K("ex")])
        pb = tt // 4
        P.op("pe", lambda e, s=s, tt=tt, pb=pb: e.transpose(C.ps[pb][0:NE, (tt % 4) * 128:(tt % 4 + 1) * 128], ex[s], ident),
             r=[K("ex"), "c_ident"] + ([("ps", pb)] if tt % 4 else []), w=[("ps", pb)])
    for pb in range(2):
        P.op("act", lambda e, pb=pb: e.activation(out=PT[0:NE, pb * 512:(pb + 1) * 512], in_=C.ps[pb][0:NE, :], func=AF.Copy),
             r=[("ps", pb)], w=[("PT", pb)])
    P.barrier()
    P.release()
    PTk = P.alloc([128, NT], F32)
    P.op("dve", lambda e: e.tensor_copy(out=PTk[0:NE, :], in_=PT[0:NE, :]), r=[], w=["PTk"])
    P.barrier()
    bgu = P.alloc([128, NE * 8], F32)
    P.dma("sp", bgu, W["bgu_p"], r=["dram_bgu"], w=["bgu"])
    sel = P.alloc([NE, NE * 128], F32)
    P.dma("sp", sel, C.dram_once("c_sel", [NE, NE * 128]), r=["dram_sel"], w=["c_sel"])
    wb = [P.alloc([128, KC, 256], BF16) for _ in range(3)]
    pbc = [P.alloc([128, 512], F32) for _ in range(4)]
    tg = [P.alloc([128, 512], F32) for _ in range(2)]
    tsg = [P.alloc([128, 512], F32) for _ in range(2)]
    tu = [P.alloc([128, 512], F32) for _ in range(2)]
    ho = [P.alloc([128, 512], BF16) for _ in range(4)]
    wi = 0
    ui = 0
    hoi = 0
    for ex_i in range(NE):
        for hb in range(2):
            b = C.bank()
            s4 = (ex_i * 2 + hb) % 4
            P.op("pe", lambda e, b=b, ex_i=ex_i, hb=hb: e.matmul(C.ps[b][:, :], lhsT=sel[0:NE, ex_i * 128:(ex_i + 1) * 128],
                                                                 rhs=PTk[0:NE, hb * 512:(hb + 1) * 512], start=True, stop=True),
                 r=["PTk", "c_sel"], w=[("ps", b)])
            P.op("act", lambda e, b=b, s4=s4: e.activation(out=pbc[s4], in_=C.ps[b][:, :], func=AF.Copy), r=[("ps", b)], w=[("pbc", s4)])
        for fc in range(4):
            s = wi % 3
            wi += 1
            P.dma("pool", wb[s], W["wgu_h"][ex_i].rearrange("(kc p) n -> p kc n", p=128)[:, :, fc * 256:(fc + 1) * 256],
                  r=["dram_wgu"], w=[("wb", s)])
            for hb in range(2):
                bg_ = C.bank()
                bu_ = C.bank()
                for kc in range(KC):
                    P.op("pe", lambda e, b=bg_, s=s, kc=kc, hb=hb: e.matmul(C.ps[b][:, :], lhsT=wb[s][:, kc, 0:128], rhs=hff[:, kc, hb * 512:(hb + 1) * 512],
                                                                            start=(kc == 0), stop=(kc == KC - 1)),
                         r=[("wb", s), ("hff", kc)], w=[("ps", bg_)])
                for kc in range(KC):
                    P.op("pe", lambda e, b=bu_, s=s, kc=kc, hb=hb: e.matmul(C.ps[b][:, :], lhsT=wb[s][:, kc, 128:256], rhs=hff[:, kc, hb * 512:(hb + 1) * 512],
                                                                            start=(kc == 0), stop=(kc == KC - 1)),
                         r=[("wb", s), ("hff", kc)], w=[("ps", bu_)])
                u = ui % 2
                ui += 1
                o = hoi % 4
                hoi += 1
                s4 = (ex_i * 2 + hb) % 4
                cg = (ex_i * 4 + fc) * 2
                P.op("dve", lambda e, b=bg_, u=u, cg=cg: e.tensor_scalar(out=tg[u], in0=C.ps[b][:, :], scalar1=bgu[:, cg:cg + 1], scalar2=7.0, op0=ALU.add, op1=ALU.min),
                     r=[("ps", bg_), "bgu"], w=[("tg", u)])
                P.op("act", lambda e, u=u: e.activation(out=tsg[u], in_=tg[u], func=AF.Sigmoid, scale=1.702), r=[("tg", u)], w=[("tsg", u)])
                P.op("dve", lambda e, b=bu_, u=u, cg=cg: e.tensor_scalar(out=tu[u], in0=C.ps[b][:, :], scalar1=bgu[:, cg + 1:cg + 2], scalar2=7.0, op0=ALU.add, op1=ALU.min),
                     r=[("ps", bu_), "bgu"], w=[("tu", u)])
                P.op("pool", lambda e, u=u: e.tensor_scalar(out=tu[u], in0=tu[u], scalar1=-7.0, scalar2=1.0, op0=ALU.max, op1=ALU.add),
                     r=[("tu", u)], w=[("tu", u)])
                P.op("pool", lambda e, u=u: e.tensor_tensor(out=tg[u], in0=tg[u], in1=tsg[u], op=ALU.mult), r=[("tg", u), ("tsg", u)], w=[("tg", u)])
                P.op("pool", lambda e, u=u: e.tensor_tensor(out=tg[u], in0=tg[u], in1=tu[u], op=ALU.mult), r=[("tg", u), ("tu", u)], w=[("tg", u)])
                P.op("dve", lambda e, u=u, o=o, s4=s4: e.tensor_tensor(out=ho[o], in0=tg[u], in1=pbc[s4], op=ALU.mult),
                     r=[("tg", u), ("pbc", s4)], w=[("ho", o)])
                row = (ex_i * 4 + fc) * 128
                P.dma("sp", HT[row:row + 128, hb * 512:(hb + 1) * 512], ho[o], r=[("ho", o)], w=["dram_HT"], semkey=("host", o))
    P.barrier()
    P.release()
    P.mark()
    PT2 = P.alloc([128, NT], F32)
    P.op("dve", lambda e: e.tensor_copy(out=PT2[0:NE, :], in_=PTk[0:NE, :]), r=[], w=["PT2"])
    P.barrier()
    bd = P.alloc([128, D], F32)
    P.dma("sp", bd[0:NE, :], W["b_down"], r=["dram_bd"], w=["bd"])
    ht = [P.alloc([128, 4, NT], BF16) for _ in range(3)]
    wd = [P.alloc([128, 4, 512], BF16) for _ in range(3)]
    xr = [P.alloc([128, 512], F32) for _ in range(4)]
    gq0 = base + 5 * 32
    hi = 0
    xi = 0
    for dg in range(8):
        for ex_i in range(NE):
            s = hi % 3
            hi += 1
            P.dma("sp", ht[s], HT[ex_i * 512:(ex_i + 1) * 512, :].rearrange("(fc p) t -> p fc t", p=128), r=["dram_HT"], w=[("ht", s)])
            P.dma("pool", wd[s], W["w_down"][ex_i].rearrange("(fc p) n -> p fc n", p=128)[:, :, dg * 512:(dg + 1) * 512],
                  r=["dram_wd"], w=[("wd", s)])
            for dc in range(4):
                for hb in range(2):
                    b = dc * 2 + hb
                    for fc in range(4):
                        P.op("pe", lambda e, b=b, s=s, fc=fc, dc=dc, hb=hb, ex_i=ex_i: e.matmul(
                            C.ps[b][:, :], lhsT=wd[s][:, fc, dc * 128:(dc + 1) * 128], rhs=ht[s][:, fc, hb * 512:(hb + 1) * 512],
                            start=(ex_i == 0 and fc == 0), stop=False),
                            r=[("wd", s), ("ht", s)], w=[("ps", b)])
        for dc in range(4):
            for hb in range(2):
                b = dc * 2 + hb
                col = dg * 512 + dc * 128
                P.op("pe", lambda e, b=b, col=col, hb=hb: e.matmul(C.ps[b][:, :], lhsT=bd[0:NE, col:col + 128], rhs=PT2[0:NE, hb * 512:(hb + 1) * 512],
                                                                   start=False, stop=True),
                     r=["bd", "PT2"], w=[("ps", b)])
                x = xi % 4
                xi += 1
                kcx = dg * 4 + dc
                P.dma("sp", xr[x], x1T[col:col + 128, hb * 512:(hb + 1) * 512], r=["dram_x1"], w=[("xr", x)])
                P.op("dve", lambda e, b=b, x=x, kcx=kcx: e.scalar_tensor_tensor(out=xr[x], in0=C.ps[b][:, :], scalar=modq[:, gq0 + kcx:gq0 + kcx + 1], in1=xr[x],
                                                                                op0=ALU.mult, op1=ALU.add),
                     r=[("ps", b), ("xr", x), "modq"], w=[("xr", x)])
                P.dma("sp", vT[col:col + 128, hb * 512:(hb + 1) * 512], xr[x], r=[("xr", x)], w=["dram_v"], semkey=("xrst", x))
    stage_end(C)


def load_consts(C):
    P = C.P
    C.const("ones_f", C.dram("c_ones", [128, 128]), [128, 128])
    C.const("ident", C.dram("c_ident", [128, 128]), [128, 128])
    C.const("lnw", C.dram("lnw_p", [128, DEPTH * 2 * KC]), [128, DEPTH * 2 * KC])
    C.const("lnb", C.dram("lnb_p", [128, DEPTH * 2 * KC]), [128, DEPTH * 2 * KC])
    modq = C.const("modq", C.dram("modp", [128, DEPTH * 6 * KC]), [128, DEPTH * 6 * KC])
    for i in range(DEPTH):
        for s_ in (1, 4):
            sl = slice(i * 192 + s_ * 32, i * 192 + s_ * 32 + 32)
            P.op("dve", lambda e, sl=sl: e.tensor_scalar(out=modq[:, sl], in0=modq[:, sl], scalar1=1.0, scalar2=None, op0=ALU.add),
                 r=["c_modq"], w=["c_modq"])
        for s_ in (2, 5):
            sl = slice(i * 192 + s_ * 32, i * 192 + s_ * 32 + 32)
            P.op("dve", lambda e, sl=sl: e.tensor_scalar(out=modq[:, sl], in0=modq[:, sl], scalar1=1.0, scalar2=1.0 / DN_ALPHA, op0=ALU.add, op1=ALU.mult),
                 r=["c_modq"], w=["c_modq"])
    P.barrier()
    return modq


def host_consts():
    c = {}
    c["c_ones"] = np.ones((128, 128), np.float32)
    c["c_ident"] = np.eye(128, dtype=np.float32)
    sel = np.zeros((NE, NE, 128), np.float32)
    for e in range(NE):
        sel[e, e, :] = 1.0
    c["c_sel"] = sel.reshape(NE, NE * 128)
    c["c_Us"] = (np.arange(128)[:, None] < np.arange(128)[None, :]).astype(np.float32)
    c["c_iota"] = np.ascontiguousarray(np.broadcast_to(np.arange(256, dtype=np.float32)[None, :], (128, 256)))
    c["c_slotid"] = np.stack([np.arange(128), np.arange(128) + 128], axis=1).astype(np.float32)
    return c


def pp(v):
    return np.ascontiguousarray(v.reshape(-1, 128).T)


def host_moe_weights(li, router_w, router_b, w_gu, b_gu, w_down, b_down, sfx):
    m = {}
    m["router_w" + sfx] = np.ascontiguousarray(router_w[li])
    m["router_b_rep" + sfx] = np.ascontiguousarray(np.broadcast_to(router_b[li][None, :], (128, NE)))
    g = w_gu[li].reshape(NE, D, 4, 128, 2).transpose(0, 1, 2, 4, 3).reshape(NE, D, 1024)
    m["wgu_h" + sfx] = np.ascontiguousarray(g)
    bg = b_gu[li].reshape(NE, 4, 128, 2).transpose(2, 0, 1, 3).reshape(128, NE * 8)
    m["bgu_p" + sfx] = np.ascontiguousarray(bg)
    m["w_down" + sfx] = np.ascontiguousarray(w_down[li])
    m["b_down" + sfx] = np.ascontiguousarray(b_down[li])
    return m


def moe_weight_aps(C, sfx):
    return {
        "router_w": C.dram("router_w" + sfx, [D, NE]),
        "router_b_rep": C.dram("router_b_rep" + sfx, [128, NE]),
        "wgu_h": C.dram("wgu_h" + sfx, [NE, D, 1024]),
        "bgu_p": C.dram("bgu_p" + sfx, [128, NE * 8]),
        "w_down": C.dram("w_down" + sfx, [NE, 512, D]),
        "b_down": C.dram("b_down" + sfx, [NE, D]),
    }


def gemm_fm(C, act, KCn, Wv, col0, ncols, toks, epi, tag, wbufs, actkey):
    P = C.P
    for g0 in range(col0, col0 + ncols, 256):
        gw = min(256, col0 + ncols - g0)
        s = C.wi % len(wbufs)
        C.wi += 1
        P.dma("pool", wbufs[s][:, 0:KCn, 0:gw], Wv[:, :, g0:g0 + gw], r=["dram_w" + tag], w=[("wbuf", s)])
        for j0 in range(0, gw, 128):
            cw = min(128, gw - j0)
            for (t0, tn) in toks:
                b = C.bank()
                for kc in range(KCn):
                    P.op("pe", lambda e, b=b, s=s, kc=kc, j0=j0, cw=cw, t0=t0, tn=tn: e.matmul(
                        C.ps[b][0:cw, 0:tn], lhsT=wbufs[s][:, kc, j0:j0 + cw], rhs=act[:, kc, t0:t0 + tn],
                        start=(kc == 0), stop=(kc == KCn - 1)),
                        r=[("wbuf", s), actkey], w=[("ps", b)])
                epi(b, g0 + j0, cw, t0, tn)


def gemm_tm(C, act, KCn, Wv, col0, ncols, tts, epi, tag, wbufs, actkey):
    P = C.P
    for g0 in range(col0, col0 + ncols, 256):
        gw = min(256, col0 + ncols - g0)
        s = C.wi % len(wbufs)
        C.wi += 1
        P.dma("pool", wbufs[s][:, 0:KCn, 0:gw], Wv[:, :, g0:g0 + gw], r=["dram_w" + tag], w=[("wbuf", s)])
        for tt in tts:
            b = C.bank()
            for kc in range(KCn):
                P.op("pe", lambda e, b=b, s=s, kc=kc, gw=gw, tt=tt: e.matmul(
                    C.ps[b][:, 0:gw], lhsT=act[:, kc, tt * 128:(tt + 1) * 128], rhs=wbufs[s][:, kc, 0:gw],
                    start=(kc == 0), stop=(kc == KCn - 1)),
                    r=[("wbuf", s), actkey], w=[("ps", b)])
            epi(b, tt, g0, gw)


def make_hmix(C, srcT, hm, modq, sc_col, sh_col, xb, key):
    P = C.P
    for kc in range(KC):
        s = kc % len(xb)
        P.dma("sp", xb[s], srcT[kc * 128:(kc + 1) * 128, :], r=["dram_src" + str(key)], w=[("xb", s)])
        P.op("dve", lambda e, s=s, kc=kc: e.tensor_scalar(out=hm[:, kc, :], in0=xb[s], scalar1=modq[:, sc_col + kc:sc_col + kc + 1],
                                                          scalar2=modq[:, sh_col + kc:sh_col + kc + 1], op0=ALU.mult, op1=ALU.add),
             r=[("xb", s), "c_modq"], w=[key])


O_XBC = 4096
O_DT = 4096 + 6144
O_U = O_DT + 64


def stage_inproj(C, srcT, modq, Win, xbcT, dt_tok, z_tok, uT, reg):
    P = C.P
    stage_begin(C)
    C.wi = 0
    Wv = Win.rearrange("(kc p) n -> p kc n", p=128)
    hm = P.alloc([128, KC, NT], BF16)
    xb = [P.alloc([128, NT], F32) for _ in range(2)]
    wbufs = [P.alloc([128, KC, 256], BF16) for _ in range(3)]
    ev = [P.alloc([128, 512], F32) for _ in range(4)]
    evi = [0]

    def evac_store(b, rows, cols, dst):
        o = evi[0] % 4
        evi[0] += 1
        P.op("act", lambda e, b=b, o=o: e.activation(out=ev[o][0:rows, 0:cols], in_=C.ps[b][0:rows, 0:cols], func=AF.Copy),
             r=[("ps", b)], w=[("ev", o)])
        P.dma("sp", dst, ev[o][0:rows, 0:cols], r=[("ev", o)], w=["dram_s1"], semkey=("evst", o))

    key = "hm"
    make_hmix(C, srcT, hm, modq, 1 * 32, 0 * 32, xb, key)
    toks = [(0, 512), (512, 512)]
    r0 = reg * NT
    gemm_fm(C, hm, KC, Wv, O_XBC, 6144, toks,
            lambda b, col, cw, t0, tn: evac_store(b, cw, tn, xbcT[col - O_XBC:col - O_XBC + cw, r0 + t0:r0 + t0 + tn]),
            "in", wbufs, key)
    gemm_tm(C, hm, KC, Wv, O_DT, 64, list(range(8)),
            lambda b, tt, col, gw: evac_store(b, 128, gw, dt_tok[r0 + tt * 128:r0 + (tt + 1) * 128, :]),
            "in", wbufs, key)
    gemm_fm(C, hm, KC, Wv, O_U, 4096, toks,
            lambda b, col, cw, t0, tn: evac_store(b, cw, tn, uT[col - O_U:col - O_U + cw, 16 + t0:16 + t0 + tn]),
            "in", wbufs, key)
    gemm_tm(C, hm, KC, Wv, 0, 4096, list(range(8)),
            lambda b, tt, col, gw: evac_store(b, 128, gw, z_tok[tt * 128:(tt + 1) * 128, col:col + gw]),
            "in", wbufs, key)
    stage_end(C)


def stage_conv(C, xbcT, xactT, convw, convb, reg, flag):
    P = C.P
    stage_begin(C)
    NCH = 6144 // 128
    r0 = reg * NT
    ib = [P.alloc([128, 3 + NT], F32) for _ in range(2)]
    ac = [P.alloc([128, NT], F32) for _ in range(2)]
    for c in range(NCH):
        s = c % 2
        if reg == 0:
            P.op("dve", lambda e, s=s: e.memset(ib[s][:, 0:3], 0.0), r=[], w=[("ibh", s)])
        else:
            P.dma("sp", ib[s][:, 0:3], xbcT[c * 128:(c + 1) * 128, NT - 3:NT], r=["dram_xbc"], w=[("ibh", s)])
            P.op("dve", lambda e, s=s: e.tensor_scalar(out=ib[s][:, 0:3], in0=ib[s][:, 0:3], scalar1=flag[:, 0:1], scalar2=None, op0=ALU.mult),
                 r=[("ibh", s), "c_flag"], w=[("ibh", s)])
        P.dma("sp", ib[s][:, 3:3 + NT], xbcT[c * 128:(c + 1) * 128, r0:r0 + NT], r=["dram_xbc"], w=[("ib", s)])
        P.op("dve", lambda e, s=s, c=c: e.tensor_scalar(out=ac[s], in0=ib[s][:, 0:NT], scalar1=convw[:, c * 4:c * 4 + 1], scalar2=None, op0=ALU.mult),
             r=[("ib", s), ("ibh", s), "c_convw"], w=[("ac", s)])
        for k in range(1, 4):
            P.op("dve", lambda e, s=s, c=c, k=k: e.scalar_tensor_tensor(out=ac[s], in0=ib[s][:, k:k + NT], scalar=convw[:, c * 4 + k:c * 4 + k + 1], in1=ac[s],
                                                                        op0=ALU.mult, op1=ALU.add),
                 r=[("ib", s), ("ibh", s), ("ac", s)], w=[("ac", s)])
        P.op("act", lambda e, s=s, c=c: e.activation(out=ac[s], in_=ac[s], func=AF.Silu, bias=convb[:, c:c + 1], scale=1.0),
             r=[("ac", s), "c_convb"], w=[("ac", s)])
        P.dma("sp", xactT[c * 128:(c + 1) * 128, r0:r0 + NT], ac[s], r=[("ac", s)], w=["dram_xact"], semkey=("acst", s))
    stage_end(C)


def stage_ssd(C, xactT, dt_tok, z_tok, catT, K_, reg, hstate):
    P = C.P
    stage_begin(C)
    ones = C.consts["ones_f"]
    ident = C.consts["ident"]
    U = K_["U"]
    maskneg = K_["maskneg"]
    aneg = K_["aneg"]
    dtb = K_["dtb"]
    Drow = P.alloc([128, D], F32)
    P.dma("sp", Drow, K_["Drow_d"], r=["dram_drow"], w=["c_Drow"])
    flag = K_["flag"]
    nw = K_["ssdnw"]
    xv = xactT[0:4096, :].rearrange("(kc p) t -> p kc t", p=128)
    bv = xactT[4096:5120, :].rearrange("(g p) t -> p g t", p=128)
    cv = xactT[5120:6144, :].rearrange("(g p) t -> p g t", p=128)
    hS = P.alloc([128, D], F32)
    hB = P.alloc([128, D], BF16)
    if reg == 0:
        P.op("dve", lambda e: e.memset(hS, 0.0), r=[], w=["hS"])
    else:
        P.dma("sp", hS, hstate, r=["dram_hstate"], w=["hS"])
        P.op("dve", lambda e: e.tensor_scalar(out=hS, in0=hS, scalar1=K_["flag"][:, 0:1], scalar2=None, op0=ALU.mult), r=["hS", "c_flag"], w=["hS"])
    P.op("act", lambda e: e.activation(out=hB, in_=hS, func=AF.Copy), r=["hS"], w=["hB"])
    xTc = [P.alloc([128, KC, 128], F32) for _ in range(1)]
    bTf = P.alloc([128, 8, 128], F32)
    bTb = P.alloc([128, 8, 128], BF16)
    cTb = P.alloc([128, 8, 128], BF16)
    dtr = P.alloc([128, 64], F32)
    sA = P.alloc([128, 64], F32)
    sB = P.alloc([128, 64], F32)
    dts = P.alloc([128, 64], F32)
    la = P.alloc([128, 64], F32)
    cum = P.alloc([128, 64], F32)
    ecum = P.alloc([128, 64], F32)
    toend = P.alloc([128, 64], F32)
    cdec = P.alloc([128, 64], F32)
    xtok = P.alloc([128, D], F32)
    xdt = P.alloc([128, D], BF16)
    xdte = P.alloc([128, D], BF16)
    btok = P.alloc([128, 8, 128], BF16)
    zc = P.alloc([128, D], F32)
    y = P.alloc([128, D], F32)
    cbT = P.alloc([128, 8, 128], F32)
    larep = [P.alloc([128, 4, 128], F32) for _ in range(2)]
    tmp = [P.alloc([128, 4, 128], F32) for _ in range(2)]
    MT = [P.alloc([128, 4, 128], BF16) for _ in range(2)]
    ms = P.alloc([128, 8], F32)
    cat = P.alloc([128, KC, 128], BF16)
    v3 = lambda t: t.rearrange("p (h q) -> p h q", q=64)
    bc3 = lambda t: t[:, 0:64].unsqueeze(2).to_broadcast([128, 64, 64])
    for c in range(reg * 8, reg * 8 + 8):
        own = True
        t0 = c * 128
        tl = t0 - reg * NT
        xs = 0
        P.dma("sp", xTc[xs], xv[:, :, t0:t0 + 128], r=["dram_xact"], w=[("xTc", xs)])
        P.dma("sp", bTf, bv[:, :, t0:t0 + 128], r=["dram_xact"], w=["bTf"])
        P.dma("sp", dtr, dt_tok[t0:t0 + 128, :], r=["dram_dt"], w=["dtr"])
        if own:
            P.dma("pool", bTb, bv[:, :, t0:t0 + 128], r=["dram_xact"], w=["bTb"])
            P.dma("pool", cTb, cv[:, :, t0:t0 + 128], r=["dram_xact"], w=["cTb"])
            P.dma("sp", zc, z_tok[tl:tl + 128, :], r=["dram_z"], w=["zc"])
        P.op("dve", lambda e: e.tensor_tensor(out=dtr, in0=dtr, in1=dtb, op=ALU.add), r=["dtr", "c_dtb"], w=["dtr"])
        P.op("act", lambda e: e.activation(out=sA, in_=dtr, func=AF.Abs), r=["dtr"], w=["sA"])
        P.op("act", lambda e: e.activation(out=sA, in_=sA, func=AF.Exp, scale=-1.0), r=["sA"], w=["sA"])
        P.op("act", lambda e: e.activation(out=sA, in_=sA, func=AF.Ln, bias=1.0, scale=1.0), r=["sA"], w=["sA"])
        P.op("dve", lambda e: e.tensor_scalar(out=sB, in0=dtr, scalar1=0.0, scalar2=None, op0=ALU.max), r=["dtr"], w=["sB"])
        P.op("dve", lambda e: e.tensor_tensor(out=dts, in0=sA, in1=sB, op=ALU.add), r=["sA", "sB"], w=["dts"])
        P.op("dve", lambda e: e.tensor_tensor(out=la, in0=dts, in1=aneg, op=ALU.mult), r=["dts", "c_aneg"], w=["la"])
        b1 = C.bank()
        P.op("pe", lambda e, b=b1: e.matmul(C.ps[b][:, 0:64], lhsT=U, rhs=la, start=True, stop=True), r=["la", "c_U"], w=[("ps", b1)])
        b2 = C.bank()
        P.op("pe", lambda e, b=b2: e.matmul(C.ps[b][:, 0:64], lhsT=ones, rhs=la, start=True, stop=True), r=["la", "c_ones_f"], w=[("ps", b2)])
        P.op("act", lambda e, b=b1: e.activation(out=cum, in_=C.ps[b][:, 0:64], func=AF.Copy), r=[("ps", b1)], w=["cum"])
        P.op("act", lambda e, b=b1: e.activation(out=ecum, in_=C.ps[b][:, 0:64], func=AF.Exp), r=[("ps", b1)], w=["ecum"])
        P.op("dve", lambda e, b=b2: e.tensor_tensor(out=toend, in0=C.ps[b][:, 0:64], in1=cum, op=ALU.subtract), r=[("ps", b2), "cum"], w=["toend"])
        P.op("act", lambda e: e.activation(out=toend, in_=toend, func=AF.Exp), r=["toend"], w=["toend"])
        P.op("act", lambda e, b=b2: e.activation(out=cdec, in_=C.ps[b][:, 0:64], func=AF.Exp), r=[("ps", b2)], w=["cdec"])
        for q in range(8):
            b = C.bank()
            for j in range(4):
                kc = q * 4 + j
                P.op("pe", lambda e, b=b, j=j, kc=kc, xs=xs: e.transpose(C.ps[b][:, j * 128:(j + 1) * 128], xTc[xs][:, kc, :], ident),
                     r=[("xTc", xs), "c_ident"], w=[("ps", b)])
            P.op("act", lambda e, b=b, q=q: e.activation(out=xtok[:, q * 512:(q + 1) * 512], in_=C.ps[b][:, :], func=AF.Copy),
                 r=[("ps", b)], w=[("xtok", q)])
        xk = [("xtok", q) for q in range(8)]
        P.op("dve", lambda e: e.tensor_tensor(out=v3(xdt), in0=v3(xtok), in1=bc3(dts), op=ALU.mult), r=xk + ["dts"], w=["xdt"])
        P.op("pool", lambda e: e.tensor_tensor(out=v3(xdte), in0=v3(xdt), in1=bc3(toend), op=ALU.mult), r=["xdt", "toend"], w=["xdte"])
        for q in range(2):
            b = C.bank()
            for j in range(4):
                g = q * 4 + j
                P.op("pe", lambda e, b=b, j=j, g=g: e.transpose(C.ps[b][:, j * 128:(j + 1) * 128], bTf[:, g, :], ident),
                     r=["bTf", "c_ident"], w=[("ps", b)])
            P.op("act", lambda e, b=b, q=q: e.activation(out=btok[:, q * 4:(q + 1) * 4, :].rearrange("p a b -> p (a b)"), in_=C.ps[b][:, :], func=AF.Copy),
                 r=[("ps", b)], w=[("btok", q)])
        if own:
            for g in range(8):
                b = C.bank()
                P.op("pe", lambda e, b=b, g=g: e.matmul(C.ps[b][:, :], lhsT=cTb[:, g, :], rhs=hB[:, g * 512:(g + 1) * 512], start=True, stop=True),
                     r=["cTb", "hB"], w=[("ps", b)])
                P.op("dve", lambda e, b=b, g=g: e.tensor_tensor(out=y[:, g * 512:(g + 1) * 512].rearrange("p (h q) -> p h q", q=64),
                                                                in0=C.ps[b][:, :].rearrange("p (h q) -> p h q", q=64),
                                                                in1=ecum[:, g * 8:(g + 1) * 8].unsqueeze(2).to_broadcast([128, 8, 64]), op=ALU.mult),
                     r=[("ps", b), "ecum"], w=[("y", g)])
            for q in range(2):
                b = C.bank()
                for j in range(4):
                    g = q * 4 + j
                    P.op("pe", lambda e, b=b, j=j, g=g: e.matmul(C.ps[b][:, j * 128:(j + 1) * 128], lhsT=bTb[:, g, :], rhs=cTb[:, g, :], start=True, stop=True),
                         r=["bTb", "cTb"], w=[("ps", b)])
                P.op("act", lambda e, b=b, q=q: e.activation(out=cbT[:, q * 4:(q + 1) * 4, :].rearrange("p a b -> p (a b)"), in_=C.ps[b][:, :], func=AF.Copy),
                     r=[("ps", b)], w=[("cbT", q)])
            yb = None
            for q in range(16):
                g = q // 2
                u = q % 2
                P.op("dve", lambda e, u=u, q=q: e.tensor_copy(out=larep[u], in_=la[:, q * 4:(q + 1) * 4].unsqueeze(2).to_broadcast([128, 4, 128])),
                     r=["la"], w=[("larep", u)])
                b = C.bank()
                for j in range(4):
                    P.op("pe", lambda e, b=b, j=j, u=u: e.matmul(C.ps[b][:, j * 128:(j + 1) * 128], lhsT=larep[u][:, j, :], rhs=U, start=True, stop=True),
                         r=[("larep", u), "c_U"], w=[("ps", b)])
                for j in range(4):
                    h = q * 4 + j
                    P.op("dve", lambda e, b=b, j=j, u=u, h=h: e.scalar_tensor_tensor(out=tmp[u][:, j, :], in0=C.ps[b][:, j * 128:(j + 1) * 128], scalar=cum[:, h:h + 1],
                                                                                    in1=maskneg, op0=ALU.subtract, op1=ALU.add),
                         r=[("ps", b), "cum", "c_maskneg"], w=[("tmp", u)])
                P.op("act", lambda e, u=u: e.activation(out=tmp[u], in_=tmp[u], func=AF.Exp), r=[("tmp", u)], w=[("tmp", u)])
                P.op("pool", lambda e, u=u, g=g: e.tensor_tensor(out=MT[u], in0=tmp[u], in1=cbT[:, g, :].unsqueeze(1).to_broadcast([128, 4, 128]), op=ALU.mult),
                     r=[("tmp", u), ("cbT", g // 4)], w=[("MT", u)])
                if q % 2 == 0:
                    yb = C.bank()
                for j in range(4):
                    h = q * 4 + j
                    hh = h % 8
                    P.op("pe", lambda e, yb=yb, j=j, u=u, h=h, hh=hh: e.matmul(C.ps[yb][:, hh * 64:(hh + 1) * 64], lhsT=MT[u][:, j, :], rhs=xdt[:, h * 64:(h + 1) * 64],
                                                                              start=True, stop=True),
                         r=[("MT", u), "xdt"], w=[("ps", yb)])
                if q % 2 == 1:
                    P.op("dve", lambda e, yb=yb, g=g: e.tensor_tensor(out=y[:, g * 512:(g + 1) * 512], in0=C.ps[yb][:, :], in1=y[:, g * 512:(g + 1) * 512], op=ALU.add),
                         r=[("ps", yb), ("y", g)], w=[("y", g)])
            yk = [("y", g) for g in range(8)]
            P.op("pool", lambda e: e.tensor_tensor(out=xtok, in0=xtok, in1=Drow, op=ALU.mult), r=xk + ["c_Drow", "xdt"], w=xk)
            P.op("dve", lambda e: e.tensor_tensor(out=y, in0=y, in1=xtok, op=ALU.add), r=yk + xk, w=yk)
            P.op("act", lambda e: e.activation(out=zc, in_=zc, func=AF.Silu), r=["zc"], w=["zc"])
            P.op("dve", lambda e: e.tensor_tensor(out=y, in0=y, in1=zc, op=ALU.mult), r=yk + ["zc"], w=yk)
            P.op("dve", lambda e: e.memset(ms, 0.0), r=[], w=["msr"])
            for g in range(8):
                P.op("act", lambda e, g=g: e.activation(out=zc[:, g * 512:(g + 1) * 512], in_=y[:, g * 512:(g + 1) * 512], func=AF.Square, accum_out=ms[:, g:g + 1]),
                     r=yk + ["zc", "msr"], w=["zc", "msr"])
            P.op("dve", lambda e: e.tensor_scalar(out=ms, in0=ms, scalar1=1.0 / 512, scalar2=LN_EPS, op0=ALU.mult, op1=ALU.add), r=["msr"], w=["msr"])
            P.op("act", lambda e: e.activation(out=ms, in_=ms, func=AF.Sqrt), r=["msr"], w=["msr"])
            P.op("dve", lambda e: e.reciprocal(out=ms, in_=ms), r=["msr"], w=["msr"])
            P.op("dve", lambda e: e.tensor_tensor(out=y.rearrange("p (g q) -> p g q", q=512), in0=y.rearrange("p (g q) -> p g q", q=512),
                                                  in1=ms[:, 0:8].unsqueeze(2).to_broadcast([128, 8, 512]), op=ALU.mult), r=yk + ["msr"], w=yk)
            for q in range(8):
                b = C.bank()
                for j in range(4):
                    kc = q * 4 + j
                    P.op("pe", lambda e, b=b, j=j, kc=kc: e.transpose(C.ps[b][:, j * 128:(j + 1) * 128], y[:, kc * 128:(kc + 1) * 128], ident),
                         r=yk + ["c_ident"], w=[("ps", b)])
                for j in range(4):
                    kc = q * 4 + j
                    P.op("act", lambda e, b=b, j=j, kc=kc: e.activation(out=cat[:, kc, :], in_=C.ps[b][:, j * 128:(j + 1) * 128], func=AF.Copy, scale=nw[:, kc:kc + 1]),
                         r=[("ps", b), "c_ssdnw"], w=["cat"])
            P.dma("sp", catT[0:4096, tl:tl + 128].rearrange("(kc p) t -> p kc t", p=128), cat, r=["cat"], w=["dram_cat"], semkey="catst")
        P.op("dve", lambda e: e.tensor_tensor(out=v3(hS), in0=v3(hS), in1=bc3(cdec), op=ALU.mult), r=["hS", "cdec"], w=["hS"])
        for g in range(8):
            b = C.bank()
            P.op("pe", lambda e, b=b, g=g: e.matmul(C.ps[b][:, :], lhsT=btok[:, g, :], rhs=xdte[:, g * 512:(g + 1) * 512], start=True, stop=True),
                 r=[("btok", g // 4), "xdte"], w=[("ps", b)])
            P.op("dve", lambda e, b=b, g=g: e.tensor_tensor(out=hS[:, g * 512:(g + 1) * 512], in0=hS[:, g * 512:(g + 1) * 512], in1=C.ps[b][:, :], op=ALU.add),
                 r=[("ps", b), "hS"], w=["hS"])
        P.op("act", lambda e: e.activation(out=hB, in_=hS, func=AF.Copy), r=["hS"], w=["hB"])
    if reg == 0:
        P.dma("sp", hstate, hS, r=["hS"], w=["dram_hstate"], semkey="hsst")
    stage_end(C)


def stage_pool(C, uT, catT, poolw, K_, reg, uT_prev):
    P = C.P
    stage_begin(C)
    C.wi = 0
    pscale = K_["pscale"]
    invc = K_["invc"]
    diff = P.alloc([128, KC, NT], BF16)
    ub = [P.alloc([128, 16 + NT], F32) for _ in range(2)]
    pa = P.alloc([128, 16 + NT], F32)
    pb = P.alloc([128, 16 + NT], F32)
    ic = P.alloc([128, NT], F32)
    W_ = 16 + NT
    for kc in range(KC):
        g = kc // 8
        s = kc % 2
        if kc % 8 == 0:
            P.dma("sp", ic, invc[g], r=["dram_invc"], w=["ic"])
        P.dma("sp", ub[s][:, 16:W_], uT[kc * 128:(kc + 1) * 128, 16:W_], r=["dram_u"], w=[("ub", s)])
        if reg == 0:
            P.op("dve", lambda e, s=s: e.memset(ub[s][:, 0:16], 0.0), r=[("ub", s)], w=[("ub", s)])
        else:
            P.dma("sp", ub[s][:, 0:16], uT_prev[kc * 128:(kc + 1) * 128, NT:NT + 16], r=["dram_u"], w=[("ubh", s)])
            P.op("dve", lambda e, s=s: e.tensor_scalar(out=ub[s][:, 0:16], in0=ub[s][:, 0:16], scalar1=K_["flag"][:, 0:1], scalar2=None, op0=ALU.mult),
                 r=[("ubh", s), ("ub", s), "c_flag"], w=[("ub", s)])
        src = ub[s]
        srck = ("ub", s)
        dsts = [(pa, "pa"), (pb, "pb")]
        for st in range(g + 1):
            sh = 1 << st
            dst, dk = dsts[st % 2]
            P.op("dve", lambda e, dst=dst, src=src, sh=sh: e.tensor_tensor(out=dst[:, sh:W_], in0=src[:, sh:W_], in1=src[:, 0:W_ - sh], op=ALU.add),
                 r=[srck], w=[dk])
            src, srck = dst, dk
        P.op("pool", lambda e, src=src: e.tensor_tensor(out=src[:, 16:W_], in0=src[:, 16:W_], in1=ic, op=ALU.mult), r=[srck, "ic"], w=[srck])
        P.op("dve", lambda e, src=src, s=s, kc=kc: e.tensor_tensor(out=diff[:, kc, :], in0=src[:, 16:W_], in1=ub[s][:, 16:W_], op=ALU.subtract),
             r=[srck, ("ub", s)], w=["diff"])
    wbufs = [P.alloc([128, 8, 256], BF16) for _ in range(3)]
    ev = [P.alloc([128, 512], BF16) for _ in range(4)]
    evi = [0]

    def epi(b, col, cw, t0, tn, g):
        o = evi[0] % 4
        evi[0] += 1
        d0 = g * 1024 + col
        kcx = d0 // 128
        P.op("act", lambda e, b=b, o=o: e.activation(out=ev[o][:, 0:tn], in_=C.ps[b][:, 0:tn], func=AF.Copy, scale=pscale[:, kcx:kcx + 1]),
             r=[("ps", b), "c_pscale"], w=[("ev", o)])
        P.dma("sp", catT[4096 + d0:4096 + d0 + 128, t0:t0 + tn], ev[o][:, 0:tn], r=[("ev", o)], w=["dram_cat"], semkey=("pevst", o))

    for g in range(4):
        Wv = poolw[g].rearrange("(kc p) n -> p kc n", p=128)
        gemm_fm(C, diff[:, g * 8:(g + 1) * 8, :], 8, Wv, 0, 1024, [(0, 512), (512, 512)],
                lambda b, col, cw, t0, tn, g=g: epi(b, col, cw, t0, tn, g), "pool", wbufs, "diff")
    stage_end(C)


def stage_proj_res(C, actT, KCn, Wd, resT, vT, modq, gcol, tag):
    P = C.P
    stage_begin(C)
    C.wi = 0
    Wv = Wd.rearrange("(kc p) n -> p kc n", p=128)
    act = P.alloc([128, KCn, 512], BF16)
    wbufs = [P.alloc([128, KCn, 256], BF16) for _ in range(3)]
    xr = [P.alloc([128, 512], F32) for _ in range(4)]
    xi = [0]
    for hb in range(2):
        P.dma("sp", act, actT[:, hb * 512:(hb + 1) * 512].rearrange("(kc p) t -> p kc t", p=128), r=["dram_" + tag + "a"], w=["pact"])

        def epi(b, col, cw, t0, tn, hb=hb):
            x = xi[0] % 4
            xi[0] += 1
            kcx = col // 128
            P.dma("sp", xr[x], resT[col:col + 128, hb * 512:(hb + 1) * 512], r=["dram_" + tag + "r"], w=[("xr", x)])
            P.op("dve", lambda e, b=b, x=x: e.scalar_tensor_tensor(out=xr[x], in0=C.ps[b][:, :], scalar=modq[:, gcol + kcx:gcol + kcx + 1], in1=xr[x],
                                                                   op0=ALU.mult, op1=ALU.add),
                 r=[("ps", b), ("xr", x), "c_modq"], w=[("xr", x)])
            P.dma("sp", vT[col:col + 128, hb * 512:(hb + 1) * 512], xr[x], r=[("xr", x)], w=["dram_" + tag + "v"], semkey=("prst", x))

        gemm_fm(C, act, KCn, Wv, 0, D, [(0, 512)], epi, tag, wbufs, "pact")
    stage_end(C)


def load_l0_consts(C, sfx=""):
    P = C.P
    K_ = {}
    K_["U"] = C.const("U", C.dram("c_U", [128, 128]), [128, 128])
    K_["maskneg"] = C.const("maskneg", C.dram("c_maskneg", [128, 128]), [128, 128])
    K_["aneg"] = C.const("aneg", C.dram("alog_rep", [128, 64]), [128, 64])
    K_["dtb"] = C.const("dtb", C.dram("dtb_rep", [128, 64]), [128, 64])
    K_["Drow_d"] = C.dram("drow_rep", [128, D])
    K_["flag"] = C.const("flag", C.dram("flag" + sfx, [128, 1]), [128, 1])
    K_["ssdnw"] = C.const("ssdnw", C.dram("ssdnw_p", [128, KC]), [128, KC])
    K_["pscale"] = C.const("pscale", C.dram("pscale_p", [128, KC]), [128, KC])
    K_["convw"] = C.const("convw", C.dram("convw_p", [128, 192]), [128, 192])
    K_["convb"] = C.const("convb", C.dram("convb_p", [128, 48]), [128, 48])
    K_["invc"] = C.dram("invc_rep" + sfx, [4, 128, NT])
    a = K_["aneg"]
    P.op("act", lambda e: e.activation(out=a, in_=a, func=AF.Exp), r=["c_aneg"], w=["c_aneg"])
    P.op("dve", lambda e: e.tensor_scalar(out=a, in0=a, scalar1=-1.0, scalar2=None, op0=ALU.mult), r=["c_aneg"], w=["c_aneg"])
    P.barrier()
    return K_


def host_l0_consts(half, conv_w, conv_b, dt_bias, a_log, d_skip, ssd_norm_w, pool_scale):
    m = {}
    m["c_U"] = np.triu(np.ones((128, 128), np.float32))
    m["c_maskneg"] = np.where(np.arange(128)[None, :] >= np.arange(128)[:, None], 0.0, -1e30).astype(np.float32)
    rep = lambda v: np.ascontiguousarray(np.broadcast_to(v[None, :], (128, v.shape[0])))
    m["alog_rep"] = rep(a_log[0])
    m["dtb_rep"] = rep(dt_bias[0])
    m["drow_rep"] = rep(np.repeat(d_skip[0], 64))
    m["flag"] = np.full((128, 1), float(half), np.float32)
    m["ssdnw_p"] = pp(ssd_norm_w[0])
    m["pscale_p"] = pp(pool_scale[0])
    m["convw_p"] = np.ascontiguousarray(conv_w[0].reshape(4, 48, 128).transpose(2, 1, 0).reshape(128, 192))
    m["convb_p"] = pp(conv_b[0])
    tg = half * NT + np.arange(NT) + 1
    ic = np.stack([1.0 / np.minimum(tg, w).astype(np.float32) for w in (2, 4, 8, 16)], 0)
    m["invc_rep"] = np.ascontiguousarray(np.broadcast_to(ic[:, None, :], (4, 128, NT))).astype(np.float32)
    return m


def layer0_mixer(C, modq, srcT, x1T, reg, sfx=""):
    K_ = load_l0_consts(C, sfx)
    Win = C.dram("in_proj", [D, 14400])
    poolw = C.dram("pool_w", [4, 1024, 1024])
    Wout = C.dram("out_proj", [2 * D, D])
    xbcT = C.dram("s_xbcT", [6144, 2 * NT])
    xactT = C.dram("s_xactT", [6144, 2 * NT])
    dt_tok = C.dram("s_dt", [2 * NT, 64])
    z_tok = C.dram("s_z", [NT, D])
    uT = C.dram("s_uT%d" % reg, [D, 16 + NT])
    uT_prev = C.dram("s_uT0", [D, 16 + NT])
    hstate = C.dram("s_hstate", [128, D])
    catT = C.dram("s_catT", [2 * D, NT], BF16)
    v1T = C.dram("s_v1T", [D, NT])
    stage_inproj(C, srcT, modq, Win, xbcT, dt_tok, z_tok, uT, reg)
    stage_conv(C, xbcT, xactT, K_["convw"], K_["convb"], reg, K_["flag"])
    stage_ssd(C, xactT, dt_tok, z_tok, catT, K_, reg, hstate)
    stage_pool(C, uT, catT, poolw, K_, reg, uT_prev)
    stage_proj_res(C, catT, 64, Wout, srcT, v1T, modq, 0 * 192 + 2 * 32, "op")
    lw = C.consts["lnw"]
    lb = C.consts["lnb"]
    stage_ln(C, v1T, x1T, lw[:, 0:32], lb[:, 0:32], LN_EPS / DN_ALPHA ** 2, "ln1")


QK_SCALE = 192 ** -0.5


def rms_feat(C, src, nch, wp, dstT, nfeat, tagk):
    P = C.P
    ones = C.consts["ones_f"]
    sq = [P.alloc([128, NT], F32) for _ in range(2)]
    rstd = P.alloc([128, NT], F32)
    ob = [P.alloc([128, NT], BF16) for _ in range(2)]
    for c in range(nch):
        q = c % 2
        P.op("act", lambda e, c=c, q=q: e.activation(out=sq[q], in_=src[:, c, :], func=AF.Square), r=[tagk], w=[("rsq", q)])
        for hb in range(2):
            P.op("pe", lambda e, q=q, hb=hb, c=c: e.matmul(C.ps[hb][:, :], lhsT=ones, rhs=sq[q][:, hb * 512:(hb + 1) * 512], start=(c == 0), stop=(c == nch - 1)),
                 r=[("rsq", q), "c_ones_f"], w=[("ps", hb)])
    for hb in range(2):
        sl = slice(hb * 512, (hb + 1) * 512)
        P.op("dve", lambda e, hb=hb, sl=sl: e.tensor_scalar(out=rstd[:, sl], in0=C.ps[hb][:, :], scalar1=1.0 / nfeat, scalar2=RMS_EPS, op0=ALU.mult, op1=ALU.add),
             r=[("ps", hb)], w=["rrstd"])
    P.op("act", lambda e: e.activation(out=rstd, in_=rstd, func=AF.Sqrt), r=["rrstd"], w=["rrstd"])
    P.op("dve", lambda e: e.reciprocal(out=rstd, in_=rstd), r=["rrstd"], w=["rrstd"])
    for c in range(nch):
        q = c % 2
        P.op("dve", lambda e, c=c, q=q: e.tensor_tensor(out=sq[q], in0=src[:, c, :], in1=rstd, op=ALU.mult), r=[tagk, "rrstd"], w=[("rsq", q)])
        P.op("act", lambda e, c=c, q=q: e.activation(out=ob[q], in_=sq[q], func=AF.Copy, scale=wp[:, c:c + 1]), r=[("rsq", q)], w=[("rob", q)])
        P.dma("sp", dstT[c * 128:(c + 1) * 128, :], ob[q], r=[("rob", q)], w=["dram_rms" + str(tagk)], semkey=("robst", q))


def rotary_fm(C, src, dst_bf, cosT, sinT, rotm, scale, key_in, key_out, t1, t2):
    P = C.P
    for hb in range(2):
        sl = slice(hb * 512, (hb + 1) * 512)
        b = 2 + hb
        P.op("pe", lambda e, b=b, sl=sl: e.matmul(C.ps[b][0:64, :], lhsT=rotm, rhs=src[:, sl], start=True, stop=True), r=[key_in, "c_rotm"], w=[("ps", b)])
        P.op("dve", lambda e, b=b, sl=sl: e.tensor_tensor(out=t1[:, sl], in0=C.ps[b][0:64, :], in1=sinT[:, sl], op=ALU.mult), r=[("ps", b), "c_sinT"], w=[("rt1", hb)])
        P.op("pool", lambda e, sl=sl: e.tensor_tensor(out=t2[:, sl], in0=src[:, sl], in1=cosT[:, sl], op=ALU.mult), r=[key_in, "c_cosT"], w=[("rt2", hb)])
        P.op("dve", lambda e, sl=sl: e.tensor_tensor(out=t1[:, sl], in0=t1[:, sl], in1=t2[:, sl], op=ALU.add), r=[("rt1", hb), ("rt2", hb)], w=[("rt1", hb)])
        P.op("act", lambda e, sl=sl: e.activation(out=dst_bf[:, sl], in_=t1[:, sl], func=AF.Copy, scale=float(scale)), r=[("rt1", hb)], w=[key_out])


def stage_mla_proj(C, x2T, modq, Wqa, Wkva, qnT, kvnT, kpeT, L1, do_q=True):
    P = C.P
    stage_begin(C)
    C.wi = 0
    hm = P.alloc([128, KC, NT], BF16)
    xb = [P.alloc([128, NT], F32) for _ in range(2)]
    wbufs = [P.alloc([128, KC, 256], BF16) for _ in range(2)]
    make_hmix(C, x2T, hm, modq, 192 + 1 * 32, 192 + 0 * 32, xb, "hm1")
    toks = [(0, 512), (512, 512)]
    P.mark()
    if do_q:
        qa = P.alloc([128, 8, NT], F32)
        gemm_fm(C, hm, KC, Wqa.rearrange("(kc p) n -> p kc n", p=128), 0, 1024, toks,
                lambda b, col, cw, t0, tn: P.op("act", lambda e, b=b: e.activation(out=qa[:, col // 128, t0:t0 + tn], in_=C.ps[b][:, 0:tn], func=AF.Copy),
                                                r=[("ps", b)], w=["qa"]),
                "qa", wbufs, "hm1")
        P.barrier()
        rms_feat(C, qa, 8, L1["qnw"], qnT, 1024, "qa")
    P.barrier()
    P.release()
    kvc = P.alloc([128, 5, NT], F32)
    gemm_fm(C, hm, KC, Wkva.rearrange("(kc p) n -> p kc n", p=128), 0, 576, toks,
            lambda b, col, cw, t0, tn: P.op("act", lambda e, b=b: e.activation(out=kvc[0:cw, col // 128, t0:t0 + tn], in_=C.ps[b][0:cw, 0:tn], func=AF.Copy),
                                            r=[("ps", b)], w=["kvc"]),
            "kva", wbufs, "hm1")
    P.barrier()
    rms_feat(C, kvc, 4, L1["kvnw"], kvnT, 512, "kvc")
    kpb = P.alloc([64, NT], BF16)
    rt1 = P.alloc([64, NT], F32)
    rt2 = P.alloc([64, NT], F32)
    rotary_fm(C, kvc[0:64, 4, :], kpb, L1["cosT"], L1["sinT"], L1["rotm"], 1.0, "kvc", "kpb", rt1, rt2)
    P.dma("sp", kpeT, kpb, r=["kpb"], w=["dram_kpe"], semkey="kpbst")
    stage_end(C)


def stage_attn(C, qnT, kvnT_prev, kpeT_prev, kvnT, kpeT, Wqb, Wkvb, oT, L1):
    P = C.P
    stage_begin(C)
    onesb = L1["ones_b"]
    qn = P.alloc([128, 8, NT], BF16)
    kvn = P.alloc([128, 4, 2 * NT], BF16)
    kpe = P.alloc([64, 2 * NT], BF16)
    P.dma("sp", qn, qnT.rearrange("(kc p) t -> p kc t", p=128), r=["dram_qn"], w=["qn"])
    P.dma("sp", kvn[:, :, 0:NT], kvnT_prev.rearrange("(kc p) t -> p kc t", p=128), r=["dram_kvp"], w=["kvn0"])
    P.dma("sp", kvn[:, :, NT:2 * NT], kvnT.rearrange("(kc p) t -> p kc t", p=128), r=["dram_kv"], w=["kvn1"])
    P.dma("sp", kpe[:, 0:NT], kpeT_prev, r=["dram_kpp"], w=["kpe0"])
    P.dma("sp", kpe[:, NT:2 * NT], kpeT, r=["dram_kp"], w=["kpe1"])
    kin = ["kvn0", "kvn1"]
    wq = [P.alloc([128, 8, 192], BF16) for _ in range(2)]
    wk = [P.alloc([128, 4, 256], BF16) for _ in range(2)]
    qno = P.alloc([128, NT], BF16)
    qpf = P.alloc([64, NT], F32)
    qpe = P.alloc([64, NT], BF16)
    rt1 = P.alloc([64, NT], F32)
    rt2 = P.alloc([64, NT], F32)
    kno = P.alloc([128, 2 * NT], BF16)
    vtok = P.alloc([128, 16, 128], BF16)
    pt = [P.alloc([128, 512], BF16) for _ in range(3)]
    rinv = P.alloc([128, 512], F32)
    ob = [P.alloc([128, 512], BF16) for _ in range(2)]
    Wqv = Wqb.rearrange("(kc p) n -> p kc n", p=128)
    Wkv = Wkvb.rearrange("(kc p) n -> p kc n", p=128)
    rot = [0]

    def rb():
        b = rot[0] % 6
        rot[0] += 1
        return b
    pti = 0
    obi = 0
    for h in range(32):
        s = h % 2
        P.dma("pool", wq[s], Wqv[:, :, h * 192:(h + 1) * 192], r=["dram_wqb"], w=[("wq", s)])
        P.dma("pool", wk[s], Wkv[:, :, h * 256:(h + 1) * 256], r=["dram_wkvb"], w=[("wk", s)])
        for hb in range(2):
            sl = slice(hb * 512, (hb + 1) * 512)
            b = rb()
            for kc in range(8):
                P.op("pe", lambda e, b=b, s=s, kc=kc, sl=sl: e.matmul(C.ps[b][:, :], lhsT=wq[s][:, kc, 0:128], rhs=qn[:, kc, sl], start=(kc == 0), stop=(kc == 7)),
                     r=[("wq", s), "qn"], w=[("ps", b)])
            P.op("act", lambda e, b=b, sl=sl: e.activation(out=qno[:, sl], in_=C.ps[b][:, :], func=AF.Copy, scale=float(QK_SCALE)), r=[("ps", b)], w=["qno"])
            b = rb()
            for kc in range(8):
                P.op("pe", lambda e, b=b, s=s, kc=kc, sl=sl: e.matmul(C.ps[b][0:64, :], lhsT=wq[s][:, kc, 128:192], rhs=qn[:, kc, sl], start=(kc == 0), stop=(kc == 7)),
                     r=[("wq", s), "qn"], w=[("ps", b)])
            P.op("act", lambda e, b=b, sl=sl: e.activation(out=qpf[:, sl], in_=C.ps[b][0:64, :], func=AF.Copy), r=[("ps", b)], w=["qpf"])
        rotary_fm(C, qpf, qpe, L1["cosT"], L1["sinT"], L1["rotm"], QK_SCALE, "qpf", "qpe", rt1, rt2)
        for kb in range(4):
            sl = slice(kb * 512, (kb + 1) * 512)
            b = rb()
            for kc in range(4):
                P.op("pe", lambda e, b=b, s=s, kc=kc, sl=sl: e.matmul(C.ps[b][:, :], lhsT=wk[s][:, kc, 0:128], rhs=kvn[:, kc, sl], start=(kc == 0), stop=(kc == 3)),
                     r=[("wk", s)] + kin, w=[("ps", b)])
            P.op("act", lambda e, b=b, sl=sl: e.activation(out=kno[:, sl], in_=C.ps[b][:, :], func=AF.Copy), r=[("ps", b)], w=["kno"])
        for q4 in range(4):
            b = rb()
            for j in range(4):
                kt = q4 * 4 + j
                for kc in range(4):
                    P.op("pe", lambda e, b=b, s=s, kc=kc, kt=kt, j=j: e.matmul(C.ps[b][:, j * 128:(j + 1) * 128], lhsT=kvn[:, kc, kt * 128:(kt + 1) * 128], rhs=wk[s][:, kc, 128:256],
                                                                              start=(kc == 0), stop=(kc == 3)),
                         r=[("wk", s)] + kin, w=[("ps", b)])
            P.op("dve", lambda e, b=b, q4=q4: e.tensor_copy(out=vtok[:, q4 * 4:(q4 + 1) * 4, :].rearrange("p a b -> p (a b)"), in_=C.ps[b][:, :]), r=[("ps", b)], w=["vtok"])
        for qb in range(2):
            qsl = slice(qb * 512, (qb + 1) * 512)
            kts = list(range(8)) + [8 + j for j in range(4 * qb + 4)]
            for i_, kt in enumerate(kts):
                ksl = slice(kt * 128, (kt + 1) * 128)
                b = rb()
                P.op("pe", lambda e, b=b, ksl=ksl, qsl=qsl: e.matmul(C.ps[b][:, :], lhsT=kno[:, ksl], rhs=qno[:, qsl], start=True, stop=False),
                     r=["kno", "qno"], w=[("ps", b)])
                P.op("pe", lambda e, b=b, ksl=ksl, qsl=qsl: e.matmul(C.ps[b][:, :], lhsT=kpe[:, ksl], rhs=qpe[:, qsl], start=False, stop=True),
                     r=["kpe0", "kpe1", "qpe"], w=[("ps", b)])
                p_ = pti % 3
                pti += 1
                if kt < 8:
                    P.op("act", lambda e, b=b, p_=p_: e.activation(out=pt[p_], in_=C.ps[b][:, :], func=AF.Exp, bias=L1["kbias"][:, 0:1], scale=1.0),
                         r=[("ps", b), "c_kbias"], w=[("pt", p_)])
                else:
                    P.op("act", lambda e, b=b, p_=p_: e.activation(out=pt[p_], in_=C.ps[b][:, :], func=AF.Exp), r=[("ps", b)], w=[("pt", p_)])
                    j = kt - 8 - 4 * qb
                    if j >= 0:
                        P.op("pool", lambda e, p_=p_, j=j: e.tensor_tensor(out=pt[p_], in0=pt[p_], in1=L1["mask01"][:, j * 512:(j + 1) * 512], op=ALU.mult),
                             r=[("pt", p_), "c_mask01"], w=[("pt", p_)])
                first = (i_ == 0)
                last = (i_ == len(kts) - 1)
                P.op("pe", lambda e, p_=p_, kt=kt, first=first, last=last: e.matmul(C.ps[6][:, :], lhsT=vtok[:, kt, :], rhs=pt[p_], start=first, stop=last),
                     r=["vtok", ("pt", p_)], w=[("ps", 6)])
                P.op("pe", lambda e, p_=p_, first=first, last=last: e.matmul(C.ps[7][:, :], lhsT=onesb, rhs=pt[p_], start=first, stop=last),
                     r=["c_ones_b", ("pt", p_)], w=[("ps", 7)])
            P.op("dve", lambda e: e.reciprocal(out=rinv, in_=C.ps[7][:, :]), r=[("ps", 7)], w=["rinv"])
            o = obi % 2
            obi += 1
            P.op("dve", lambda e, o=o: e.tensor_tensor(out=ob[o], in0=C.ps[6][:, :], in1=rinv, op=ALU.mult), r=[("ps", 6), "rinv"], w=[("aob", o)])
            P.dma("sp", oT[h * 128:(h + 1) * 128, qsl], ob[o], r=[("aob", o)], w=["dram_oT"], semkey=("aobst", o))
    stage_end(C)


def load_l1_consts(C, attn, sfx=""):
    L1 = {}
    L1["qnw"] = C.const("qnw", C.dram_once("qnw_p", [128, 8]), [128, 8])
    L1["kvnw"] = C.const("kvnw", C.dram_once("kvnw_p", [128, 4]), [128, 4])
    L1["cosT"] = C.const("cosT", C.dram_once("cosT" + sfx, [64, NT]), [64, NT])
    L1["sinT"] = C.const("sinT", C.dram_once("sinT" + sfx, [64, NT]), [64, NT])
    L1["rotm"] = C.const("rotm", C.dram_once("rotm", [64, 64]), [64, 64])
    if attn:
        L1["ones_b"] = C.const("ones_b", C.dram_once("c_ones_b", [128, 128], BF16), [128, 128], BF16)
        L1["kbias"] = C.const("kbias", C.dram_once("kbias", [128, 1]), [128, 1])
        L1["mask01"] = C.const("mask01", C.dram_once("c_mask01", [128, 4 * 512], BF16), [128, 4 * 512], BF16)
    C.P.barrier()
    return L1


def host_l1_consts(half, q_norm_w, kv_norm_w):
    import ml_dtypes
    m = {}
    m["qnw_p"] = pp(q_norm_w[0])
    m["kvnw_p"] = pp(kv_norm_w[0])
    pos = (half * NT + np.arange(NT)).astype(np.float32)
    inv = (10000.0 ** (-np.arange(32, dtype=np.float32) * 2.0 / 64)).astype(np.float32)
    ang = pos[None, :] * inv[:, None]
    m["cosT"] = np.repeat(np.cos(ang), 2, axis=0).astype(np.float32)
    m["sinT"] = np.repeat(np.sin(ang), 2, axis=0).astype(np.float32)
    rot = np.zeros((64, 64), np.float32)
    for i in range(32):
        rot[2 * i + 1, 2 * i] = -1.0
        rot[2 * i, 2 * i + 1] = 1.0
    m["rotm"] = rot
    m["c_ones_b"] = np.ones((128, 128), ml_dtypes.bfloat16)
    m["kbias"] = np.full((128, 1), 0.0 if half == 1 else -1e30, np.float32)
    k = np.arange(128)[:, None]
    q = np.arange(512)[None, :]
    m["c_mask01"] = np.concatenate([(q >= j * 128 + k) for j in range(4)], axis=1).astype(ml_dtypes.bfloat16)
    return m


ADA_COLS = 6 * D // 8
ADA_CC = ADA_COLS // 128


def stage_ada(C, cT, adaw, adab_p, modo):
    P = C.P
    stage_begin(C)
    cs = P.alloc([128, KC, 4], F32)
    P.dma("sp", cs, cT.rearrange("(kc p) b -> p kc b", p=128), r=["dram_c"], w=["cs"])
    P.op("act", lambda e: e.activation(out=cs, in_=cs, func=AF.Silu), r=["cs"], w=["cs"])
    ab = P.alloc([128, DEPTH * ADA_CC], F32)
    P.dma("sp", ab, adab_p, r=["dram_ab"], w=["ab"])
    ob = P.alloc([128, DEPTH * ADA_CC * 4], F32)
    wb = [P.alloc([128, KC, 128], F32) for _ in range(3)]
    n = 0
    for i in range(DEPTH):
        Wv = adaw[i].rearrange("(kc p) n -> p kc n", p=128)
        for cc in range(ADA_CC):
            s = n % 3
            P.dma("sp", wb[s], Wv[:, :, cc * 128:(cc + 1) * 128], r=["dram_adaw"], w=[("awb", s)])
            b = C.bank()
            for kc in range(KC):
                P.op("pe", lambda e, b=b, s=s, kc=kc: e.matmul(C.ps[b][:, 0:4], lhsT=wb[s][:, kc, :], rhs=cs[:, kc, :], start=(kc == 0), stop=(kc == KC - 1)),
                     r=[("awb", s), "cs"], w=[("ps", b)])
            P.op("dve", lambda e, b=b, n=n: e.tensor_scalar(out=ob[:, n * 4:(n + 1) * 4], in0=C.ps[b][:, 0:4], scalar1=ab[:, n:n + 1], scalar2=None, op0=ALU.add),
                 r=[("ps", b), "ab"], w=["aob"])
            n += 1
    P.dma("sp", modo, ob, r=["aob"], w=["dram_modo"], semkey="aobst")
    stage_end(C)


def _finish(C):
    C.P.final_wait("sp")
    C.P.emit()


def build_ada():
    nc = bass.Bass("TRN2", target_bir_lowering=False)
    with contextlib.ExitStack() as stack:
        C = Ctx(nc, stack, {"cT": "in", "adaw": "in", "adab_p": "in", "modo": "out"})
        stage_ada(C, C.dram("cT", [D, 4]), C.dram("adaw", [DEPTH, D, ADA_COLS]), C.dram("adab_p", [128, DEPTH * ADA_CC]),
                  C.dram("modo", [128, DEPTH * ADA_CC * 4]))
        _finish(C)
    return nc


class _AllIn(dict):
    def __init__(self, outs, scratch_prefix="s_"):
        super().__init__()
        self.outs = set(outs)
        self.sp = scratch_prefix

    def get(self, k, default=None):
        if k in self.outs:
            return "out"
        if k.startswith(self.sp):
            return None
        return "in"


def load_consts_fused(C):
    C.const("ones_f", C.dram("c_ones", [128, 128]), [128, 128])
    C.const("ident", C.dram("c_ident", [128, 128]), [128, 128])
    C.const("lnw", C.dram("lnw_p", [128, DEPTH * 2 * KC]), [128, DEPTH * 2 * KC])
    C.const("lnb", C.dram("lnb_p", [128, DEPTH * 2 * KC]), [128, DEPTH * 2 * KC])
    modq = C.P.alloc([128, DEPTH * 6 * KC], F32)
    C.consts["modq"] = modq
    return modq


def stage_ada_own(C, modq):
    P = C.P
    stage_begin(C)
    cs = P.alloc([128, KC], F32)
    P.dma("sp", cs, C.dram("c_own_p", [128, KC]), r=["dram_c"], w=["cs"])
    P.op("act", lambda e: e.activation(out=cs, in_=cs, func=AF.Silu), r=["cs"], w=["cs"])
    cb = P.alloc([128, KC], BF16)
    P.op("dve", lambda e: e.tensor_copy(out=cb, in_=cs), r=["cs"], w=["cb"])
    ab = P.alloc([128, DEPTH * 192], F32)
    P.dma("sp", ab, C.dram("adab_full_p", [128, DEPTH * 192]), r=["dram_ab"], w=["ab"])
    wb = [P.alloc([128, KC, 512], BF16) for _ in range(3)]
    adaw = C.dram("ada_w", [DEPTH, D, 6 * D])
    n = 0
    for i in range(DEPTH):
        Wv = adaw[i].rearrange("(kc p) n -> p kc n", p=128)
        for c4 in range(48):
            s = n % 3
            n += 1
            P.dma("pool", wb[s], Wv[:, :, c4 * 512:(c4 + 1) * 512], r=["dram_adaw"], w=[("awb", s)])
            for j in range(4):
                b = C.bank()
                col = i * 192 + c4 * 4 + j
                for kc in range(KC):
                    P.op("pe", lambda e, b=b, s=s, kc=kc, j=j: e.matmul(C.ps[b][:, 0:1], lhsT=wb[s][:, kc, j * 128:(j + 1) * 128], rhs=cb[:, kc:kc + 1],
                                                                       start=(kc == 0), stop=(kc == KC - 1)),
                         r=[("awb", s), "cb"], w=[("ps", b)])
                P.op("dve", lambda e, b=b, col=col: e.tensor_tensor(out=modq[:, col:col + 1], in0=C.ps[b][:, 0:1], in1=ab[:, col:col + 1], op=ALU.add),
                     r=[("ps", b), "ab"], w=["c_modq"])
    for i in range(DEPTH):
        for s_ in (1, 4):
            sl = slice(i * 192 + s_ * 32, i * 192 + s_ * 32 + 32)
            P.op("dve", lambda e, sl=sl: e.tensor_scalar(out=modq[:, sl], in0=modq[:, sl], scalar1=1.0, scalar2=None, op0=ALU.add),
                 r=["c_modq"], w=["c_modq"])
        for s_ in (2, 5):
            sl = slice(i * 192 + s_ * 32, i * 192 + s_ * 32 + 32)
            P.op("dve", lambda e, sl=sl: e.tensor_scalar(out=modq[:, sl], in0=modq[:, sl], scalar1=1.0, scalar2=1.0 / DN_ALPHA, op0=ALU.add, op1=ALU.mult),
                 r=["c_modq"], w=["c_modq"])
    stage_end(C)


def build_fused(upto=None, outs=("outT",)):
    nc = bass.Bass("TRN2", target_bir_lowering=False)
    with contextlib.ExitStack() as stack:
        C = Ctx(nc, stack, _AllIn(list(outs)))
        P = C.P
        modq = load_consts_fused(C)
        stage_ada_own(C, modq)
        xT = C.dram("xT", [D, NT])
        xpT = C.dram("xpT", [D, NT])
        x1T = C.dram("s_x1T", [D, NT])
        v2T = C.dram("s_v2T", [D, NT])
        YE = C.dram("s_YE", [NE * 2 * 128, D], BF16)
        STP = C.dram("s_STP", [NE * 2 * 128, NT], BF16)
        x2T = C.dram("s_x2T", [D, NT])
        qnT = C.dram("s_qnT", [1024, NT], BF16)
        lw = C.consts["lnw"]
        lb = C.consts["lnb"]
        eps2 = LN_EPS / DN_ALPHA ** 2
        for ps_ in ("P", "O"):
            src = xpT if ps_ == "P" else xT
            P.mark()
            layer0_mixer(C, modq, src, x1T, 0 if ps_ == "P" else 1, "_" + ps_)
            P.barrier()
            P.release()
            stage_moe_c(C, 0, x1T, v2T, YE, STP, modq, moe_weight_aps(C, "0"))
            stage_ln(C, v2T, x2T, lw[:, 32:64], lb[:, 32:64], eps2, "ln2")
            P.mark()
            L1 = load_l1_consts(C, False, "_" + ps_)
            stage_mla_proj(C, x2T, modq, C.dram("wq_a", [D, 1024]), C.dram("wkv_a", [D, 576]),
                           qnT, C.dram("s_kvnT_" + ps_, [512, NT], BF16), C.dram("s_kpeT_" + ps_, [64, NT], BF16), L1, do_q=(ps_ == "O"))
            P.barrier()
            P.release()
            if upto == ps_:
                _finish(C)
                return nc
        P.mark()
        L1 = load_l1_consts(C, True, "_O")
        oT = C.dram("s_oT", [D, NT], BF16)
        stage_attn(C, qnT, C.dram("s_kvnT_P", [512, NT], BF16), C.dram("s_kpeT_P", [64, NT], BF16),
                   C.dram("s_kvnT_O", [512, NT], BF16), C.dram("s_kpeT_O", [64, NT], BF16),
                   C.dram("wq_b", [1024, 6144]), C.dram("wkv_b", [512, 8192]), oT, L1)
        P.barrier()
        P.release()
        v3T = C.dram("s_v3T", [D, NT])
        x3T = C.dram("s_x3T", [D, NT])
        stage_proj_res(C, oT, 32, C.dram("wo", [D, D]), x2T, v3T, modq, 192 + 2 * 32, "wo")
        stage_ln(C, v3T, x3T, lw[:, 64:96], lb[:, 64:96], eps2, "ln3")
        v4T = C.dram("s_v4T", [D, NT])
        outT = C.dram("outT", [D, NT])
        stage_moe_c(C, 1, x3T, v4T, YE, STP, modq, moe_weight_aps(C, "1"))
        stage_ln(C, v4T, outT, lw[:, 96:128], lb[:, 96:128], eps2, "ln4")
        _finish(C)
    return nc


def kernel(x, c, ada_w, ada_b, ln_w, ln_b, in_proj, conv_w, conv_b, dt_bias, a_log, d_skip,
           ssd_norm_w, pool_w, pool_scale, out_proj, wq_a, q_norm_w, wq_b, wkv_a, kv_norm_w,
           wkv_b, wo, router_w, router_b, w_gu, b_gu, w_down, b_down):
    A = lambda a: np.asarray(a)
    x, c, ada_w, ada_b, ln_w, ln_b = map(A, (x, c, ada_w, ada_b, ln_w, ln_b))
    cores = list(range(8))
    nc = build_fused()
    W = dict(host_consts())
    del W["c_sel"]
    W["c_sel"] = host_consts()["c_sel"]
    W["lnw_p"] = np.concatenate([pp(ln_w[i, j]) for i in range(2) for j in range(2)], axis=1)
    W["lnb_p"] = np.concatenate([pp(ln_b[i, j]) for i in range(2) for j in range(2)], axis=1)
    W["ada_w"] = ada_w
    W["adab_full_p"] = np.concatenate([pp(ada_b[i]) for i in range(DEPTH)], axis=1)
    for li in range(2):
        W.update(host_moe_weights(li, A(router_w), A(router_b), A(w_gu), A(b_gu), A(w_down), A(b_down), str(li)))
    W["in_proj"] = A(in_proj)[0]
    W["pool_w"] = A(pool_w)[0]
    W["out_proj"] = A(out_proj)[0]
    W["wq_a"] = A(wq_a)[0]
    W["wkv_a"] = A(wkv_a)[0]
    W["wq_b"] = A(wq_b)[0]
    W["wkv_b"] = A(wkv_b)[0]
    W["wo"] = A(wo)[0]
    l0 = [host_l0_consts(h, A(conv_w), A(conv_b), A(dt_bias), A(a_log), A(d_skip), A(ssd_norm_w), A(pool_scale)) for h in range(2)]
    l1 = [host_l1_consts(h, A(q_norm_w), A(kv_norm_w)) for h in range(2)]
    for k_, v_ in l0[0].items():
        if k_ not in ("flag", "invc_rep"):
            W[k_] = v_
    for k_, v_ in l1[0].items():
        if k_ not in ("cosT", "sinT", "kbias"):
            W[k_] = v_
    ins = []
    for k in cores:
        b, half = k // 2, k % 2
        m = dict(W)
        m["flag_P"] = l0[0]["flag"]
        m["invc_rep_P"] = l0[0]["invc_rep"]
        m["flag_O"] = l0[half]["flag"]
        m["invc_rep_O"] = l0[half]["invc_rep"]
        m["cosT_P"] = l1[0]["cosT"]
        m["sinT_P"] = l1[0]["sinT"]
        m["cosT_O"] = l1[half]["cosT"]
        m["sinT_O"] = l1[half]["sinT"]
        m["kbias"] = l1[half]["kbias"]
        m["c_own_p"] = pp(c[b])
        m["xT"] = np.ascontiguousarray(x[b, half * NT:(half + 1) * NT].T)
        m["xpT"] = np.ascontiguousarray(x[b, 0:NT].T)
        ins.append(m)
    r = run_bass_kernel_spmd(nc, ins, core_ids=cores)
    out = np.zeros((4, 2 * NT, D), np.float32)
    for k in cores:
        b, half = k // 2, k % 2
        out[b, half * NT:(half + 1) * NT, :] = r.results[k]["outT"].T
    return out


CAP = 256


def stage_moe_c(C, li, x1T, vT, YE, STP, modq, W):
    P = C.P
    ones = C.consts["ones_f"]
    ident = C.consts["ident"]
    base = li * 192
    stage_begin(C)
    hfft = P.alloc([128, 8, D], BF16)
    PTk = P.alloc([128, NT], F32)
    POSMT = P.alloc([128, NT], F32)
    posm = P.alloc([128, 8, NE], F32)
    P.mark()
    rw = P.alloc([128, KC, NE], F32)
    P.dma("sp", rw, W["router_w"].rearrange("(kc p) n -> p kc n", p=128), r=["dram_rw"], w=["rw"])
    rb = P.alloc([128, NE], F32)
    P.dma("sp", rb, W["router_b_rep"], r=["dram_rb"], w=["rb"])
    us = P.alloc([128, 128], F32)
    P.dma("sp", us, C.dram("c_Us", [128, 128]), r=["dram_us"], w=["us"])
    xb = [P.alloc([128, NT], F32) for _ in range(2)]
    sc_c = base + 4 * 32
    sh_c = base + 3 * 32
    for kc in range(KC):
        s = kc % 2
        P.dma("sp", xb[s], x1T[kc * 128:(kc + 1) * 128, :], r=["dram_x1"], w=[("xb", s)])
        P.op("dve", lambda e, s=s, kc=kc: e.tensor_scalar(out=xb[s], in0=xb[s], scalar1=modq[:, sc_c + kc:sc_c + kc + 1],
                                                          scalar2=modq[:, sh_c + kc:sh_c + kc + 1], op0=ALU.mult, op1=ALU.add),
             r=[("xb", s), "c_modq"], w=[("xb", s)])
        for tt in range(8):
            P.op("pe", lambda e, s=s, kc=kc, tt=tt: e.matmul(C.ps[tt][:, 0:NE], lhsT=xb[s][:, tt * 128:(tt + 1) * 128], rhs=rw[:, kc, :],
                                                             start=(kc == 0), stop=(kc == KC - 1)),
                 r=[("xb", s), "rw"], w=[("ps", tt)])
    mkall = P.alloc([128, 8, NE], F32)
    exall = P.alloc([128, 8, NE], F32)
    lg = [P.alloc([128, NE], F32) for _ in range(2)]
    t8 = [P.alloc([128, 8], F32) for _ in range(2)]
    sm = [P.alloc([128, 2], F32) for _ in range(2)]
    for tt in range(8):
        s = tt % 2
        K = lambda n, s=s: (n, s)
        mk_ = mkall[:, tt, :]
        ex_ = exall[:, tt, :]
        P.op("dve", lambda e, s=s, tt=tt: e.tensor_tensor(out=lg[s], in0=C.ps[tt][:, 0:NE], in1=rb, op=ALU.add), r=[("ps", tt), "rb"], w=[K("lg")])
        P.op("dve", lambda e, s=s: e.max(out=t8[s], in_=lg[s]), r=[K("lg")], w=[K("t8")])
        P.op("dve", lambda e, s=s, mk_=mk_: e.tensor_scalar(out=mk_, in0=lg[s], scalar1=t8[s][:, 3:4], scalar2=None, op0=ALU.is_ge),
             r=[K("lg"), K("t8")], w=[("mk", tt)])
        P.op("dve", lambda e, s=s: e.tensor_scalar(out=sm[s][:, 0:1], in0=t8[s][:, 0:1], scalar1=-1.0, scalar2=None, op0=ALU.mult), r=[K("t8")], w=[K("sm0")])
        P.op("act", lambda e, s=s, ex_=ex_: e.activation(out=ex_, in_=lg[s], func=AF.Exp, bias=sm[s][:, 0:1], scale=1.0), r=[K("lg"), K("sm0")], w=[("ex", tt)])
        P.op("dve", lambda e, ex_=ex_, mk_=mk_: e.tensor_tensor(out=ex_, in0=ex_, in1=mk_, op=ALU.mult), r=[("ex", tt), ("mk", tt)], w=[("ex", tt)])
        P.op("dve", lambda e, s=s, ex_=ex_: e.reduce_sum(out=sm[s][:, 1:2], in_=ex_, axis=AX.X), r=[("ex", tt)], w=[K("sm1")])
        P.op("dve", lambda e, s=s: e.reciprocal(out=sm[s][:, 1:2], in_=sm[s][:, 1:2]), r=[K("sm1")], w=[K("sm1")])
        P.op("dve", lambda e, s=s, ex_=ex_: e.tensor_scalar(out=ex_, in0=ex_, scalar1=sm[s][:, 1:2], scalar2=None, op0=ALU.mult), r=[("ex", tt), K("sm1")], w=[("ex", tt)])
    P.barrier()
    for tt in range(8):
        b = C.bank()
        P.op("pe", lambda e, b=b, tt=tt: e.matmul(C.ps[b][:, 0:NE], lhsT=us, rhs=mkall[:, tt, :], start=True, stop=(tt == 0)), r=["us", ("mk", tt)], w=[("ps", b)])
        for t2 in range(tt):
            P.op("pe", lambda e, b=b, t2=t2, tt=tt: e.matmul(C.ps[b][:, 0:NE], lhsT=ones, rhs=mkall[:, t2, :], start=False, stop=(t2 == tt - 1)),
                 r=["c_ones_f", ("mk", t2)], w=[("ps", b)])
        P.op("dve", lambda e, b=b, tt=tt: e.scalar_tensor_tensor(out=posm[:, tt, :], in0=C.ps[b][:, 0:NE], scalar=1.0, in1=mkall[:, tt, :], op0=ALU.add, op1=ALU.mult),
             r=[("ps", b), ("mk", tt)], w=[("posm", tt)])
        P.op("dve", lambda e, tt=tt: e.tensor_scalar(out=posm[:, tt, :], in0=posm[:, tt, :], scalar1=-1.0, scalar2=None, op0=ALU.add), r=[("posm", tt)], w=[("posm", tt)])
    P.barrier()
    for src_, dst_, nm in ((exall, PTk, "PTk"), (posm, POSMT, "POSMT")):
        for pb in range(2):
            b = C.bank()
            for j in range(4):
                tt = pb * 4 + j
                P.op("pe", lambda e, b=b, j=j, tt=tt, src_=src_: e.transpose(C.ps[b][0:NE, j * 128:(j + 1) * 128], src_[:, tt, :], ident), r=["c_ident"], w=[("ps", b)])
            P.op("act", lambda e, b=b, pb=pb, dst_=dst_: e.activation(out=dst_[0:NE, pb * 512:(pb + 1) * 512], in_=C.ps[b][0:NE, :], func=AF.Copy), r=[("ps", b)], w=[nm])
    P.barrier()
    for kc in range(KC):
        s = kc % 2
        P.dma("sp", xb[s], x1T[kc * 128:(kc + 1) * 128, :], r=["dram_x1"], w=[("xb", s)])
        P.op("dve", lambda e, s=s, kc=kc: e.tensor_scalar(out=xb[s], in0=xb[s], scalar1=modq[:, sc_c + kc:sc_c + kc + 1],
                                                          scalar2=modq[:, sh_c + kc:sh_c + kc + 1], op0=ALU.mult, op1=ALU.add),
             r=[("xb", s), "c_modq"], w=[("xb", s)])
        for q in range(2):
            b = C.bank()
            for j in range(4):
                tt = q * 4 + j
                P.op("pe", lambda e, b=b, j=j, tt=tt, s=s: e.transpose(C.ps[b][:, j * 128:(j + 1) * 128], xb[s][:, tt * 128:(tt + 1) * 128], ident),
                     r=[("xb", s), "c_ident"], w=[("ps", b)])
            P.op("act", lambda e, b=b, q=q, kc=kc: e.activation(out=hfft[:, q * 4:(q + 1) * 4, kc * 128:(kc + 1) * 128],
                                                               in_=C.ps[b][:, :].rearrange("p (a c) -> p a c", a=4), func=AF.Copy),
                 r=[("ps", b)], w=["hfft"])
    P.barrier()
    P.release()
    sel = P.alloc([NE, NE * 128], F32)
    P.dma("sp", sel, C.dram("c_sel", [NE, NE * 128]), r=["dram_sel"], w=["c_sel"])
    bgu = P.alloc([128, NE * 8], F32)
    P.dma("sp", bgu, W["bgu_p"], r=["dram_bgu"], w=["bgu"])
    iota = P.alloc([128, CAP], F32)
    P.dma("sp", iota, C.dram("c_iota", [128, CAP]), r=["dram_iota"], w=["iota"])
    slotid = P.alloc([128, 2], F32)
    P.dma("sp", slotid, C.dram("c_slotid", [128, 2]), r=["dram_slotid"], w=["slotid"])
    XeT = P.alloc([128, KC, CAP], BF16)
    wb = [P.alloc([128, KC, 256], BF16) for _ in range(2)]
    wdq = [P.alloc([128, 4, 1024], BF16) for _ in range(2)]
    Se = P.alloc([128, 8, CAP], BF16)
    pbc = P.alloc([128, NT], F32)
    stp = [P.alloc([128, NT], BF16) for _ in range(2)]
    tg = [P.alloc([128, CAP], F32) for _ in range(2)]
    tsg = [P.alloc([128, CAP], F32) for _ in range(2)]
    tu = [P.alloc([128, CAP], F32) for _ in range(2)]
    hTe = [P.alloc([128, 4, CAP], BF16) for _ in range(2)]
    yeo = [P.alloc([128, 512], BF16) for _ in range(4)]
    wi = 0
    wqi = 0
    ui = 0
    yi = 0
    si = 0
    for ex_i in range(NE):
        v = ex_i % 2
        for hb in range(2):
            b = C.bank()
            P.op("pe", lambda e, b=b, ex_i=ex_i, hb=hb: e.matmul(C.ps[b][:, :], lhsT=sel[0:NE, ex_i * 128:(ex_i + 1) * 128], rhs=PTk[0:NE, hb * 512:(hb + 1) * 512],
                                                                 start=True, stop=True), r=["PTk", "c_sel"], w=[("ps", b)])
            P.op("act", lambda e, b=b, hb=hb: e.activation(out=pbc[:, hb * 512:(hb + 1) * 512], in_=C.ps[b][:, :], func=AF.Copy), r=[("ps", b)], w=[("pbc", hb)])
        pbk = []
        for hb in range(2):
            b = C.bank()
            pbk.append(b)
            P.op("pe", lambda e, b=b, ex_i=ex_i, hb=hb: e.matmul(C.ps[b][:, :], lhsT=sel[0:NE, ex_i * 128:(ex_i + 1) * 128], rhs=POSMT[0:NE, hb * 512:(hb + 1) * 512],
                                                                 start=True, stop=True), r=["POSMT", "c_sel"], w=[("ps", b)])
        for st in range(2):
            u_ = si % 2
            si += 1
            for hb in range(2):
                P.op("dve", lambda e, b=pbk[hb], st=st, hb=hb, u_=u_: e.scalar_tensor_tensor(out=stp[u_][:, hb * 512:(hb + 1) * 512], in0=C.ps[b][:, :], scalar=slotid[:, st:st + 1],
                                                                                          in1=pbc[:, hb * 512:(hb + 1) * 512], op0=ALU.is_equal, op1=ALU.mult),
                     r=[("ps", pbk[hb]), ("pbc", hb), "slotid"], w=[("stp", u_)])
            row = (ex_i * 2 + st) * 128
            P.dma("sp", STP[row:row + 128, :], stp[u_], r=[("stp", u_)], w=["dram_STP"], semkey=("stpst", u_))
        for tt in range(8):
            P.op("dve", lambda e, tt=tt, ex_i=ex_i: e.tensor_scalar(out=Se[:, tt, :], in0=iota, scalar1=posm[:, tt, ex_i:ex_i + 1], scalar2=None, op0=ALU.is_equal),
                 r=["iota", ("posm", tt)], w=[("Se", tt)])
        sek = [("Se", tt) for tt in range(8)]
        for fcx in range(KC):
            b = C.bank()
            for tt in range(8):
                P.op("pe", lambda e, b=b, tt=tt, fcx=fcx: e.matmul(C.ps[b][:, 0:CAP], lhsT=hfft[:, tt, fcx * 128:(fcx + 1) * 128], rhs=Se[:, tt, :], start=(tt == 0), stop=(tt == 7)),
                     r=sek + ["hfft"], w=[("ps", b)])
            eng = "act" if fcx % 2 == 0 else "dve"
            if eng == "act":
                P.op("act", lambda e, b=b, fcx=fcx: e.activation(out=XeT[:, fcx, :], in_=C.ps[b][:, 0:CAP], func=AF.Copy), r=[("ps", b)], w=[("XeT", fcx)])
            else:
                P.op("dve", lambda e, b=b, fcx=fcx: e.tensor_copy(out=XeT[:, fcx, :], in_=C.ps[b][:, 0:CAP]), r=[("ps", b)], w=[("XeT", fcx)])
        for fc in range(4):
            s = wi % 2
            wi += 1
            P.dma("pool", wb[s], W["wgu_h"][ex_i].rearrange("(kc p) n -> p kc n", p=128)[:, :, fc * 256:(fc + 1) * 256], r=["dram_wgu"], w=[("wb", s)])
            bg_ = C.bank()
            bu_ = C.bank()
            for kc in range(KC):
                P.op("pe", lambda e, b=bg_, s=s, kc=kc: e.matmul(C.ps[b][:, 0:CAP], lhsT=wb[s][:, kc, 0:128], rhs=XeT[:, kc, :], start=(kc == 0), stop=(kc == KC - 1)),
                     r=[("wb", s), ("XeT", kc)], w=[("ps", bg_)])
            for kc in range(KC):
                P.op("pe", lambda e, b=bu_, s=s, kc=kc: e.matmul(C.ps[b][:, 0:CAP], lhsT=wb[s][:, kc, 128:256], rhs=XeT[:, kc, :], start=(kc == 0), stop=(kc == KC - 1)),
                     r=[("wb", s), ("XeT", kc)], w=[("ps", bu_)])
            u = ui % 2
            ui += 1
            cg = (ex_i * 4 + fc) * 2
            P.op("dve", lambda e, b=bg_, u=u, cg=cg: e.tensor_scalar(out=tg[u], in0=C.ps[b][:, 0:CAP], scalar1=bgu[:, cg:cg + 1], scalar2=7.0, op0=ALU.add, op1=ALU.min),
                 r=[("ps", bg_), "bgu"], w=[("tg", u)])
            P.op("act", lambda e, u=u: e.activation(out=tsg[u], in_=tg[u], func=AF.Sigmoid, scale=1.702), r=[("tg", u)], w=[("tsg", u)])
            P.op("dve", lambda e, b=bu_, u=u, cg=cg: e.tensor_scalar(out=tu[u], in0=C.ps[b][:, 0:CAP], scalar1=bgu[:, cg + 1:cg + 2], scalar2=7.0, op0=ALU.add, op1=ALU.min),
                 r=[("ps", bu_), "bgu"], w=[("tu", u)])
            P.op("pool", lambda e, u=u: e.tensor_scalar(out=tu[u], in0=tu[u], scalar1=-7.0, scalar2=1.0, op0=ALU.max, op1=ALU.add), r=[("tu", u)], w=[("tu", u)])
            P.op("pool", lambda e, u=u: e.tensor_tensor(out=tg[u], in0=tg[u], in1=tsg[u], op=ALU.mult), r=[("tg", u), ("tsg", u)], w=[("tg", u)])
            P.op("pool", lambda e, u=u, v=v, fc=fc: e.tensor_tensor(out=hTe[v][:, fc, :], in0=tg[u], in1=tu[u], op=ALU.mult), r=[("tg", u), ("tu", u)], w=[("hTe", v)])
        for qd in range(4):
            sq_ = wqi % 2
            wqi += 1
            P.dma("pool", wdq[sq_], W["w_down"][ex_i].rearrange("(fc p) n -> p fc n", p=128)[:, :, qd * 1024:(qd + 1) * 1024], r=["dram_wd"], w=[("wdq", sq_)])
            for st in range(2):
                for dgl in range(2):
                    b = C.bank()
                    for fc in range(4):
                        P.op("pe", lambda e, b=b, v=v, fc=fc, st=st, sq_=sq_, dgl=dgl: e.matmul(C.ps[b][:, :], lhsT=hTe[v][:, fc, st * 128:(st + 1) * 128],
                                                                                             rhs=wdq[sq_][:, fc, dgl * 512:(dgl + 1) * 512], start=(fc == 0), stop=(fc == 3)),
                             r=[("hTe", v), ("wdq", sq_)], w=[("ps", b)])
                    o = yi % 4
                    yi += 1
                    P.op("act", lambda e, b=b, o=o: e.activation(out=yeo[o], in_=C.ps[b][:, :], func=AF.Copy), r=[("ps", b)], w=[("yeo", o)])
                    row = (ex_i * 2 + st) * 128
                    col = qd * 1024 + dgl * 512
                    P.dma("sp", YE[row:row + 128, col:col + 512], yeo[o], r=[("yeo", o)], w=["dram_YE"], semkey=("yeost", o))
    P.barrier()
    P.release()
    P.mark()
    PT2 = P.alloc([128, NT], F32)
    P.op("dve", lambda e: e.tensor_copy(out=PT2[0:NE, :], in_=PTk[0:NE, :]), r=[], w=["PT2"])
    P.barrier()
    bd = P.alloc([128, D], F32)
    P.dma("sp", bd[0:NE, :], W["b_down"], r=["dram_bd"], w=["bd"])
    ye = [P.alloc([128, 512], BF16) for _ in range(4)]
    st_ = [P.alloc([128, NT], BF16) for _ in range(4)]
    xr = [P.alloc([128, 512], F32) for _ in range(4)]
    gq0 = base + 5 * 32
    NQ = NE * 2
    hi = 0
    xi = 0
    for dg in range(8):
        for q in range(NQ):
            s = hi % 4
            hi += 1
            P.dma("sp", ye[s], YE[q * 128:(q + 1) * 128, dg * 512:(dg + 1) * 512], r=["dram_YE"], w=[("ye", s)])
            P.dma("sp", st_[s], STP[q * 128:(q + 1) * 128, :], r=["dram_STP"], w=[("st", s)])
            for dc in range(4):
                for hb in range(2):
                    b = dc * 2 + hb
                    P.op("pe", lambda e, b=b, s=s, dc=dc, hb=hb, q=q: e.matmul(C.ps[b][:, :], lhsT=ye[s][:, dc * 128:(dc + 1) * 128], rhs=st_[s][:, hb * 512:(hb + 1) * 512],
                                                                              start=(q == 0), stop=False),
                         r=[("ye", s), ("st", s)], w=[("ps", b)])
        for dc in range(4):
            for hb in range(2):
                b = dc * 2 + hb
                col = dg * 512 + dc * 128
                P.op("pe", lambda e, b=b, col=col, hb=hb: e.matmul(C.ps[b][:, :], lhsT=bd[0:NE, col:col + 128], rhs=PT2[0:NE, hb * 512:(hb + 1) * 512], start=False, stop=True),
                     r=["bd", "PT2"], w=[("ps", b)])
                x = xi % 4
                xi += 1
                kcx = dg * 4 + dc
                P.dma("sp", xr[x], x1T[col:col + 128, hb * 512:(hb + 1) * 512], r=["dram_x1"], w=[("xr", x)])
                P.op("dve", lambda e, b=b, x=x, kcx=kcx: e.scalar_tensor_tensor(out=xr[x], in0=C.ps[b][:, :], scalar=modq[:, gq0 + kcx:gq0 + kcx + 1], in1=xr[x],
                                                                                op0=ALU.mult, op1=ALU.add),
                     r=[("ps", b), ("xr", x), "c_modq"], w=[("xr", x)])
                P.dma("sp", vT[col:col + 128, hb * 512:(hb + 1) * 512], xr[x], r=[("xr", x)], w=["dram_v"], semkey=("xrst", x))
    stage_end(C)
```

```python
import numpy as np, contextlib, time
import concourse.bass as bass
import concourse.mybir as mybir
from concourse.bass_utils import run_bass_kernel_spmd

F32 = mybir.dt.float32
BF16 = mybir.dt.bfloat16
I32 = mybir.dt.int32
AF = mybir.ActivationFunctionType
ALU = mybir.AluOpType
AX = mybir.AxisListType


class Tok:
    __slots__ = ("sem", "val", "eng")

    def __init__(self, sem, val, eng):
        self.sem = sem
        self.val = val
        self.eng = eng


class Prog:
    ENGS = ("pe", "act", "dve", "pool", "sp")
    LIMIT = 30000

    def __init__(self, nc, stack):
        self.nc = nc
        self.stack = stack
        self.q = {e: [] for e in self.ENGS}
        self.cur = {}
        self.cnt = {}
        self.nsem = 0
        for e in self.ENGS:
            self._new_eng_sem(e)
        self.lastw = {}
        self.readers = {}
        self.seen = {e: {} for e in self.ENGS}
        self.dsem = {}
        self.dcnt = {}
        self.all_tokens = {}
        self.arena = None
        self.arena_off = 0
        self.arena_marks = []

    def _sem(self, name):
        self.nsem += 1
        return self.stack.enter_context(self.nc.semaphore(name))

    def _new_eng_sem(self, e):
        self.cur[e] = self._sem("s_%s_%d" % (e, self.nsem))
        self.cnt[e] = 0

    def _deps(self, eng, r, w, is_dma):
        deps = []
        for k in r:
            t = self.lastw.get(k)
            if t is not None:
                deps.append(t)
        for k in w:
            t = self.lastw.get(k)
            if t is not None and (t.eng != eng or is_dma or t.eng is None):
                deps.append(t)
            for t in self.readers.get(k, ()):
                if t.eng != eng or is_dma or t.eng is None:
                    deps.append(t)
        out = []
        seen = self.seen[eng]
        for t in deps:
            sid = id(t.sem)
            if seen.get(sid, 0) >= t.val:
                continue
            seen[sid] = t.val
            out.append((t.sem, t.val))
        best = {}
        for s, v in out:
            if id(s) not in best or best[id(s)][1] < v:
                best[id(s)] = (s, v)
        return list(best.values())

    def _commit(self, tok, r, w):
        for k in w:
            self.lastw[k] = tok
            self.readers[k] = []
        for k in r:
            self.readers.setdefault(k, []).append(tok)
        self.all_tokens[id(tok.sem)] = tok

    def op(self, eng, fn, r=(), w=()):
        waits = self._deps(eng, r, w, False)
        if self.cnt[eng] >= self.LIMIT:
            self._new_eng_sem(eng)
        self.cnt[eng] += 1
        tok = Tok(self.cur[eng], self.cnt[eng], eng)
        self.q[eng].append((waits, fn, tok.sem, 1))
        self._commit(tok, r, w)
        return tok

    def _dma_sem(self, semkey, queue):
        if not hasattr(self, "dpool"):
            self.dpool = {}
            self.dfree = {}
        semkey = (queue, semkey)
        pool = self.dpool.setdefault(queue, [])
        free = self.dfree.setdefault(queue, [])
        ent = self.dsem.get(semkey)
        if ent is None or ent[1] >= self.LIMIT:
            ent = None
            while free:
                cand = free.pop()
                if cand[1] < self.LIMIT:
                    ent = cand
                    break
            if ent is None:
                ent = [self._sem("d%d" % self.nsem), 0]
                pool.append(ent)
            self.dsem[semkey] = ent
        ent[1] += 16
        return ent[0], ent[1]

    def dma(self, queue, out, in_, r=(), w=(), semkey=None, **kw):
        waits = self._deps(queue, r, w, True)
        if semkey is None:
            semkey = w[0] if (w and not str(w[0]).startswith("dram")) else r[0]
        sem, val = self._dma_sem(semkey, queue)
        tok = Tok(sem, val, None)
        self.q[queue].append((waits, lambda e: e.dma_start(out=out, in_=in_, **kw), tok.sem, 16))
        self._commit(tok, r, w)
        return tok

    def coll(self, fn, r=(), w=(), semkey=None):
        waits = self._deps("pool", r, w, True)
        sem, val = self._dma_sem(semkey, "pool")
        tok = Tok(sem, val, None)
        self.q["pool"].append((waits, fn, tok.sem, 16))
        self._commit(tok, r, w)
        return tok

    def barrier(self):
        toks = list(self.all_tokens.values())
        for e in self.ENGS:
            seen = self.seen[e]
            waits = []
            for t in toks:
                if t.eng == e:
                    continue
                if seen.get(id(t.sem), 0) >= t.val:
                    continue
                seen[id(t.sem)] = t.val
                waits.append((t.sem, t.val))
            if waits:
                self.q[e].append((waits, None, None, 0))
        self.lastw.clear()
        self.readers.clear()
        if hasattr(self, "dpool"):
            self.dsem.clear()
            self.dfree = {q: [e for e in lst if e[1] < self.LIMIT] for q, lst in self.dpool.items()}

    def final_wait(self, eng="sp"):
        toks = list(self.all_tokens.values())
        waits = [(t.sem, t.val) for t in toks]
        self.q[eng].append((waits, None, None, 0))

    def emit(self):
        nc = self.nc
        q = self.q

        def run(e, ename):
            for waits, fn, sem, inc in q[ename]:
                for s, v in waits:
                    e.wait_ge(s, v)
                if fn is not None:
                    ins = fn(e)
                    ins.then_inc(sem, inc)

        with nc.Block() as block:
            @block.tensor
            def _(e):
                run(e, "pe")

            @block.scalar
            def _(e):
                run(e, "act")

            @block.vector
            def _(e):
                run(e, "dve")

            @block.gpsimd
            def _(e):
                run(e, "pool")

            @block.sync
            def _(e):
                run(e, "sp")

    def init_arena(self, nbytes):
        self.arena = self.stack.enter_context(self.nc.sbuf_tensor("arena", [128, nbytes // 4], F32))
        self.arena_bytes = nbytes
        self.arena_off = 0

    def mark(self):
        self.arena_marks.append(self.arena_off)

    def release(self):
        self.arena_off = self.arena_marks.pop()

    def alloc(self, shape, dt):
        n = 1
        for s in shape[1:]:
            n *= s
        esz = 2 if dt == BF16 else 4
        nb = (n * esz + 31) // 32 * 32
        assert self.arena_off + nb <= self.arena_bytes, ("SBUF arena overflow", self.arena_off, nb)
        a = self.arena[:, self.arena_off // 4:(self.arena_off + nb) // 4]
        self.arena_off += nb
        if dt != F32:
            a = a.bitcast(dt)
        a = a[:, 0:n]
        if len(shape) == 3:
            a = a.rearrange("p (a b) -> p a b", a=shape[1])
        elif len(shape) == 4:
            a = a.rearrange("p (a b c) -> p a b c", a=shape[1], b=shape[2])
        if shape[0] != 128:
            a = a[0:shape[0]]
        return a


D = 4096
KC = 32
NT = 1024
NE = 32
DEPTH = 2
DN_ALPHA = (2 * DEPTH) ** 0.25
LN_EPS = 1e-5
RMS_EPS = 1e-6


class Ctx:
    def __init__(self, nc, stack, ext):
        self.nc = nc
        self.stack = stack
        self.ext = ext
        self.P = Prog(nc, stack)
        self.P.init_arena(190 * 1024)
        self.ps = [stack.enter_context(nc.psum_tensor("ps%d" % i, [128, 512], F32)) for i in range(8)]
        self.pi = 0
        self.consts = {}

    def dram(self, name, shape, dt=F32):
        if not hasattr(self, "_drams"):
            self._drams = {}
        if name in self._drams:
            return self._drams[name]
        k = self.ext.get(name)
        kind = {"in": "ExternalInput", "out": "ExternalOutput", None: "Internal"}[k]
        ap = self.nc.dram_tensor(name, list(shape), dt, kind=kind).ap()
        self._drams[name] = ap
        return ap

    def dram_once(self, name, shape, dt=F32):
        return self.dram(name, shape, dt)

    def bank(self):
        b = self.pi % 8
        self.pi += 1
        return b

    def const(self, name, dram_ap, shape, dt=F32, queue="sp"):
        t = self.P.alloc(shape, dt)
        self.P.dma(queue, t, dram_ap, r=["dram_c_" + name], w=["c_" + name])
        self.consts[name] = t
        return t


def stage_begin(C):
    C.P.barrier()
    C.P.mark()


def stage_end(C):
    C.P.barrier()
    C.P.release()


def stage_ln(C, vT, outT, w_p, b_p, eps, tag):
    P = C.P
    stage_begin(C)
    ones = C.consts["ones_f"]
    zb = [P.alloc([128, NT], F32) for _ in range(3)]
    sq = [P.alloc([128, NT], F32) for _ in range(2)]
    for kc in range(KC):
        s = kc % 3
        q = kc % 2
        P.dma("sp", zb[s], vT[kc * 128:(kc + 1) * 128, :], r=["dram_" + tag + "v"], w=[("zb", s)])
        P.op("act", lambda e, s=s, q=q: e.activation(out=sq[q], in_=zb[s], func=AF.Square), r=[("zb", s)], w=[("sq", q)])
        for hb in range(2):
            P.op("pe", lambda e, s=s, hb=hb, kc=kc: e.matmul(C.ps[hb][:, :], lhsT=ones, rhs=zb[s][:, hb * 512:(hb + 1) * 512],
                                                             start=(kc == 0), stop=(kc == KC - 1)),
                 r=[("zb", s), "c_ones_f"], w=[("ps", hb)])
            P.op("pe", lambda e, q=q, hb=hb, kc=kc: e.matmul(C.ps[2 + hb][:, :], lhsT=ones, rhs=sq[q][:, hb * 512:(hb + 1) * 512],
                                                             start=(kc == 0), stop=(kc == KC - 1)),
                 r=[("sq", q), "c_ones_f"], w=[("ps", 2 + hb)])
    mean = P.alloc([128, NT], F32)
    rstd = P.alloc([128, NT], F32)
    tmp = P.alloc([128, NT], F32)
    for hb in range(2):
        sl = slice(hb * 512, (hb + 1) * 512)
        P.op("dve", lambda e, hb=hb, sl=sl: e.tensor_scalar(out=mean[:, sl], in0=C.ps[hb][:, :], scalar1=1.0 / D, scalar2=None, op0=ALU.mult),
             r=[("ps", hb)], w=[("mean", hb)])
        P.op("dve", lambda e, sl=sl: e.tensor_tensor(out=tmp[:, sl], in0=mean[:, sl], in1=mean[:, sl], op=ALU.mult),
             r=[("mean", hb)], w=[("tmp", hb)])
        P.op("dve", lambda e, hb=hb, sl=sl: e.scalar_tensor_tensor(out=rstd[:, sl], in0=C.ps[2 + hb][:, :], scalar=1.0 / D, in1=tmp[:, sl],
                                                                   op0=ALU.mult, op1=ALU.subtract),
             r=[("ps", 2 + hb), ("tmp", hb)], w=[("rstd", hb)])
        P.op("dve", lambda e, sl=sl: e.tensor_scalar(out=rstd[:, sl], in0=rstd[:, sl], scalar1=float(eps), scalar2=None, op0=ALU.add),
             r=[("rstd", hb)], w=[("rstd", hb)])
        P.op("act", lambda e, sl=sl: e.activation(out=rstd[:, sl], in_=rstd[:, sl], func=AF.Sqrt), r=[("rstd", hb)], w=[("rstd", hb)])
        P.op("dve", lambda e, sl=sl: e.reciprocal(out=rstd[:, sl], in_=rstd[:, sl]), r=[("rstd", hb)], w=[("rstd", hb)])
    for kc in range(KC):
        s = kc % 3
        q = kc % 2
        P.dma("sp", zb[s], vT[kc * 128:(kc + 1) * 128, :], r=["dram_" + tag + "v"], w=[("zb", s)])
        P.op("dve", lambda e, s=s: e.tensor_tensor(out=zb[s], in0=zb[s], in1=mean, op=ALU.subtract),
             r=[("zb", s), ("mean", 0), ("mean", 1)], w=[("zb", s)])
        P.op("pool", lambda e, s=s: e.tensor_tensor(out=zb[s], in0=zb[s], in1=rstd, op=ALU.mult),
             r=[("zb", s), ("rstd", 0), ("rstd", 1)], w=[("zb", s)])
        P.op("act", lambda e, s=s, q=q, kc=kc: e.activation(out=sq[q], in_=zb[s], func=AF.Identity, bias=b_p[:, kc:kc + 1], scale=w_p[:, kc:kc + 1]),
             r=[("zb", s)], w=[("sq", q)])
        P.dma("sp", outT[kc * 128:(kc + 1) * 128, :], sq[q], r=[("sq", q)], w=["dram_" + tag + "o"], semkey=("lnst", q))
    stage_end(C)


def stage_moe(C, li, x1T, vT, HT, modq, W):
    P = C.P
    ones = C.consts["ones_f"]
    ident = C.consts["ident"]
    base = li * 192
    stage_begin(C)
    hff = P.alloc([128, KC, NT], BF16)
    P.mark()
    rw = P.alloc([128, KC, NE], F32)
    P.dma("sp", rw, W["router_w"].rearrange("(kc p) n -> p kc n", p=128), r=["dram_rw"], w=["rw"])
    rb = P.alloc([128, NE], F32)
    P.dma("sp", rb, W["router_b_rep"], r=["dram_rb"], w=["rb"])
    xb = [P.alloc([128, NT], F32) for _ in range(3)]
    for kc in range(KC):
        s = kc % 3
        P.dma("sp", xb[s], x1T[kc * 128:(kc + 1) * 128, :], r=["dram_x1"], w=[("xb", s)])
        P.op("dve", lambda e, s=s, kc=kc: e.tensor_scalar(out=xb[s], in0=xb[s], scalar1=modq[:, base + 4 * 32 + kc:base + 4 * 32 + kc + 1],
                                                          scalar2=modq[:, base + 3 * 32 + kc:base + 3 * 32 + kc + 1], op0=ALU.mult, op1=ALU.add),
             r=[("xb", s), "modq"], w=[("xb", s)])
        P.op("act", lambda e, s=s, kc=kc: e.activation(out=hff[:, kc, :], in_=xb[s], func=AF.Copy), r=[("xb", s)], w=[("hff", kc)])
        for tt in range(8):
            P.op("pe", lambda e, s=s, kc=kc, tt=tt: e.matmul(C.ps[tt][:, 0:NE], lhsT=xb[s][:, tt * 128:(tt + 1) * 128], rhs=rw[:, kc, :],
                                                             start=(kc == 0), stop=(kc == KC - 1)),
                 r=[("xb", s), "rw"], w=[("ps", tt)])
    PT = P.alloc([128, NT], F32)
    lg = [P.alloc([128, NE], F32) for _ in range(2)]
    t8 = [P.alloc([128, 8], F32) for _ in range(2)]
    mk = [P.alloc([128, NE], F32) for _ in range(2)]
    ex = [P.alloc([128, NE], F32) for _ in range(2)]
    sm = [P.alloc([128, 2], F32) for _ in range(2)]
    for tt in range(8):
        s = tt % 2
        K = lambda n, s=s: (n, s)
        P.op("dve", lambda e, s=s, tt=tt: e.tensor_tensor(out=lg[s], in0=C.ps[tt][:, 0:NE], in1=rb, op=ALU.add),
             r=[("ps", tt), "rb"], w=[K("lg")])
        P.op("dve", lambda e, s=s: e.max(out=t8[s], in_=lg[s]), r=[K("lg")], w=[K("t8")])
        P.op("dve", lambda e, s=s: e.tensor_scalar(out=mk[s], in0=lg[s], scalar1=t8[s][:, 3:4], scalar2=None, op0=ALU.is_ge),
             r=[K("lg"), K("t8")], w=[K("mk")])
        P.op("dve", lambda e, s=s: e.tensor_scalar(out=sm[s][:, 0:1], in0=t8[s][:, 0:1], scalar1=-1.0, scalar2=None, op0=ALU.mult),
             r=[K("t8")], w=[K("sm0")])
        P.op("act", lambda e, s=s: e.activation(out=ex[s], in_=lg[s], func=AF.Exp, bias=sm[s][:, 0:1], scale=1.0),
             r=[K("lg"), K("sm0")], w=[K("ex")])
        P.op("dve", lambda e, s=s: e.tensor_tensor(out=ex[s], in0=ex[s], in1=mk[s], op=ALU.mult), r=[K("ex"), K("mk")], w=[K("ex")])
        P.op("dve", lambda e, s=s: e.reduce_sum(out=sm[s][:, 1:2], in_=ex[s], axis=AX.X), r=[K("ex")], w=[K("sm1")])
        P.op("dve", lambda e, s=s: e.reciprocal(out=sm[s][:, 1:2], in_=sm[s][:, 1:2]), r=[K("sm1")], w=[K("sm1")])
        P.op("dve", lambda e, s=s: e.tensor_scalar(out=ex[s], in0=ex[s], scalar1=sm[s][:, 1:2], scalar2=None, op0=ALU.mult),
             r=[K("ex"), K("sm1")], w=[K("ex")])
        pb = tt // 4
        P.op("pe", lambda e, s=s, tt=tt, pb=pb: e.transpose(C.ps[pb][0:NE, (tt % 4) * 128:(tt % 4 + 1) * 128], ex[s], ident),
             r=[K("ex"), "c_ident"] + ([("ps", pb)] if tt % 4 else []), w=[("ps", pb)])
    for pb in range(2):
        P.op("act", lambda e, pb=pb: e.activation(out=PT[0:NE, pb * 512:(pb + 1) * 512], in_=C.ps[pb][0:NE, :], func=AF.Copy),
             r=[("ps", pb)], w=[("PT", pb)])
    P.barrier()
    P.release()
    PTk = P.alloc([128, NT], F32)
    P.op("dve", lambda e: e.tensor_copy(out=PTk[0:NE, :], in_=PT[0:NE, :]), r=[], w=["PTk"])
    P.barrier()
    bgu = P.alloc([128, NE * 8], F32)
    P.dma("sp", bgu, W["bgu_p"], r=["dram_bgu"], w=["bgu"])
    sel = P.alloc([NE, NE * 128], F32)
    P.dma("sp", sel, C.dram_once("c_sel", [NE, NE * 128]), r=["dram_sel"], w=["c_sel"])
    wb = [P.alloc([128, KC, 256], BF16) for _ in range(3)]
    pbc = [P.alloc([128, 512], F32) for _ in range(4)]
    tg = [P.alloc([128, 512], F32) for _ in range(2)]
    tsg = [P.alloc([128, 512], F32) for _ in range(2)]
    tu = [P.alloc([128, 512], F32) for _ in range(2)]
    ho = [P.alloc([128, 512], BF16) for _ in range(4)]
    wi = 0
    ui = 0
    hoi = 0
    for ex_i in range(NE):
        for hb in range(2):
            b = C.bank()
            s4 = (ex_i * 2 + hb) % 4
            P.op("pe", lambda e, b=b, ex_i=ex_i, hb=hb: e.matmul(C.ps[b][:, :], lhsT=sel[0:NE, ex_i * 128:(ex_i + 1) * 128],
                                                                 rhs=PTk[0:NE, hb * 512:(hb + 1) * 512], start=True, stop=True),
                 r=["PTk", "c_sel"], w=[("ps", b)])
            P.op("act", lambda e, b=b, s4=s4: e.activation(out=pbc[s4], in_=C.ps[b][:, :], func=AF.Copy), r=[("ps", b)], w=[("pbc", s4)])
        for fc in range(4):
            s = wi % 3
            wi += 1
            P.dma("pool", wb[s], W["wgu_h"][ex_i].rearrange("(kc p) n -> p kc n", p=128)[:, :, fc * 256:(fc + 1) * 256],
                  r=["dram_wgu"], w=[("wb", s)])
            for hb in range(2):
                bg_ = C.bank()
                bu_ = C.bank()
                for kc in range(KC):
                    P.op("pe", lambda e, b=bg_, s=s, kc=kc, hb=hb: e.matmul(C.ps[b][:, :], lhsT=wb[s][:, kc, 0:128], rhs=hff[:, kc, hb * 512:(hb + 1) * 512],
                                                                            start=(kc == 0), stop=(kc == KC - 1)),
                         r=[("wb", s), ("hff", kc)], w=[("ps", bg_)])
                for kc in range(KC):
                    P.op("pe", lambda e, b=bu_, s=s, kc=kc, hb=hb: e.matmul(C.ps[b][:, :], lhsT=wb[s][:, kc, 128:256], rhs=hff[:, kc, hb * 512:(hb + 1) * 512],
                                                                            start=(kc == 0), stop=(kc == KC - 1)),
                         r=[("wb", s), ("hff", kc)], w=[("ps", bu_)])
                u = ui % 2
                ui += 1
                o = hoi % 4
                hoi += 1
                s4 = (ex_i * 2 + hb) % 4
                cg = (ex_i * 4 + fc) * 2
                P.op("dve", lambda e, b=bg_, u=u, cg=cg: e.tensor_scalar(out=tg[u], in0=C.ps[b][:, :], scalar1=bgu[:, cg:cg + 1], scalar2=7.0, op0=ALU.add, op1=ALU.min),
                     r=[("ps", bg_), "bgu"], w=[("tg", u)])
                P.op("act", lambda e, u=u: e.activation(out=tsg[u], in_=tg[u], func=AF.Sigmoid, scale=1.702), r=[("tg", u)], w=[("tsg", u)])
                P.op("dve", lambda e, b=bu_, u=u, cg=cg: e.tensor_scalar(out=tu[u], in0=C.ps[b][:, :], scalar1=bgu[:, cg + 1:cg + 2], scalar2=7.0, op0=ALU.add, op1=ALU.min),
                     r=[("ps", bu_), "bgu"], w=[("tu", u)])
                P.op("pool", lambda e, u=u: e.tensor_scalar(out=tu[u], in0=tu[u], scalar1=-7.0, scalar2=1.0, op0=ALU.max, op1=ALU.add),
                     r=[("tu", u)], w=[("tu", u)])
                P.op("pool", lambda e, u=u: e.tensor_tensor(out=tg[u], in0=tg[u], in1=tsg[u], op=ALU.mult), r=[("tg", u), ("tsg", u)], w=[("tg", u)])
                P.op("pool", lambda e, u=u: e.tensor_tensor(out=tg[u], in0=tg[u], in1=tu[u], op=ALU.mult), r=[("tg", u), ("tu", u)], w=[("tg", u)])
                P.op("dve", lambda e, u=u, o=o, s4=s4: e.tensor_tensor(out=ho[o], in0=tg[u], in1=pbc[s4], op=ALU.mult),
                     r=[("tg", u), ("pbc", s4)], w=[("ho", o)])
                row = (ex_i * 4 + fc) * 128
                P.dma("sp", HT[row:row + 128, hb * 512:(hb + 1) * 512], ho[o], r=[("ho", o)], w=["dram_HT"], semkey=("host", o))
    P.barrier()
    P.release()
    P.mark()
    PT2 = P.alloc([128, NT], F32)
    P.op("dve", lambda e: e.tensor_copy(out=PT2[0:NE, :], in_=PTk[0:NE, :]), r=[], w=["PT2"])
    P.barrier()
    bd = P.alloc([128, D], F32)
    P.dma("sp", bd[0:NE, :], W["b_down"], r=["dram_bd"], w=["bd"])
    ht = [P.alloc([128, 4, NT], BF16) for _ in range(3)]
    wd = [P.alloc([128, 4, 512], BF16) for _ in range(3)]
    xr = [P.alloc([128, 512], F32) for _ in range(4)]
    gq0 = base + 5 * 32
    hi = 0
    xi = 0
    for dg in range(8):
        for ex_i in range(NE):
            s = hi % 3
            hi += 1
            P.dma("sp", ht[s], HT[ex_i * 512:(ex_i + 1) * 512, :].rearrange("(fc p) t -> p fc t", p=128), r=["dram_HT"], w=[("ht", s)])
            P.dma("pool", wd[s], W["w_down"][ex_i].rearrange("(fc p) n -> p fc n", p=128)[:, :, dg * 512:(dg + 1) * 512],
                  r=["dram_wd"], w=[("wd", s)])
            for dc in range(4):
                for hb in range(2):
                    b = dc * 2 + hb
                    for fc in range(4):
                        P.op("pe", lambda e, b=b, s=s, fc=fc, dc=dc, hb=hb, ex_i=ex_i: e.matmul(
                            C.ps[b][:, :], lhsT=wd[s][:, fc, dc * 128:(dc + 1) * 128], rhs=ht[s][:, fc, hb * 512:(hb + 1) * 512],
                            start=(ex_i == 0 and fc == 0), stop=False),
                            r=[("wd", s), ("ht", s)], w=[("ps", b)])
        for dc in range(4):
            for hb in range(2):
                b = dc * 2 + hb
                col = dg * 512 + dc * 128
                P.op("pe", lambda e, b=b, col=col, hb=hb: e.matmul(C.ps[b][:, :], lhsT=bd[0:NE, col:col + 128], rhs=PT2[0:NE, hb * 512:(hb + 1) * 512],
                                                                   start=False, stop=True),
                     r=["bd", "PT2"], w=[("ps", b)])
                x = xi % 4
                xi += 1
                kcx = dg * 4 + dc
                P.dma("sp", xr[x], x1T[col:col + 128, hb * 512:(hb + 1) * 512], r=["dram_x1"], w=[("xr", x)])
                P.op("dve", lambda e, b=b, x=x, kcx=kcx: e.scalar_tensor_tensor(out=xr[x], in0=C.ps[b][:, :], scalar=modq[:, gq0 + kcx:gq0 + kcx + 1], in1=xr[x],
                                                                                op0=ALU.mult, op1=ALU.add),
                     r=[("ps", b), ("xr", x), "modq"], w=[("xr", x)])
                P.dma("sp", vT[col:col + 128, hb * 512:(hb + 1) * 512], xr[x], r=[("xr", x)], w=["dram_v"], semkey=("xrst", x))
    stage_end(C)


def load_consts(C):
    P = C.P
    C.const("ones_f", C.dram("c_ones", [128, 128]), [128, 128])
    C.const("ident", C.dram("c_ident", [128, 128]), [128, 128])
    C.const("lnw", C.dram("lnw_p", [128, DEPTH * 2 * KC]), [128, DEPTH * 2 * KC])
    C.const("lnb", C.dram("lnb_p", [128, DEPTH * 2 * KC]), [128, DEPTH * 2 * KC])
    modq = C.const("modq", C.dram("modp", [128, DEPTH * 6 * KC]), [128, DEPTH * 6 * KC])
    for i in range(DEPTH):
        for s_ in (1, 4):
            sl = slice(i * 192 + s_ * 32, i * 192 + s_ * 32 + 32)
            P.op("dve", lambda e, sl=sl: e.tensor_scalar(out=modq[:, sl], in0=modq[:, sl], scalar1=1.0, scalar2=None, op0=ALU.add),
                 r=["c_modq"], w=["c_modq"])
        for s_ in (2, 5):
            sl = slice(i * 192 + s_ * 32, i * 192 + s_ * 32 + 32)
            P.op("dve", lambda e, sl=sl: e.tensor_scalar(out=modq[:, sl], in0=modq[:, sl], scalar1=1.0, scalar2=1.0 / DN_ALPHA, op0=ALU.add, op1=ALU.mult),
                 r=["c_modq"], w=["c_modq"])
    P.barrier()
    return modq


def host_consts():
    c = {}
    c["c_ones"] = np.ones((128, 128), np.float32)
    c["c_ident"] = np.eye(128, dtype=np.float32)
    sel = np.zeros((NE, NE, 128), np.float32)
    for e in range(NE):
        sel[e, e, :] = 1.0
    c["c_sel"] = sel.reshape(NE, NE * 128)
    c["c_Us"] = (np.arange(128)[:, None] < np.arange(128)[None, :]).astype(np.float32)
    c["c_iota"] = np.ascontiguousarray(np.broadcast_to(np.arange(256, dtype=np.float32)[None, :], (128, 256)))
    c["c_slotid"] = np.stack([np.arange(128), np.arange(128) + 128], axis=1).astype(np.float32)
    return c


def pp(v):
    return np.ascontiguousarray(v.reshape(-1, 128).T)


def host_moe_weights(li, router_w, router_b, w_gu, b_gu, w_down, b_down, sfx):
    m = {}
    m["router_w" + sfx] = np.ascontiguousarray(router_w[li])
    m["router_b_rep" + sfx] = np.ascontiguousarray(np.broadcast_to(router_b[li][None, :], (128, NE)))
    g = w_gu[li].reshape(NE, D, 4, 128, 2).transpose(0, 1, 2, 4, 3).reshape(NE, D, 1024)
    m["wgu_h" + sfx] = np.ascontiguousarray(g)
    bg = b_gu[li].reshape(NE, 4, 128, 2).transpose(2, 0, 1, 3).reshape(128, NE * 8)
    m["bgu_p" + sfx] = np.ascontiguousarray(bg)
    m["w_down" + sfx] = np.ascontiguousarray(w_down[li])
    m["b_down" + sfx] = np.ascontiguousarray(b_down[li])
    return m


def moe_weight_aps(C, sfx):
    return {
        "router_w": C.dram("router_w" + sfx, [D, NE]),
        "router_b_rep": C.dram("router_b_rep" + sfx, [128, NE]),
        "wgu_h": C.dram("wgu_h" + sfx, [NE, D, 1024]),
        "bgu_p": C.dram("bgu_p" + sfx, [128, NE * 8]),
        "w_down": C.dram("w_down" + sfx, [NE, 512, D]),
        "b_down": C.dram("b_down" + sfx, [NE, D]),
    }


def gemm_fm(C, act, KCn, Wv, col0, ncols, toks, epi, tag, wbufs, actkey):
    P = C.P
    for g0 in range(col0, col0 + ncols, 256):
        gw = min(256, col0 + ncols - g0)
        s = C.wi % len(wbufs)
        C.wi += 1
        P.dma("pool", wbufs[s][:, 0:KCn, 0:gw], Wv[:, :, g0:g0 + gw], r=["dram_w" + tag], w=[("wbuf", s)])
        for j0 in range(0, gw, 128):
            cw = min(128, gw - j0)
            for (t0, tn) in toks:
                b = C.bank()
                for kc in range(KCn):
                    P.op("pe", lambda e, b=b, s=s, kc=kc, j0=j0, cw=cw, t0=t0, tn=tn: e.matmul(
                        C.ps[b][0:cw, 0:tn], lhsT=wbufs[s][:, kc, j0:j0 + cw], rhs=act[:, kc, t0:t0 + tn],
                        start=(kc == 0), stop=(kc == KCn - 1)),
                        r=[("wbuf", s), actkey], w=[("ps", b)])
                epi(b, g0 + j0, cw, t0, tn)


def gemm_tm(C, act, KCn, Wv, col0, ncols, tts, epi, tag, wbufs, actkey):
    P = C.P
    for g0 in range(col0, col0 + ncols, 256):
        gw = min(256, col0 + ncols - g0)
        s = C.wi % len(wbufs)
        C.wi += 1
        P.dma("pool", wbufs[s][:, 0:KCn, 0:gw], Wv[:, :, g0:g0 + gw], r=["dram_w" + tag], w=[("wbuf", s)])
        for tt in tts:
            b = C.bank()
            for kc in range(KCn):
                P.op("pe", lambda e, b=b, s=s, kc=kc, gw=gw, tt=tt: e.matmul(
                    C.ps[b][:, 0:gw], lhsT=act[:, kc, tt * 128:(tt + 1) * 128], rhs=wbufs[s][:, kc, 0:gw],
                    start=(kc == 0), stop=(kc == KCn - 1)),
                    r=[("wbuf", s), actkey], w=[("ps", b)])
            epi(b, tt, g0, gw)


def make_hmix(C, srcT, hm, modq, sc_col, sh_col, xb, key):
    P = C.P
    for kc in range(KC):
        s = kc % len(xb)
        P.dma("sp", xb[s], srcT[kc * 128:(kc + 1) * 128, :], r=["dram_src" + str(key)], w=[("xb", s)])
        P.op("dve", lambda e, s=s, kc=kc: e.tensor_scalar(out=hm[:, kc, :], in0=xb[s], scalar1=modq[:, sc_col + kc:sc_col + kc + 1],
                                                          scalar2=modq[:, sh_col + kc:sh_col + kc + 1], op0=ALU.mult, op1=ALU.add),
             r=[("xb", s), "c_modq"], w=[key])


O_XBC = 4096
O_DT = 4096 + 6144
O_U = O_DT + 64


def stage_inproj(C, srcT, modq, Win, xbcT, dt_tok, z_tok, uT, reg):
    P = C.P
    stage_begin(C)
    C.wi = 0
    Wv = Win.rearrange("(kc p) n -> p kc n", p=128)
    hm = P.alloc([128, KC, NT], BF16)
    xb = [P.alloc([128, NT], F32) for _ in range(2)]
    wbufs = [P.alloc([128, KC, 256], BF16) for _ in range(3)]
    ev = [P.alloc([128, 512], F32) for _ in range(4)]
    evi = [0]

    def evac_store(b, rows, cols, dst):
        o = evi[0] % 4
        evi[0] += 1
        P.op("act", lambda e, b=b, o=o: e.activation(out=ev[o][0:rows, 0:cols], in_=C.ps[b][0:rows, 0:cols], func=AF.Copy),
             r=[("ps", b)], w=[("ev", o)])
        P.dma("sp", dst, ev[o][0:rows, 0:cols], r=[("ev", o)], w=["dram_s1"], semkey=("evst", o))

    key = "hm"
    make_hmix(C, srcT, hm, modq, 1 * 32, 0 * 32, xb, key)
    toks = [(0, 512), (512, 512)]
    r0 = reg * NT
    gemm_fm(C, hm, KC, Wv, O_XBC, 6144, toks,
            lambda b, col, cw, t0, tn: evac_store(b, cw, tn, xbcT[col - O_XBC:col - O_XBC + cw, r0 + t0:r0 + t0 + tn]),
            "in", wbufs, key)
    gemm_tm(C, hm, KC, Wv, O_DT, 64, list(range(8)),
            lambda b, tt, col, gw: evac_store(b, 128, gw, dt_tok[r0 + tt * 128:r0 + (tt + 1) * 128, :]),
            "in", wbufs, key)
    gemm_fm(C, hm, KC, Wv, O_U, 4096, toks,
            lambda b, col, cw, t0, tn: evac_store(b, cw, tn, uT[col - O_U:col - O_U + cw, 16 + t0:16 + t0 + tn]),
            "in", wbufs, key)
    gemm_tm(C, hm, KC, Wv, 0, 4096, list(range(8)),
            lambda b, tt, col, gw: evac_store(b, 128, gw, z_tok[tt * 128:(tt + 1) * 128, col:col + gw]),
            "in", wbufs, key)
    stage_end(C)


def stage_conv(C, xbcT, xactT, convw, convb, reg, flag):
    P = C.P
    stage_begin(C)
    NCH = 6144 // 128
    r0 = reg * NT
    ib = [P.alloc([128, 3 + NT], F32) for _ in range(2)]
    ac = [P.alloc([128, NT], F32) for _ in range(2)]
    for c in range(NCH):
        s = c % 2
        if reg == 0:
            P.op("dve", lambda e, s=s: e.memset(ib[s][:, 0:3], 0.0), r=[], w=[("ibh", s)])
        else:
            P.dma("sp", ib[s][:, 0:3], xbcT[c * 128:(c + 1) * 128, NT - 3:NT], r=["dram_xbc"], w=[("ibh", s)])
            P.op("dve", lambda e, s=s: e.tensor_scalar(out=ib[s][:, 0:3], in0=ib[s][:, 0:3], scalar1=flag[:, 0:1], scalar2=None, op0=ALU.mult),
                 r=[("ibh", s), "c_flag"], w=[("ibh", s)])
        P.dma("sp", ib[s][:, 3:3 + NT], xbcT[c * 128:(c + 1) * 128, r0:r0 + NT], r=["dram_xbc"], w=[("ib", s)])
        P.op("dve", lambda e, s=s, c=c: e.tensor_scalar(out=ac[s], in0=ib[s][:, 0:NT], scalar1=convw[:, c * 4:c * 4 + 1], scalar2=None, op0=ALU.mult),
             r=[("ib", s), ("ibh", s), "c_convw"], w=[("ac", s)])
        for k in range(1, 4):
            P.op("dve", lambda e, s=s, c=c, k=k: e.scalar_tensor_tensor(out=ac[s], in0=ib[s][:, k:k + NT], scalar=convw[:, c * 4 + k:c * 4 + k + 1], in1=ac[s],
                                                                        op0=ALU.mult, op1=ALU.add),
                 r=[("ib", s), ("ibh", s), ("ac", s)], w=[("ac", s)])
        P.op("act", lambda e, s=s, c=c: e.activation(out=ac[s], in_=ac[s], func=AF.Silu, bias=convb[:, c:c + 1], scale=1.0),
             r=[("ac", s), "c_convb"], w=[("ac", s)])
        P.dma("sp", xactT[c * 128:(c + 1) * 128, r0:r0 + NT], ac[s], r=[("ac", s)], w=["dram_xact"], semkey=("acst", s))
    stage_end(C)


def stage_ssd(C, xactT, dt_tok, z_tok, catT, K_, reg, hstate):
    P = C.P
    stage_begin(C)
    ones = C.consts["ones_f"]
    ident = C.consts["ident"]
    U = K_["U"]
    maskneg = K_["maskneg"]
    aneg = K_["aneg"]
    dtb = K_["dtb"]
    Drow = P.alloc([128, D], F32)
    P.dma("sp", Drow, K_["Drow_d"], r=["dram_drow"], w=["c_Drow"])
    flag = K_["flag"]
    nw = K_["ssdnw"]
    xv = xactT[0:4096, :].rearrange("(kc p) t -> p kc t", p=128)
    bv = xactT[4096:5120, :].rearrange("(g p) t -> p g t", p=128)
    cv = xactT[5120:6144, :].rearrange("(g p) t -> p g t", p=128)
    hS = P.alloc([128, D], F32)
    hB = P.alloc([128, D], BF16)
    if reg == 0:
        P.op("dve", lambda e: e.memset(hS, 0.0), r=[], w=["hS"])
    else:
        P.dma("sp", hS, hstate, r=["dram_hstate"], w=["hS"])
        P.op("dve", lambda e: e.tensor_scalar(out=hS, in0=hS, scalar1=K_["flag"][:, 0:1], scalar2=None, op0=ALU.mult), r=["hS", "c_flag"], w=["hS"])
    P.op("act", lambda e: e.activation(out=hB, in_=hS, func=AF.Copy), r=["hS"], w=["hB"])
    xTc = [P.alloc([128, KC, 128], F32) for _ in range(1)]
    bTf = P.alloc([128, 8, 128], F32)
    bTb = P.alloc([128, 8, 128], BF16)
    cTb = P.alloc([128, 8, 128], BF16)
    dtr = P.alloc([128, 64], F32)
    sA = P.alloc([128, 64], F32)
    sB = P.alloc([128, 64], F32)
    dts = P.alloc([128, 64], F32)
    la = P.alloc([128, 64], F32)
    cum = P.alloc([128, 64], F32)
    ecum = P.alloc([128, 64], F32)
    toend = P.alloc([128, 64], F32)
    cdec = P.alloc([128, 64], F32)
    xtok = P.alloc([128, D], F32)
    xdt = P.alloc([128, D], BF16)
    xdte = P.alloc([128, D], BF16)
    btok = P.alloc([128, 8, 128], BF16)
    zc = P.alloc([128, D], F32)
    y = P.alloc([128, D], F32)
    cbT = P.alloc([128, 8, 128], F32)
    larep = [P.alloc([128, 4, 128], F32) for _ in range(2)]
    tmp = [P.alloc([128, 4, 128], F32) for _ in range(2)]
    MT = [P.alloc([128, 4, 128], BF16) for _ in range(2)]
    ms = P.alloc([128, 8], F32)
    cat = P.alloc([128, KC, 128], BF16)
    v3 = lambda t: t.rearrange("p (h q) -> p h q", q=64)
    bc3 = lambda t: t[:, 0:64].unsqueeze(2).to_broadcast([128, 64, 64])
    for c in range(reg * 8, reg * 8 + 8):
        own = True
        t0 = c * 128
        tl = t0 - reg * NT
        xs = 0
        P.dma("sp", xTc[xs], xv[:, :, t0:t0 + 128], r=["dram_xact"], w=[("xTc", xs)])
        P.dma("sp", bTf, bv[:, :, t0:t0 + 128], r=["dram_xact"], w=["bTf"])
        P.dma("sp", dtr, dt_tok[t0:t0 + 128, :], r=["dram_dt"], w=["dtr"])
        if own:
            P.dma("pool", bTb, bv[:, :, t0:t0 + 128], r=["dram_xact"], w=["bTb"])
            P.dma("pool", cTb, cv[:, :, t0:t0 + 128], r=["dram_xact"], w=["cTb"])
            P.dma("sp", zc, z_tok[tl:tl + 128, :], r=["dram_z"], w=["zc"])
        P.op("dve", lambda e: e.tensor_tensor(out=dtr, in0=dtr, in1=dtb, op=ALU.add), r=["dtr", "c_dtb"], w=["dtr"])
        P.op("act", lambda e: e.activation(out=sA, in_=dtr, func=AF.Abs), r=["dtr"], w=["sA"])
        P.op("act", lambda e: e.activation(out=sA, in_=sA, func=AF.Exp, scale=-1.0), r=["sA"], w=["sA"])
        P.op("act", lambda e: e.activation(out=sA, in_=sA, func=AF.Ln, bias=1.0, scale=1.0), r=["sA"], w=["sA"])
        P.op("dve", lambda e: e.tensor_scalar(out=sB, in0=dtr, scalar1=0.0, scalar2=None, op0=ALU.max), r=["dtr"], w=["sB"])
        P.op("dve", lambda e: e.tensor_tensor(out=dts, in0=sA, in1=sB, op=ALU.add), r=["sA", "sB"], w=["dts"])
        P.op("dve", lambda e: e.tensor_tensor(out=la, in0=dts, in1=aneg, op=ALU.mult), r=["dts", "c_aneg"], w=["la"])
        b1 = C.bank()
        P.op("pe", lambda e, b=b1: e.matmul(C.ps[b][:, 0:64], lhsT=U, rhs=la, start=True, stop=True), r=["la", "c_U"], w=[("ps", b1)])
        b2 = C.bank()
        P.op("pe", lambda e, b=b2: e.matmul(C.ps[b][:, 0:64], lhsT=ones, rhs=la, start=True, stop=True), r=["la", "c_ones_f"], w=[("ps", b2)])
        P.op("act", lambda e, b=b1: e.activation(out=cum, in_=C.ps[b][:, 0:64], func=AF.Copy), r=[("ps", b1)], w=["cum"])
        P.op("act", lambda e, b=b1: e.activation(out=ecum, in_=C.ps[b][:, 0:64], func=AF.Exp), r=[("ps", b1)], w=["ecum"])
        P.op("dve", lambda e, b=b2: e.tensor_tensor(out=toend, in0=C.ps[b][:, 0:64], in1=cum, op=ALU.subtract), r=[("ps", b2), "cum"], w=["toend"])
        P.op("act", lambda e: e.activation(out=toend, in_=toend, func=AF.Exp), r=["toend"], w=["toend"])
        P.op("act", lambda e, b=b2: e.activation(out=cdec, in_=C.ps[b][:, 0:64], func=AF.Exp), r=[("ps", b2)], w=["cdec"])
        for q in range(8):
            b = C.bank()
            for j in range(4):
                kc = q * 4 + j
                P.op("pe", lambda e, b=b, j=j, kc=kc, xs=xs: e.transpose(C.ps[b][:, j * 128:(j + 1) * 128], xTc[xs][:, kc, :], ident),
                     r=[("xTc", xs), "c_ident"], w=[("ps", b)])
            P.op("act", lambda e, b=b, q=q: e.activation(out=xtok[:, q * 512:(q + 1) * 512], in_=C.ps[b][:, :], func=AF.Copy),
                 r=[("ps", b)], w=[("xtok", q)])
        xk = [("xtok", q) for q in range(8)]
        P.op("dve", lambda e: e.tensor_tensor(out=v3(xdt), in0=v3(xtok), in1=bc3(dts), op=ALU.mult), r=xk + ["dts"], w=["xdt"])
        P.op("pool", lambda e: e.tensor_tensor(out=v3(xdte), in0=v3(xdt), in1=bc3(toend), op=ALU.mult), r=["xdt", "toend"], w=["xdte"])
        for q in range(2):
            b = C.bank()
            for j in range(4):
                g = q * 4 + j
                P.op("pe", lambda e, b=b, j=j, g=g: e.transpose(C.ps[b][:, j * 128:(j + 1) * 128], bTf[:, g, :], ident),
                     r=["bTf", "c_ident"], w=[("ps", b)])
            P.op("act", lambda e, b=b, q=q: e.activation(out=btok[:, q * 4:(q + 1) * 4, :].rearrange("p a b -> p (a b)"), in_=C.ps[b][:, :], func=AF.Copy),
                 r=[("ps", b)], w=[("btok", q)])
        if own:
            for g in range(8):
                b = C.bank()
                P.op("pe", lambda e, b=b, g=g: e.matmul(C.ps[b][:, :], lhsT=cTb[:, g, :], rhs=hB[:, g * 512:(g + 1) * 512], start=True, stop=True),
                     r=["cTb", "hB"], w=[("ps", b)])
                P.op("dve", lambda e, b=b, g=g: e.tensor_tensor(out=y[:, g * 512:(g + 1) * 512].rearrange("p (h q) -> p h q", q=64),
                                                                in0=C.ps[b][:, :].rearrange("p (h q) -> p h q", q=64),
                                                                in1=ecum[:, g * 8:(g + 1) * 8].unsqueeze(2).to_broadcast([128, 8, 64]), op=ALU.mult),
                     r=[("ps", b), "ecum"], w=[("y", g)])
            for q in range(2):
                b = C.bank()
                for j in range(4):
                    g = q * 4 + j
                    P.op("pe", lambda e, b=b, j=j, g=g: e.matmul(C.ps[b][:, j * 128:(j + 1) * 128], lhsT=bTb[:, g, :], rhs=cTb[:, g, :], start=True, stop=True),
                         r=["bTb", "cTb"], w=[("ps", b)])
                P.op("act", lambda e, b=b, q=q: e.activation(out=cbT[:, q * 4:(q + 1) * 4, :].rearrange("p a b -> p (a b)"), in_=C.ps[b][:, :], func=AF.Copy),
                     r=[("ps", b)], w=[("cbT", q)])
            yb = None
            for q in range(16):
                g = q // 2
                u = q % 2
                P.op("dve", lambda e, u=u, q=q: e.tensor_copy(out=larep[u], in_=la[:, q * 4:(q + 1) * 4].unsqueeze(2).to_broadcast([128, 4, 128])),
                     r=["la"], w=[("larep", u)])
                b = C.bank()
                for j in range(4):
                    P.op("pe", lambda e, b=b, j=j, u=u: e.matmul(C.ps[b][:, j * 128:(j + 1) * 128], lhsT=larep[u][:, j, :], rhs=U, start=True, stop=True),
                         r=[("larep", u), "c_U"], w=[("ps", b)])
                for j in range(4):
                    h = q * 4 + j
                    P.op("dve", lambda e, b=b, j=j, u=u, h=h: e.scalar_tensor_tensor(out=tmp[u][:, j, :], in0=C.ps[b][:, j * 128:(j + 1) * 128], scalar=cum[:, h:h + 1],
                                                                                    in1=maskneg, op0=ALU.subtract, op1=ALU.add),
                         r=[("ps", b), "cum", "c_maskneg"], w=[("tmp", u)])
                P.op("act", lambda e, u=u: e.activation(out=tmp[u], in_=tmp[u], func=AF.Exp), r=[("tmp", u)], w=[("tmp", u)])
                P.op("pool", lambda e, u=u, g=g: e.tensor_tensor(out=MT[u], in0=tmp[u], in1=cbT[:, g, :].unsqueeze(1).to_broadcast([128, 4, 128]), op=ALU.mult),
                     r=[("tmp", u), ("cbT", g // 4)], w=[("MT", u)])
                if q % 2 == 0:
                    yb = C.bank()
                for j in range(4):
                    h = q * 4 + j
                    hh = h % 8
                    P.op("pe", lambda e, yb=yb, j=j, u=u, h=h, hh=hh: e.matmul(C.ps[yb][:, hh * 64:(hh + 1) * 64], lhsT=MT[u][:, j, :], rhs=xdt[:, h * 64:(h + 1) * 64],
                                                                              start=True, stop=True),
                         r=[("MT", u), "xdt"], w=[("ps", yb)])
                if q % 2 == 1:
                    P.op("dve", lambda e, yb=yb, g=g: e.tensor_tensor(out=y[:, g * 512:(g + 1) * 512], in0=C.ps[yb][:, :], in1=y[:, g * 512:(g + 1) * 512], op=ALU.add),
                         r=[("ps", yb), ("y", g)], w=[("y", g)])
            yk = [("y", g) for g in range(8)]
            P.op("pool", lambda e: e.tensor_tensor(out=xtok, in0=xtok, in1=Drow, op=ALU.mult), r=xk + ["c_Drow", "xdt"], w=xk)
            P.op("dve", lambda e: e.tensor_tensor(out=y, in0=y, in1=xtok, op=ALU.add), r=yk + xk, w=yk)
            P.op("act", lambda e: e.activation(out=zc, in_=zc, func=AF.Silu), r=["zc"], w=["zc"])
            P.op("dve", lambda e: e.tensor_tensor(out=y, in0=y, in1=zc, op=ALU.mult), r=yk + ["zc"], w=yk)
            P.op("dve", lambda e: e.memset(ms, 0.0), r=[], w=["msr"])
            for g in range(8):
                P.op("act", lambda e, g=g: e.activation(out=zc[:, g * 512:(g + 1) * 512], in_=y[:, g * 512:(g + 1) * 512], func=AF.Square, accum_out=ms[:, g:g + 1]),
                     r=yk + ["zc", "msr"], w=["zc", "msr"])
            P.op("dve", lambda e: e.tensor_scalar(out=ms, in0=ms, scalar1=1.0 / 512, scalar2=LN_EPS, op0=ALU.mult, op1=ALU.add), r=["msr"], w=["msr"])
            P.op("act", lambda e: e.activation(out=ms, in_=ms, func=AF.Sqrt), r=["msr"], w=["msr"])
            P.op("dve", lambda e: e.reciprocal(out=ms, in_=ms), r=["msr"], w=["msr"])
            P.op("dve", lambda e: e.tensor_tensor(out=y.rearrange("p (g q) -> p g q", q=512), in0=y.rearrange("p (g q) -> p g q", q=512),
                                                  in1=ms[:, 0:8].unsqueeze(2).to_broadcast([128, 8, 512]), op=ALU.mult), r=yk + ["msr"], w=yk)
            for q in range(8):
                b = C.bank()
                for j in range(4):
                    kc = q * 4 + j
                    P.op("pe", lambda e, b=b, j=j, kc=kc: e.transpose(C.ps[b][:, j * 128:(j + 1) * 128], y[:, kc * 128:(kc + 1) * 128], ident),
                         r=yk + ["c_ident"], w=[("ps", b)])
                for j in range(4):
                    kc = q * 4 + j
                    P.op("act", lambda e, b=b, j=j, kc=kc: e.activation(out=cat[:, kc, :], in_=C.ps[b][:, j * 128:(j + 1) * 128], func=AF.Copy, scale=nw[:, kc:kc + 1]),
                         r=[("ps", b), "c_ssdnw"], w=["cat"])
            P.dma("sp", catT[0:4096, tl:tl + 128].rearrange("(kc p) t -> p kc t", p=128), cat, r=["cat"], w=["dram_cat"], semkey="catst")
        P.op("dve", lambda e: e.tensor_tensor(out=v3(hS), in0=v3(hS), in1=bc3(cdec), op=ALU.mult), r=["hS", "cdec"], w=["hS"])
        for g in range(8):
            b = C.bank()
            P.op("pe", lambda e, b=b, g=g: e.matmul(C.ps[b][:, :], lhsT=btok[:, g, :], rhs=xdte[:, g * 512:(g + 1) * 512], start=True, stop=True),
                 r=[("btok", g // 4), "xdte"], w=[("ps", b)])
            P.op("dve", lambda e, b=b, g=g: e.tensor_tensor(out=hS[:, g * 512:(g + 1) * 512], in0=hS[:, g * 512:(g + 1) * 512], in1=C.ps[b][:, :], op=ALU.add),
                 r=[("ps", b), "hS"], w=["hS"])
        P.op("act", lambda e: e.activation(out=hB, in_=hS, func=AF.Copy), r=["hS"], w=["hB"])
    if reg == 0:
        P.dma("sp", hstate, hS, r=["hS"], w=["dram_hstate"], semkey="hsst")
    stage_end(C)


def stage_pool(C, uT, catT, poolw, K_, reg, uT_prev):
    P = C.P
    stage_begin(C)
    C.wi = 0
    pscale = K_["pscale"]
    invc = K_["invc"]
    diff = P.alloc([128, KC, NT], BF16)
    ub = [P.alloc([128, 16 + NT], F32) for _ in range(2)]
    pa = P.alloc([128, 16 + NT], F32)
    pb = P.alloc([128, 16 + NT], F32)
    ic = P.alloc([128, NT], F32)
    W_ = 16 + NT
    for kc in range(KC):
        g = kc // 8
        s = kc % 2
        if kc % 8 == 0:
            P.dma("sp", ic, invc[g], r=["dram_invc"], w=["ic"])
        P.dma("sp", ub[s][:, 16:W_], uT[kc * 128:(kc + 1) * 128, 16:W_], r=["dram_u"], w=[("ub", s)])
        if reg == 0:
            P.op("dve", lambda e, s=s: e.memset(ub[s][:, 0:16], 0.0), r=[("ub", s)], w=[("ub", s)])
        else:
            P.dma("sp", ub[s][:, 0:16], uT_prev[kc * 128:(kc + 1) * 128, NT:NT + 16], r=["dram_u"], w=[("ubh", s)])
            P.op("dve", lambda e, s=s: e.tensor_scalar(out=ub[s][:, 0:16], in0=ub[s][:, 0:16], scalar1=K_["flag"][:, 0:1], scalar2=None, op0=ALU.mult),
                 r=[("ubh", s), ("ub", s), "c_flag"], w=[("ub", s)])
        src = ub[s]
        srck = ("ub", s)
        dsts = [(pa, "pa"), (pb, "pb")]
        for st in range(g + 1):
            sh = 1 << st
            dst, dk = dsts[st % 2]
            P.op("dve", lambda e, dst=dst, src=src, sh=sh: e.tensor_tensor(out=dst[:, sh:W_], in0=src[:, sh:W_], in1=src[:, 0:W_ - sh], op=ALU.add),
                 r=[srck], w=[dk])
            src, srck = dst, dk
        P.op("pool", lambda e, src=src: e.tensor_tensor(out=src[:, 16:W_], in0=src[:, 16:W_], in1=ic, op=ALU.mult), r=[srck, "ic"], w=[srck])
        P.op("dve", lambda e, src=src, s=s, kc=kc: e.tensor_tensor(out=diff[:, kc, :], in0=src[:, 16:W_], in1=ub[s][:, 16:W_], op=ALU.subtract),
             r=[srck, ("ub", s)], w=["diff"])
    wbufs = [P.alloc([128, 8, 256], BF16) for _ in range(3)]
    ev = [P.alloc([128, 512], BF16) for _ in range(4)]
    evi = [0]

    def epi(b, col, cw, t0, tn, g):
        o = evi[0] % 4
        evi[0] += 1
        d0 = g * 1024 + col
        kcx = d0 // 128
        P.op("act", lambda e, b=b, o=o: e.activation(out=ev[o][:, 0:tn], in_=C.ps[b][:, 0:tn], func=AF.Copy, scale=pscale[:, kcx:kcx + 1]),
             r=[("ps", b), "c_pscale"], w=[("ev", o)])
        P.dma("sp", catT[4096 + d0:4096 + d0 + 128, t0:t0 + tn], ev[o][:, 0:tn], r=[("ev", o)], w=["dram_cat"], semkey=("pevst", o))

    for g in range(4):
        Wv = poolw[g].rearrange("(kc p) n -> p kc n", p=128)
        gemm_fm(C, diff[:, g * 8:(g + 1) * 8, :], 8, Wv, 0, 1024, [(0, 512), (512, 512)],
                lambda b, col, cw, t0, tn, g=g: epi(b, col, cw, t0, tn, g), "pool", wbufs, "diff")
    stage_end(C)


def stage_proj_res(C, actT, KCn, Wd, resT, vT, modq, gcol, tag):
    P = C.P
    stage_begin(C)
    C.wi = 0
    Wv = Wd.rearrange("(kc p) n -> p kc n", p=128)
    act = P.alloc([128, KCn, 512], BF16)
    wbufs = [P.alloc([128, KCn, 256], BF16) for _ in range(3)]
    xr = [P.alloc([128, 512], F32) for _ in range(4)]
    xi = [0]
    for hb in range(2):
        P.dma("sp", act, actT[:, hb * 512:(hb + 1) * 512].rearrange("(kc p) t -> p kc t", p=128), r=["dram_" + tag + "a"], w=["pact"])

        def epi(b, col, cw, t0, tn, hb=hb):
            x = xi[0] % 4
            xi[0] += 1
            kcx = col // 128
            P.dma("sp", xr[x], resT[col:col + 128, hb * 512:(hb + 1) * 512], r=["dram_" + tag + "r"], w=[("xr", x)])
            P.op("dve", lambda e, b=b, x=x: e.scalar_tensor_tensor(out=xr[x], in0=C.ps[b][:, :], scalar=modq[:, gcol + kcx:gcol + kcx + 1], in1=xr[x],
                                                                   op0=ALU.mult, op1=ALU.add),
                 r=[("ps", b), ("xr", x), "c_modq"], w=[("xr", x)])
            P.dma("sp", vT[col:col + 128, hb * 512:(hb + 1) * 512], xr[x], r=[("xr", x)], w=["dram_" + tag + "v"], semkey=("prst", x))

        gemm_fm(C, act, KCn, Wv, 0, D, [(0, 512)], epi, tag, wbufs, "pact")
    stage_end(C)


def load_l0_consts(C, sfx=""):
    P = C.P
    K_ = {}
    K_["U"] = C.const("U", C.dram("c_U", [128, 128]), [128, 128])
    K_["maskneg"] = C.const("maskneg", C.dram("c_maskneg", [128, 128]), [128, 128])
    K_["aneg"] = C.const("aneg", C.dram("alog_rep", [128, 64]), [128, 64])
    K_["dtb"] = C.const("dtb", C.dram("dtb_rep", [128, 64]), [128, 64])
    K_["Drow_d"] = C.dram("drow_rep", [128, D])
    K_["flag"] = C.const("flag", C.dram("flag" + sfx, [128, 1]), [128, 1])
    K_["ssdnw"] = C.const("ssdnw", C.dram("ssdnw_p", [128, KC]), [128, KC])
    K_["pscale"] = C.const("pscale", C.dram("pscale_p", [128, KC]), [128, KC])
    K_["convw"] = C.const("convw", C.dram("convw_p", [128, 192]), [128, 192])
    K_["convb"] = C.const("convb", C.dram("convb_p", [128, 48]), [128, 48])
    K_["invc"] = C.dram("invc_rep" + sfx, [4, 128, NT])
    a = K_["aneg"]
    P.op("act", lambda e: e.activation(out=a, in_=a, func=AF.Exp), r=["c_aneg"], w=["c_aneg"])
    P.op("dve", lambda e: e.tensor_scalar(out=a, in0=a, scalar1=-1.0, scalar2=None, op0=ALU.mult), r=["c_aneg"], w=["c_aneg"])
    P.barrier()
    return K_


def host_l0_consts(half, conv_w, conv_b, dt_bias, a_log, d_skip, ssd_norm_w, pool_scale):
    m = {}
    m["c_U"] = np.triu(np.ones((128, 128), np.float32))
    m["c_maskneg"] = np.where(np.arange(128)[None, :] >= np.arange(128)[:, None], 0.0, -1e30).astype(np.float32)
    rep = lambda v: np.ascontiguousarray(np.broadcast_to(v[None, :], (128, v.shape[0])))
    m["alog_rep"] = rep(a_log[0])
    m["dtb_rep"] = rep(dt_bias[0])
    m["drow_rep"] = rep(np.repeat(d_skip[0], 64))
    m["flag"] = np.full((128, 1), float(half), np.float32)
    m["ssdnw_p"] = pp(ssd_norm_w[0])
    m["pscale_p"] = pp(pool_scale[0])
    m["convw_p"] = np.ascontiguousarray(conv_w[0].reshape(4, 48, 128).transpose(2, 1, 0).reshape(128, 192))
    m["convb_p"] = pp(conv_b[0])
    tg = half * NT + np.arange(NT) + 1
    ic = np.stack([1.0 / np.minimum(tg, w).astype(np.float32) for w in (2, 4, 8, 16)], 0)
    m["invc_rep"] = np.ascontiguousarray(np.broadcast_to(ic[:, None, :], (4, 128, NT))).astype(np.float32)
    return m


def layer0_mixer(C, modq, srcT, x1T, reg, sfx=""):
    K_ = load_l0_consts(C, sfx)
    Win = C.dram("in_proj", [D, 14400])
    poolw = C.dram("pool_w", [4, 1024, 1024])
    Wout = C.dram("out_proj", [2 * D, D])
    xbcT = C.dram("s_xbcT", [6144, 2 * NT])
    xactT = C.dram("s_xactT", [6144, 2 * NT])
    dt_tok = C.dram("s_dt", [2 * NT, 64])
    z_tok = C.dram("s_z", [NT, D])
    uT = C.dram("s_uT%d" % reg, [D, 16 + NT])
    uT_prev = C.dram("s_uT0", [D, 16 + NT])
    hstate = C.dram("s_hstate", [128, D])
    catT = C.dram("s_catT", [2 * D, NT], BF16)
    v1T = C.dram("s_v1T", [D, NT])
    stage_inproj(C, srcT, modq, Win, xbcT, dt_tok, z_tok, uT, reg)
    stage_conv(C, xbcT, xactT, K_["convw"], K_["convb"], reg, K_["flag"])
    stage_ssd(C, xactT, dt_tok, z_tok, catT, K_, reg, hstate)
    stage_pool(C, uT, catT, poolw, K_, reg, uT_prev)
    stage_proj_res(C, catT, 64, Wout, srcT, v1T, modq, 0 * 192 + 2 * 32, "op")
    lw = C.consts["lnw"]
    lb = C.consts["lnb"]
    stage_ln(C, v1T, x1T, lw[:, 0:32], lb[:, 0:32], LN_EPS / DN_ALPHA ** 2, "ln1")


QK_SCALE = 192 ** -0.5


def rms_feat(C, src, nch, wp, dstT, nfeat, tagk):
    P = C.P
    ones = C.consts["ones_f"]
    sq = [P.alloc([128, NT], F32) for _ in range(2)]
    rstd = P.alloc([128, NT], F32)
    ob = [P.alloc([128, NT], BF16) for _ in range(2)]
    for c in range(nch):
        q = c % 2
        P.op("act", lambda e, c=c, q=q: e.activation(out=sq[q], in_=src[:, c, :], func=AF.Square), r=[tagk], w=[("rsq", q)])
        for hb in range(2):
            P.op("pe", lambda e, q=q, hb=hb, c=c: e.matmul(C.ps[hb][:, :], lhsT=ones, rhs=sq[q][:, hb * 512:(hb + 1) * 512], start=(c == 0), stop=(c == nch - 1)),
                 r=[("rsq", q), "c_ones_f"], w=[("ps", hb)])
    for hb in range(2):
        sl = slice(hb * 512, (hb + 1) * 512)
        P.op("dve", lambda e, hb=hb, sl=sl: e.tensor_scalar(out=rstd[:, sl], in0=C.ps[hb][:, :], scalar1=1.0 / nfeat, scalar2=RMS_EPS, op0=ALU.mult, op1=ALU.add),
             r=[("ps", hb)], w=["rrstd"])
    P.op("act", lambda e: e.activation(out=rstd, in_=rstd, func=AF.Sqrt), r=["rrstd"], w=["rrstd"])
    P.op("dve", lambda e: e.reciprocal(out=rstd, in_=rstd), r=["rrstd"], w=["rrstd"])
    for c in range(nch):
        q = c % 2
        P.op("dve", lambda e, c=c, q=q: e.tensor_tensor(out=sq[q], in0=src[:, c, :], in1=rstd, op=ALU.mult), r=[tagk, "rrstd"], w=[("rsq", q)])
        P.op("act", lambda e, c=c, q=q: e.activation(out=ob[q], in_=sq[q], func=AF.Copy, scale=wp[:, c:c + 1]), r=[("rsq", q)], w=[("rob", q)])
        P.dma("sp", dstT[c * 128:(c + 1) * 128, :], ob[q], r=[("rob", q)], w=["dram_rms" + str(tagk)], semkey=("robst", q))


def rotary_fm(C, src, dst_bf, cosT, sinT, rotm, scale, key_in, key_out, t1, t2):
    P = C.P
    for hb in range(2):
        sl = slice(hb * 512, (hb + 1) * 512)
        b = 2 + hb
        P.op("pe", lambda e, b=b, sl=sl: e.matmul(C.ps[b][0:64, :], lhsT=rotm, rhs=src[:, sl], start=True, stop=True), r=[key_in, "c_rotm"], w=[("ps", b)])
        P.op("dve", lambda e, b=b, sl=sl: e.tensor_tensor(out=t1[:, sl], in0=C.ps[b][0:64, :], in1=sinT[:, sl], op=ALU.mult), r=[("ps", b), "c_sinT"], w=[("rt1", hb)])
        P.op("pool", lambda e, sl=sl: e.tensor_tensor(out=t2[:, sl], in0=src[:, sl], in1=cosT[:, sl], op=ALU.mult), r=[key_in, "c_cosT"], w=[("rt2", hb)])
        P.op("dve", lambda e, sl=sl: e.tensor_tensor(out=t1[:, sl], in0=t1[:, sl], in1=t2[:, sl], op=ALU.add), r=[("rt1", hb), ("rt2", hb)], w=[("rt1", hb)])
        P.op("act", lambda e, sl=sl: e.activation(out=dst_bf[:, sl], in_=t1[:, sl], func=AF.Copy, scale=float(scale)), r=[("rt1", hb)], w=[key_out])


def stage_mla_proj(C, x2T, modq, Wqa, Wkva, qnT, kvnT, kpeT, L1, do_q=True):
    P = C.P
    stage_begin(C)
    C.wi = 0
    hm = P.alloc([128, KC, NT], BF16)
    xb = [P.alloc([128, NT], F32) for _ in range(2)]
    wbufs = [P.alloc([128, KC, 256], BF16) for _ in range(2)]
    make_hmix(C, x2T, hm, modq, 192 + 1 * 32, 192 + 0 * 32, xb, "hm1")
    toks = [(0, 512), (512, 512)]
    P.mark()
    if do_q:
        qa = P.alloc([128, 8, NT], F32)
        gemm_fm(C, hm, KC, Wqa.rearrange("(kc p) n -> p kc n", p=128), 0, 1024, toks,
                lambda b, col, cw, t0, tn: P.op("act", lambda e, b=b: e.activation(out=qa[:, col // 128, t0:t0 + tn], in_=C.ps[b][:, 0:tn], func=AF.Copy),
                                                r=[("ps", b)], w=["qa"]),
                "qa", wbufs, "hm1")
        P.barrier()
        rms_feat(C, qa, 8, L1["qnw"], qnT, 1024, "qa")
    P.barrier()
    P.release()
    kvc = P.alloc([128, 5, NT], F32)
    gemm_fm(C, hm, KC, Wkva.rearrange("(kc p) n -> p kc n", p=128), 0, 576, toks,
            lambda b, col, cw, t0, tn: P.op("act", lambda e, b=b: e.activation(out=kvc[0:cw, col // 128, t0:t0 + tn], in_=C.ps[b][0:cw, 0:tn], func=AF.Copy),
                                            r=[("ps", b)], w=["kvc"]),
            "kva", wbufs, "hm1")
    P.barrier()
    rms_feat(C, kvc, 4, L1["kvnw"], kvnT, 512, "kvc")
    kpb = P.alloc([64, NT], BF16)
    rt1 = P.alloc([64, NT], F32)
    rt2 = P.alloc([64, NT], F32)
    rotary_fm(C, kvc[0:64, 4, :], kpb, L1["cosT"], L1["sinT"], L1["rotm"], 1.0, "kvc", "kpb", rt1, rt2)
    P.dma("sp", kpeT, kpb, r=["kpb"], w=["dram_kpe"], semkey="kpbst")
    stage_end(C)


def stage_attn(C, qnT, kvnT_prev, kpeT_prev, kvnT, kpeT, Wqb, Wkvb, oT, L1):
    P = C.P
    stage_begin(C)
    onesb = L1["ones_b"]
    qn = P.alloc([128, 8, NT], BF16)
    kvn = P.alloc([128, 4, 2 * NT], BF16)
    kpe = P.alloc([64, 2 * NT], BF16)
    P.dma("sp", qn, qnT.rearrange("(kc p) t -> p kc t", p=128), r=["dram_qn"], w=["qn"])
    P.dma("sp", kvn[:, :, 0:NT], kvnT_prev.rearrange("(kc p) t -> p kc t", p=128), r=["dram_kvp"], w=["kvn0"])
    P.dma("sp", kvn[:, :, NT:2 * NT], kvnT.rearrange("(kc p) t -> p kc t", p=128), r=["dram_kv"], w=["kvn1"])
    P.dma("sp", kpe[:, 0:NT], kpeT_prev, r=["dram_kpp"], w=["kpe0"])
    P.dma("sp", kpe[:, NT:2 * NT], kpeT, r=["dram_kp"], w=["kpe1"])
    kin = ["kvn0", "kvn1"]
    wq = [P.alloc([128, 8, 192], BF16) for _ in range(2)]
    wk = [P.alloc([128, 4, 256], BF16) for _ in range(2)]
    qno_ = [P.alloc([128, NT], BF16) for _ in range(2)]
    qpf_ = [P.alloc([64, NT], F32) for _ in range(2)]
    qpe_ = [P.alloc([64, NT], BF16) for _ in range(2)]
    rt1 = P.alloc([64, NT], F32)
    rt2 = P.alloc([64, NT], F32)
    kno_ = [P.alloc([128, 2 * NT], BF16) for _ in range(2)]
    vtok_ = [P.alloc([128, 16, 128], BF16) for _ in range(2)]
    pt = [P.alloc([128, 512], BF16) for _ in range(3)]
    rinv = P.alloc([128, 512], F32)
    ob = [P.alloc([128, 512], BF16) for _ in range(2)]
    Wqv = Wqb.rearrange("(kc p) n -> p kc n", p=128)
    Wkv = Wkvb.rearrange("(kc p) n -> p kc n", p=128)
    rot = [0]

    def rb():
        b = rot[0] % 4
        rot[0] += 1
        return b
    pti = 0
    obi = 0
    for h in range(32):
        s = h % 2
        qno, qpf, qpe, kno, vtok = qno_[s], qpf_[s], qpe_[s], kno_[s], vtok_[s]
        kq = lambda n, s=s: (n, s)
        if h == 0:
            P.dma("pool", wq[0], Wqv[:, :, 0:192], r=["dram_wqb"], w=[("wq", 0)])
            P.dma("pool", wk[0], Wkv[:, :, 0:256], r=["dram_wkvb"], w=[("wk", 0)])
        for hb in range(2):
            sl = slice(hb * 512, (hb + 1) * 512)
            b = rb()
            for kc in range(8):
                P.op("pe", lambda e, qno=qno, qpf=qpf, qpe=qpe, kno=kno, vtok=vtok, b=b, s=s, kc=kc, sl=sl: e.matmul(C.ps[b][:, :], lhsT=wq[s][:, kc, 0:128], rhs=qn[:, kc, sl], start=(kc == 0), stop=(kc == 7)),
                     r=[("wq", s), "qn"], w=[("ps", b)])
            P.op("act", lambda e, qno=qno, qpf=qpf, qpe=qpe, kno=kno, vtok=vtok, b=b, sl=sl: e.activation(out=qno[:, sl], in_=C.ps[b][:, :], func=AF.Copy, scale=float(QK_SCALE)), r=[("ps", b)], w=[kq("qno")])
            b = rb()
            for kc in range(8):
                P.op("pe", lambda e, qno=qno, qpf=qpf, qpe=qpe, kno=kno, vtok=vtok, b=b, s=s, kc=kc, sl=sl: e.matmul(C.ps[b][0:64, :], lhsT=wq[s][:, kc, 128:192], rhs=qn[:, kc, sl], start=(kc == 0), stop=(kc == 7)),
                     r=[("wq", s), "qn"], w=[("ps", b)])
            P.op("act", lambda e, qno=qno, qpf=qpf, qpe=qpe, kno=kno, vtok=vtok, b=b, sl=sl: e.activation(out=qpf[:, sl], in_=C.ps[b][0:64, :], func=AF.Copy), r=[("ps", b)], w=[kq("qpf")])
        rotary_fm(C, qpf, qpe, L1["cosT"], L1["sinT"], L1["rotm"], QK_SCALE, kq("qpf"), kq("qpe"), rt1, rt2)
        for kb in range(4):
            sl = slice(kb * 512, (kb + 1) * 512)
            b = rb()
            for kc in range(4):
                P.op("pe", lambda e, qno=qno, qpf=qpf, qpe=qpe, kno=kno, vtok=vtok, b=b, s=s, kc=kc, sl=sl: e.matmul(C.ps[b][:, :], lhsT=wk[s][:, kc, 0:128], rhs=kvn[:, kc, sl], start=(kc == 0), stop=(kc == 3)),
                     r=[("wk", s)] + kin, w=[("ps", b)])
            P.op("act", lambda e, qno=qno, qpf=qpf, qpe=qpe, kno=kno, vtok=vtok, b=b, sl=sl: e.activation(out=kno[:, sl], in_=C.ps[b][:, :], func=AF.Copy), r=[("ps", b)], w=[kq("kno")])
        for q4 in range(4):
            b = rb()
            for j in range(4):
                kt = q4 * 4 + j
                for kc in range(4):
                    P.op("pe", lambda e, qno=qno, qpf=qpf, qpe=qpe, kno=kno, vtok=vtok, b=b, s=s, kc=kc, kt=kt, j=j: e.matmul(C.ps[b][:, j * 128:(j + 1) * 128], lhsT=kvn[:, kc, kt * 128:(kt + 1) * 128], rhs=wk[s][:, kc, 128:256],
                                                                              start=(kc == 0), stop=(kc == 3)),
                         r=[("wk", s)] + kin, w=[("ps", b)])
            P.op("dve", lambda e, qno=qno, qpf=qpf, qpe=qpe, kno=kno, vtok=vtok, b=b, q4=q4: e.tensor_copy(out=vtok[:, q4 * 4:(q4 + 1) * 4, :].rearrange("p a b -> p (a b)"), in_=C.ps[b][:, :]), r=[("ps", b)], w=[kq("vtok")])
        if h + 1 < 32:
            P.dma("pool", wq[1 - s], Wqv[:, :, (h + 1) * 192:(h + 2) * 192], r=["dram_wqb"], w=[("wq", 1 - s)])
            P.dma("pool", wk[1 - s], Wkv[:, :, (h + 1) * 256:(h + 2) * 256], r=["dram_wkvb"], w=[("wk", 1 - s)])
        for qb in range(2):
            qsl = slice(qb * 512, (qb + 1) * 512)
            kts = list(range(8)) + [8 + j for j in range(4 * qb + 4)]
            for i_, kt in enumerate(kts):
                ksl = slice(kt * 128, (kt + 1) * 128)
                b = rb()
                P.op("pe", lambda e, qno=qno, qpf=qpf, qpe=qpe, kno=kno, vtok=vtok, b=b, ksl=ksl, qsl=qsl: e.matmul(C.ps[b][:, :], lhsT=kno[:, ksl], rhs=qno[:, qsl], start=True, stop=False),
                     r=[kq("kno"), kq("qno")], w=[("ps", b)])
                P.op("pe", lambda e, qno=qno, qpf=qpf, qpe=qpe, kno=kno, vtok=vtok, b=b, ksl=ksl, qsl=qsl: e.matmul(C.ps[b][:, :], lhsT=kpe[:, ksl], rhs=qpe[:, qsl], start=False, stop=True),
                     r=["kpe0", "kpe1", kq("qpe")], w=[("ps", b)])
                p_ = pti % 3
                pti += 1
                if kt < 8:
                    P.op("act", lambda e, qno=qno, qpf=qpf, qpe=qpe, kno=kno, vtok=vtok, b=b, p_=p_: e.activation(out=pt[p_], in_=C.ps[b][:, :], func=AF.Exp, bias=L1["kbias"][:, 0:1], scale=1.0),
                         r=[("ps", b), "c_kbias"], w=[("pt", p_)])
                else:
                    P.op("act", lambda e, qno=qno, qpf=qpf, qpe=qpe, kno=kno, vtok=vtok, b=b, p_=p_: e.activation(out=pt[p_], in_=C.ps[b][:, :], func=AF.Exp), r=[("ps", b)], w=[("pt", p_)])
                    j = kt - 8 - 4 * qb
                    if j >= 0:
                        P.op("dve", lambda e, qno=qno, qpf=qpf, qpe=qpe, kno=kno, vtok=vtok, p_=p_, j=j: e.tensor_tensor(out=pt[p_], in0=pt[p_], in1=L1["mask01"][:, j * 512:(j + 1) * 512], op=ALU.mult),
                             r=[("pt", p_), "c_mask01"], w=[("pt", p_)])
                first = (i_ == 0)
                last = (i_ == len(kts) - 1)
                P.op("pe", lambda e, qno=qno, qpf=qpf, qpe=qpe, kno=kno, vtok=vtok, qb=qb, p_=p_, kt=kt, first=first, last=last: e.matmul(C.ps[4 + 2 * qb][:, :], lhsT=vtok[:, kt, :], rhs=pt[p_], start=first, stop=last),
                     r=[kq("vtok"), ("pt", p_)], w=[("ps", 4 + 2 * qb)])
                P.op("pe", lambda e, qno=qno, qpf=qpf, qpe=qpe, kno=kno, vtok=vtok, qb=qb, p_=p_, first=first, last=last: e.matmul(C.ps[5 + 2 * qb][:, :], lhsT=onesb, rhs=pt[p_], start=first, stop=last),
                     r=["c_ones_b", ("pt", p_)], w=[("ps", 5 + 2 * qb)])
            P.op("dve", lambda e, qno=qno, qpf=qpf, qpe=qpe, kno=kno, vtok=vtok, qb=qb: e.reciprocal(out=rinv, in_=C.ps[5 + 2 * qb][:, :]), r=[("ps", 5 + 2 * qb)], w=["rinv"])
            o = obi % 2
            obi += 1
            P.op("dve", lambda e, qno=qno, qpf=qpf, qpe=qpe, kno=kno, vtok=vtok, qb=qb, o=o: e.tensor_tensor(out=ob[o], in0=C.ps[4 + 2 * qb][:, :], in1=rinv, op=ALU.mult), r=[("ps", 4 + 2 * qb), "rinv"], w=[("aob", o)])
            P.dma("sp", oT[h * 128:(h + 1) * 128, qsl], ob[o], r=[("aob", o)], w=["dram_oT"], semkey=("aobst", o))
    stage_end(C)


def load_l1_consts(C, attn, sfx=""):
    L1 = {}
    L1["qnw"] = C.const("qnw", C.dram_once("qnw_p", [128, 8]), [128, 8])
    L1["kvnw"] = C.const("kvnw", C.dram_once("kvnw_p", [128, 4]), [128, 4])
    L1["cosT"] = C.const("cosT", C.dram_once("cosT" + sfx, [64, NT]), [64, NT])
    L1["sinT"] = C.const("sinT", C.dram_once("sinT" + sfx, [64, NT]), [64, NT])
    L1["rotm"] = C.const("rotm", C.dram_once("rotm", [64, 64]), [64, 64])
    if attn:
        L1["ones_b"] = C.const("ones_b", C.dram_once("c_ones_b", [128, 128], BF16), [128, 128], BF16)
        L1["kbias"] = C.const("kbias", C.dram_once("kbias", [128, 1]), [128, 1])
        L1["mask01"] = C.const("mask01", C.dram_once("c_mask01", [128, 4 * 512], BF16), [128, 4 * 512], BF16)
    C.P.barrier()
    return L1


def host_l1_consts(half, q_norm_w, kv_norm_w):
    import ml_dtypes
    m = {}
    m["qnw_p"] = pp(q_norm_w[0])
    m["kvnw_p"] = pp(kv_norm_w[0])
    pos = (half * NT + np.arange(NT)).astype(np.float32)
    inv = (10000.0 ** (-np.arange(32, dtype=np.float32) * 2.0 / 64)).astype(np.float32)
    ang = pos[None, :] * inv[:, None]
    m["cosT"] = np.repeat(np.cos(ang), 2, axis=0).astype(np.float32)
    m["sinT"] = np.repeat(np.sin(ang), 2, axis=0).astype(np.float32)
    rot = np.zeros((64, 64), np.float32)
    for i in range(32):
        rot[2 * i + 1, 2 * i] = -1.0
        rot[2 * i, 2 * i + 1] = 1.0
    m["rotm"] = rot
    m["c_ones_b"] = np.ones((128, 128), ml_dtypes.bfloat16)
    m["kbias"] = np.full((128, 1), 0.0 if half == 1 else -1e30, np.float32)
    k = np.arange(128)[:, None]
    q = np.arange(512)[None, :]
    m["c_mask01"] = np.concatenate([(q >= j * 128 + k) for j in range(4)], axis=1).astype(ml_dtypes.bfloat16)
    return m


ADA_COLS = 6 * D // 8
ADA_CC = ADA_COLS // 128


def stage_ada(C, cT, adaw, adab_p, modo):
    P = C.P
    stage_begin(C)
    cs = P.alloc([128, KC, 4], F32)
    P.dma("sp", cs, cT.rearrange("(kc p) b -> p kc b", p=128), r=["dram_c"], w=["cs"])
    P.op("act", lambda e: e.activation(out=cs, in_=cs, func=AF.Silu), r=["cs"], w=["cs"])
    ab = P.alloc([128, DEPTH * ADA_CC], F32)
    P.dma("sp", ab, adab_p, r=["dram_ab"], w=["ab"])
    ob = P.alloc([128, DEPTH * ADA_CC * 4], F32)
    wb = [P.alloc([128, KC, 128], F32) for _ in range(3)]
    n = 0
    for i in range(DEPTH):
        Wv = adaw[i].rearrange("(kc p) n -> p kc n", p=128)
        for cc in range(ADA_CC):
            s = n % 3
            P.dma("sp", wb[s], Wv[:, :, cc * 128:(cc + 1) * 128], r=["dram_adaw"], w=[("awb", s)])
            b = C.bank()
            for kc in range(KC):
                P.op("pe", lambda e, b=b, s=s, kc=kc: e.matmul(C.ps[b][:, 0:4], lhsT=wb[s][:, kc, :], rhs=cs[:, kc, :], start=(kc == 0), stop=(kc == KC - 1)),
                     r=[("awb", s), "cs"], w=[("ps", b)])
            P.op("dve", lambda e, b=b, n=n: e.tensor_scalar(out=ob[:, n * 4:(n + 1) * 4], in0=C.ps[b][:, 0:4], scalar1=ab[:, n:n + 1], scalar2=None, op0=ALU.add),
                 r=[("ps", b), "ab"], w=["aob"])
            n += 1
    P.dma("sp", modo, ob, r=["aob"], w=["dram_modo"], semkey="aobst")
    stage_end(C)


def _finish(C):
    C.P.final_wait("sp")
    C.P.emit()


def build_ada():
    nc = bass.Bass("TRN2", target_bir_lowering=False)
    with contextlib.ExitStack() as stack:
        C = Ctx(nc, stack, {"cT": "in", "adaw": "in", "adab_p": "in", "modo": "out"})
        stage_ada(C, C.dram("cT", [D, 4]), C.dram("adaw", [DEPTH, D, ADA_COLS]), C.dram("adab_p", [128, DEPTH * ADA_CC]),
                  C.dram("modo", [128, DEPTH * ADA_CC * 4]))
        _finish(C)
    return nc


class _AllIn(dict):
    def __init__(self, outs, scratch_prefix="s_"):
        super().__init__()
        self.outs = set(outs)
        self.sp = scratch_prefix

    def get(self, k, default=None):
        if k in self.outs:
            return "out"
        if k.startswith(self.sp):
            return None
        return "in"


def load_consts_fused(C):
    C.const("ones_f", C.dram("c_ones", [128, 128]), [128, 128])
    C.const("ident", C.dram("c_ident", [128, 128]), [128, 128])
    C.const("lnw", C.dram("lnw_p", [128, DEPTH * 2 * KC]), [128, DEPTH * 2 * KC])
    C.const("lnb", C.dram("lnb_p", [128, DEPTH * 2 * KC]), [128, DEPTH * 2 * KC])
    modq = C.P.alloc([128, DEPTH * 6 * KC], F32)
    C.consts["modq"] = modq
    return modq


def stage_ada_own(C, modq):
    P = C.P
    stage_begin(C)
    cs = P.alloc([128, KC], F32)
    P.dma("sp", cs, C.dram("c_own_p", [128, KC]), r=["dram_c"], w=["cs"])
    P.op("act", lambda e: e.activation(out=cs, in_=cs, func=AF.Silu), r=["cs"], w=["cs"])
    cb = P.alloc([128, KC], BF16)
    P.op("dve", lambda e: e.tensor_copy(out=cb, in_=cs), r=["cs"], w=["cb"])
    ab = P.alloc([128, DEPTH * 192], F32)
    P.dma("sp", ab, C.dram("adab_full_p", [128, DEPTH * 192]), r=["dram_ab"], w=["ab"])
    wb = [P.alloc([128, KC, 512], BF16) for _ in range(3)]
    adaw = C.dram("ada_w", [DEPTH, D, 6 * D])
    n = 0
    for i in range(DEPTH):
        Wv = adaw[i].rearrange("(kc p) n -> p kc n", p=128)
        for c4 in range(48):
            s = n % 3
            n += 1
            P.dma("pool", wb[s], Wv[:, :, c4 * 512:(c4 + 1) * 512], r=["dram_adaw"], w=[("awb", s)])
            for j in range(4):
                b = C.bank()
                col = i * 192 + c4 * 4 + j
                for kc in range(KC):
                    P.op("pe", lambda e, b=b, s=s, kc=kc, j=j: e.matmul(C.ps[b][:, 0:1], lhsT=wb[s][:, kc, j * 128:(j + 1) * 128], rhs=cb[:, kc:kc + 1],
                                                                       start=(kc == 0), stop=(kc == KC - 1)),
                         r=[("awb", s), "cb"], w=[("ps", b)])
                P.op("dve", lambda e, b=b, col=col: e.tensor_tensor(out=modq[:, col:col + 1], in0=C.ps[b][:, 0:1], in1=ab[:, col:col + 1], op=ALU.add),
                     r=[("ps", b), "ab"], w=["c_modq"])
    for i in range(DEPTH):
        for s_ in (1, 4):
            sl = slice(i * 192 + s_ * 32, i * 192 + s_ * 32 + 32)
            P.op("dve", lambda e, sl=sl: e.tensor_scalar(out=modq[:, sl], in0=modq[:, sl], scalar1=1.0, scalar2=None, op0=ALU.add),
                 r=["c_modq"], w=["c_modq"])
        for s_ in (2, 5):
            sl = slice(i * 192 + s_ * 32, i * 192 + s_ * 32 + 32)
            P.op("dve", lambda e, sl=sl: e.tensor_scalar(out=modq[:, sl], in0=modq[:, sl], scalar1=1.0, scalar2=1.0 / DN_ALPHA, op0=ALU.add, op1=ALU.mult),
                 r=["c_modq"], w=["c_modq"])
    stage_end(C)


def build_fused(upto=None, outs=("outT",)):
    nc = bass.Bass("TRN2", target_bir_lowering=False)
    with contextlib.ExitStack() as stack:
        C = Ctx(nc, stack, _AllIn(list(outs)))
        P = C.P
        modq = load_consts_fused(C)
        stage_ada_own(C, modq)
        xT = C.dram("xT", [D, NT])
        xpT = C.dram("xpT", [D, NT])
        x1T = C.dram("s_x1T", [D, NT])
        v2T = C.dram("s_v2T", [D, NT])
        YE = C.dram("s_YE", [NE * 2 * 128, D], BF16)
        STP = C.dram("s_STP", [NE * 2 * 128, NT], BF16)
        x2T = C.dram("s_x2T", [D, NT])
        qnT = C.dram("s_qnT", [1024, NT], BF16)
        lw = C.consts["lnw"]
        lb = C.consts["lnb"]
        eps2 = LN_EPS / DN_ALPHA ** 2
        for ps_ in ("P", "O"):
            src = xpT if ps_ == "P" else xT
            P.mark()
            layer0_mixer(C, modq, src, x1T, 0 if ps_ == "P" else 1, "_" + ps_)
            P.barrier()
            P.release()
            stage_moe_c(C, 0, x1T, v2T, YE, STP, modq, moe_weight_aps(C, "0"))
            stage_ln(C, v2T, x2T, lw[:, 32:64], lb[:, 32:64], eps2, "ln2")
            P.mark()
            L1 = load_l1_consts(C, False, "_" + ps_)
            stage_mla_proj(C, x2T, modq, C.dram("wq_a", [D, 1024]), C.dram("wkv_a", [D, 576]),
                           qnT, C.dram("s_kvnT_" + ps_, [512, NT], BF16), C.dram("s_kpeT_" + ps_, [64, NT], BF16), L1, do_q=(ps_ == "O"))
            P.barrier()
            P.release()
            if upto == ps_:
                _finish(C)
                return nc
        P.mark()
        L1 = load_l1_consts(C, True, "_O")
        oT = C.dram("s_oT", [D, NT], BF16)
        stage_attn(C, qnT, C.dram("s_kvnT_P", [512, NT], BF16), C.dram("s_kpeT_P", [64, NT], BF16),
                   C.dram("s_kvnT_O", [512, NT], BF16), C.dram("s_kpeT_O", [64, NT], BF16),
                   C.dram("wq_b", [1024, 6144]), C.dram("wkv_b", [512, 8192]), oT, L1)
        P.barrier()
        P.release()
        v3T = C.dram("s_v3T", [D, NT])
        x3T = C.dram("s_x3T", [D, NT])
        stage_proj_res(C, oT, 32, C.dram("wo", [D, D]), x2T, v3T, modq, 192 + 2 * 32, "wo")
        stage_ln(C, v3T, x3T, lw[:, 64:96], lb[:, 64:96], eps2, "ln3")
        v4T = C.dram("s_v4T", [D, NT])
        outT = C.dram("outT", [D, NT])
        stage_moe_c(C, 1, x3T, v4T, YE, STP, modq, moe_weight_aps(C, "1"))
        stage_ln(C, v4T, outT, lw[:, 96:128], lb[:, 96:128], eps2, "ln4")
        _finish(C)
    return nc


def kernel(x, c, ada_w, ada_b, ln_w, ln_b, in_proj, conv_w, conv_b, dt_bias, a_log, d_skip,
           ssd_norm_w, pool_w, pool_scale, out_proj, wq_a, q_norm_w, wq_b, wkv_a, kv_norm_w,
           wkv_b, wo, router_w, router_b, w_gu, b_gu, w_down, b_down):
    A = lambda a: np.asarray(a)
    x, c, ada_w, ada_b, ln_w, ln_b = map(A, (x, c, ada_w, ada_b, ln_w, ln_b))
    cores = list(range(8))
    nc = build_fused()
    W = dict(host_consts())
    del W["c_sel"]
    W["c_sel"] = host_consts()["c_sel"]
    W["lnw_p"] = np.concatenate([pp(ln_w[i, j]) for i in range(2) for j in range(2)], axis=1)
    W["lnb_p"] = np.concatenate([pp(ln_b[i, j]) for i in range(2) for j in range(2)], axis=1)
    W["ada_w"] = ada_w
    W["adab_full_p"] = np.concatenate([pp(ada_b[i]) for i in range(DEPTH)], axis=1)
    for li in range(2):
        W.update(host_moe_weights(li, A(router_w), A(router_b), A(w_gu), A(b_gu), A(w_down), A(b_down), str(li)))
    W["in_proj"] = A(in_proj)[0]
    W["pool_w"] = A(pool_w)[0]
    W["out_proj"] = A(out_proj)[0]
    W["wq_a"] = A(wq_a)[0]
    W["wkv_a"] = A(wkv_a)[0]
    W["wq_b"] = A(wq_b)[0]
    W["wkv_b"] = A(wkv_b)[0]
    W["wo"] = A(wo)[0]
    l0 = [host_l0_consts(h, A(conv_w), A(conv_b), A(dt_bias), A(a_log), A(d_skip), A(ssd_norm_w), A(pool_scale)) for h in range(2)]
    l1 = [host_l1_consts(h, A(q_norm_w), A(kv_norm_w)) for h in range(2)]
    for k_, v_ in l0[0].items():
        if k_ not in ("flag", "invc_rep"):
            W[k_] = v_
    for k_, v_ in l1[0].items():
        if k_ not in ("cosT", "sinT", "kbias"):
            W[k_] = v_
    ins = []
    for k in cores:
        b, half = k // 2, k % 2
        m = dict(W)
        m["flag_P"] = l0[0]["flag"]
        m["invc_rep_P"] = l0[0]["invc_rep"]
        m["flag_O"] = l0[half]["flag"]
        m["invc_rep_O"] = l0[half]["invc_rep"]
        m["cosT_P"] = l1[0]["cosT"]
        m["sinT_P"] = l1[0]["sinT"]
        m["cosT_O"] = l1[half]["cosT"]
        m["sinT_O"] = l1[half]["sinT"]
        m["kbias"] = l1[half]["kbias"]
        m["c_own_p"] = pp(c[b])
        m["xT"] = np.ascontiguousarray(x[b, half * NT:(half + 1) * NT].T)
        m["xpT"] = np.ascontiguousarray(x[b, 0:NT].T)
        ins.append(m)
    r = run_bass_kernel_spmd(nc, ins, core_ids=cores)
    out = np.zeros((4, 2 * NT, D), np.float32)
    for k in cores:
        b, half = k // 2, k % 2
        out[b, half * NT:(half + 1) * NT, :] = r.results[k]["outT"].T
    return out


CAP = 256


def stage_moe_c(C, li, x1T, vT, YE, STP, modq, W):
    P = C.P
    ones = C.consts["ones_f"]
    ident = C.consts["ident"]
    base = li * 192
    stage_begin(C)
    hfft = P.alloc([128, 8, D], BF16)
    PTk = P.alloc([128, NT], F32)
    POSMT = P.alloc([128, NT], F32)
    posm = P.alloc([128, 8, NE], F32)
    P.mark()
    rw = P.alloc([128, KC, NE], F32)
    P.dma("sp", rw, W["router_w"].rearrange("(kc p) n -> p kc n", p=128), r=["dram_rw"], w=["rw"])
    rb = P.alloc([128, NE], F32)
    P.dma("sp", rb, W["router_b_rep"], r=["dram_rb"], w=["rb"])
    us = P.alloc([128, 128], F32)
    P.dma("sp", us, C.dram("c_Us", [128, 128]), r=["dram_us"], w=["us"])
    xb = [P.alloc([128, NT], F32) for _ in range(2)]
    sc_c = base + 4 * 32
    sh_c = base + 3 * 32
    for kc in range(KC):
        s = kc % 2
        P.dma("sp", xb[s], x1T[kc * 128:(kc + 1) * 128, :], r=["dram_x1"], w=[("xb", s)])
        P.op("dve", lambda e, s=s, kc=kc: e.tensor_scalar(out=xb[s], in0=xb[s], scalar1=modq[:, sc_c + kc:sc_c + kc + 1],
                                                          scalar2=modq[:, sh_c + kc:sh_c + kc + 1], op0=ALU.mult, op1=ALU.add),
             r=[("xb", s), "c_modq"], w=[("xb", s)])
        for tt in range(8):
            P.op("pe", lambda e, s=s, kc=kc, tt=tt: e.matmul(C.ps[tt][:, 0:NE], lhsT=xb[s][:, tt * 128:(tt + 1) * 128], rhs=rw[:, kc, :],
                                                             start=(kc == 0), stop=(kc == KC - 1)),
                 r=[("xb", s), "rw"], w=[("ps", tt)])
    mkall = P.alloc([128, 8, NE], F32)
    exall = P.alloc([128, 8, NE], F32)
    lg = [P.alloc([128, NE], F32) for _ in range(2)]
    t8 = [P.alloc([128, 8], F32) for _ in range(2)]
    sm = [P.alloc([128, 2], F32) for _ in range(2)]
    for tt in range(8):
        s = tt % 2
        K = lambda n, s=s: (n, s)
        mk_ = mkall[:, tt, :]
        ex_ = exall[:, tt, :]
        P.op("dve", lambda e, s=s, tt=tt: e.tensor_tensor(out=lg[s], in0=C.ps[tt][:, 0:NE], in1=rb, op=ALU.add), r=[("ps", tt), "rb"], w=[K("lg")])
        P.op("dve", lambda e, s=s: e.max(out=t8[s], in_=lg[s]), r=[K("lg")], w=[K("t8")])
        P.op("dve", lambda e, s=s, mk_=mk_: e.tensor_scalar(out=mk_, in0=lg[s], scalar1=t8[s][:, 3:4], scalar2=None, op0=ALU.is_ge),
             r=[K("lg"), K("t8")], w=[("mk", tt)])
        P.op("dve", lambda e, s=s: e.tensor_scalar(out=sm[s][:, 0:1], in0=t8[s][:, 0:1], scalar1=-1.0, scalar2=None, op0=ALU.mult), r=[K("t8")], w=[K("sm0")])
        P.op("act", lambda e, s=s, ex_=ex_: e.activation(out=ex_, in_=lg[s], func=AF.Exp, bias=sm[s][:, 0:1], scale=1.0), r=[K("lg"), K("sm0")], w=[("ex", tt)])
        P.op("dve", lambda e, ex_=ex_, mk_=mk_: e.tensor_tensor(out=ex_, in0=ex_, in1=mk_, op=ALU.mult), r=[("ex", tt), ("mk", tt)], w=[("ex", tt)])
        P.op("dve", lambda e, s=s, ex_=ex_: e.reduce_sum(out=sm[s][:, 1:2], in_=ex_, axis=AX.X), r=[("ex", tt)], w=[K("sm1")])
        P.op("dve", lambda e, s=s: e.reciprocal(out=sm[s][:, 1:2], in_=sm[s][:, 1:2]), r=[K("sm1")], w=[K("sm1")])
        P.op("dve", lambda e, s=s, ex_=ex_: e.tensor_scalar(out=ex_, in0=ex_, scalar1=sm[s][:, 1:2], scalar2=None, op0=ALU.mult), r=[("ex", tt), K("sm1")], w=[("ex", tt)])
    P.barrier()
    for tt in range(8):
        b = C.bank()
        P.op("pe", lambda e, b=b, tt=tt: e.matmul(C.ps[b][:, 0:NE], lhsT=us, rhs=mkall[:, tt, :], start=True, stop=(tt == 0)), r=["us", ("mk", tt)], w=[("ps", b)])
        for t2 in range(tt):
            P.op("pe", lambda e, b=b, t2=t2, tt=tt: e.matmul(C.ps[b][:, 0:NE], lhsT=ones, rhs=mkall[:, t2, :], start=False, stop=(t2 == tt - 1)),
                 r=["c_ones_f", ("mk", t2)], w=[("ps", b)])
        P.op("dve", lambda e, b=b, tt=tt: e.scalar_tensor_tensor(out=posm[:, tt, :], in0=C.ps[b][:, 0:NE], scalar=1.0, in1=mkall[:, tt, :], op0=ALU.add, op1=ALU.mult),
             r=[("ps", b), ("mk", tt)], w=[("posm", tt)])
        P.op("dve", lambda e, tt=tt: e.tensor_scalar(out=posm[:, tt, :], in0=posm[:, tt, :], scalar1=-1.0, scalar2=None, op0=ALU.add), r=[("posm", tt)], w=[("posm", tt)])
    P.barrier()
    for src_, dst_, nm in ((exall, PTk, "PTk"), (posm, POSMT, "POSMT")):
        for pb in range(2):
            b = C.bank()
            for j in range(4):
                tt = pb * 4 + j
                P.op("pe", lambda e, b=b, j=j, tt=tt, src_=src_: e.transpose(C.ps[b][0:NE, j * 128:(j + 1) * 128], src_[:, tt, :], ident), r=["c_ident"], w=[("ps", b)])
            P.op("act", lambda e, b=b, pb=pb, dst_=dst_: e.activation(out=dst_[0:NE, pb * 512:(pb + 1) * 512], in_=C.ps[b][0:NE, :], func=AF.Copy), r=[("ps", b)], w=[nm])
    P.barrier()
    for kc in range(KC):
        s = kc % 2
        P.dma("sp", xb[s], x1T[kc * 128:(kc + 1) * 128, :], r=["dram_x1"], w=[("xb", s)])
        P.op("dve", lambda e, s=s, kc=kc: e.tensor_scalar(out=xb[s], in0=xb[s], scalar1=modq[:, sc_c + kc:sc_c + kc + 1],
                                                          scalar2=modq[:, sh_c + kc:sh_c + kc + 1], op0=ALU.mult, op1=ALU.add),
             r=[("xb", s), "c_modq"], w=[("xb", s)])
        for q in range(2):
            b = C.bank()
            for j in range(4):
                tt = q * 4 + j
                P.op("pe", lambda e, b=b, j=j, tt=tt, s=s: e.transpose(C.ps[b][:, j * 128:(j + 1) * 128], xb[s][:, tt * 128:(tt + 1) * 128], ident),
                     r=[("xb", s), "c_ident"], w=[("ps", b)])
            P.op("act", lambda e, b=b, q=q, kc=kc: e.activation(out=hfft[:, q * 4:(q + 1) * 4, kc * 128:(kc + 1) * 128],
                                                               in_=C.ps[b][:, :].rearrange("p (a c) -> p a c", a=4), func=AF.Copy),
                 r=[("ps", b)], w=["hfft"])
    P.barrier()
    P.release()
    sel = P.alloc([NE, NE * 128], F32)
    P.dma("sp", sel, C.dram("c_sel", [NE, NE * 128]), r=["dram_sel"], w=["c_sel"])
    bgu = P.alloc([128, NE * 8], F32)
    P.dma("sp", bgu, W["bgu_p"], r=["dram_bgu"], w=["bgu"])
    iota = P.alloc([128, CAP], F32)
    P.dma("sp", iota, C.dram("c_iota", [128, CAP]), r=["dram_iota"], w=["iota"])
    slotid = P.alloc([128, 2], F32)
    P.dma("sp", slotid, C.dram("c_slotid", [128, 2]), r=["dram_slotid"], w=["slotid"])
    XeT = P.alloc([128, KC, CAP], BF16)
    wb = [P.alloc([128, KC, 256], BF16) for _ in range(2)]
    wdq = [P.alloc([128, 4, 1024], BF16) for _ in range(2)]
    Se = P.alloc([128, 8, CAP], BF16)
    pbc = P.alloc([128, NT], F32)
    stp = [P.alloc([128, NT], BF16) for _ in range(2)]
    tg = [P.alloc([128, CAP], F32) for _ in range(2)]
    tsg = [P.alloc([128, CAP], F32) for _ in range(2)]
    tu = [P.alloc([128, CAP], F32) for _ in range(2)]
    hTe = [P.alloc([128, 4, CAP], BF16) for _ in range(2)]
    yeo = [P.alloc([128, 512], BF16) for _ in range(4)]
    wi = 0
    wqi = 0
    ui = 0
    yi = 0
    si = 0
    for ex_i in range(NE):
        v = ex_i % 2
        for hb in range(2):
            b = C.bank()
            P.op("pe", lambda e, b=b, ex_i=ex_i, hb=hb: e.matmul(C.ps[b][:, :], lhsT=sel[0:NE, ex_i * 128:(ex_i + 1) * 128], rhs=PTk[0:NE, hb * 512:(hb + 1) * 512],
                                                                 start=True, stop=True), r=["PTk", "c_sel"], w=[("ps", b)])
            P.op("act", lambda e, b=b, hb=hb: e.activation(out=pbc[:, hb * 512:(hb + 1) * 512], in_=C.ps[b][:, :], func=AF.Copy), r=[("ps", b)], w=[("pbc", hb)])
        pbk = []
        for hb in range(2):
            b = C.bank()
            pbk.append(b)
            P.op("pe", lambda e, b=b, ex_i=ex_i, hb=hb: e.matmul(C.ps[b][:, :], lhsT=sel[0:NE, ex_i * 128:(ex_i + 1) * 128], rhs=POSMT[0:NE, hb * 512:(hb + 1) * 512],
                                                                 start=True, stop=True), r=["POSMT", "c_sel"], w=[("ps", b)])
        for st in range(2):
            u_ = si % 2
            si += 1
            for hb in range(2):
                P.op("dve", lambda e, b=pbk[hb], st=st, hb=hb, u_=u_: e.scalar_tensor_tensor(out=stp[u_][:, hb * 512:(hb + 1) * 512], in0=C.ps[b][:, :], scalar=slotid[:, st:st + 1],
                                                                                          in1=pbc[:, hb * 512:(hb + 1) * 512], op0=ALU.is_equal, op1=ALU.mult),
                     r=[("ps", pbk[hb]), ("pbc", hb), "slotid"], w=[("stp", u_)])
            row = (ex_i * 2 + st) * 128
            P.dma("sp", STP[row:row + 128, :], stp[u_], r=[("stp", u_)], w=["dram_STP"], semkey=("stpst", u_))
        for tt in range(8):
            P.op("dve", lambda e, tt=tt, ex_i=ex_i: e.tensor_scalar(out=Se[:, tt, :], in0=iota, scalar1=posm[:, tt, ex_i:ex_i + 1], scalar2=None, op0=ALU.is_equal),
                 r=["iota", ("posm", tt)], w=[("Se", tt)])
        sek = [("Se", tt) for tt in range(8)]
        for fcx in range(KC):
            b = C.bank()
            for tt in range(8):
                P.op("pe", lambda e, b=b, tt=tt, fcx=fcx: e.matmul(C.ps[b][:, 0:CAP], lhsT=hfft[:, tt, fcx * 128:(fcx + 1) * 128], rhs=Se[:, tt, :], start=(tt == 0), stop=(tt == 7)),
                     r=sek + ["hfft"], w=[("ps", b)])
            eng = "act" if fcx % 2 == 0 else "dve"
            if eng == "act":
                P.op("act", lambda e, b=b, fcx=fcx: e.activation(out=XeT[:, fcx, :], in_=C.ps[b][:, 0:CAP], func=AF.Copy), r=[("ps", b)], w=[("XeT", fcx)])
            else:
                P.op("dve", lambda e, b=b, fcx=fcx: e.tensor_copy(out=XeT[:, fcx, :], in_=C.ps[b][:, 0:CAP]), r=[("ps", b)], w=[("XeT", fcx)])
        for fc in range(4):
            s = wi % 2
            wi += 1
            P.dma("pool", wb[s], W["wgu_h"][ex_i].rearrange("(kc p) n -> p kc n", p=128)[:, :, fc * 256:(fc + 1) * 256], r=["dram_wgu"], w=[("wb", s)])
            bg_ = C.bank()
            bu_ = C.bank()
            for kc in range(KC):
                P.op("pe", lambda e, b=bg_, s=s, kc=kc: e.matmul(C.ps[b][:, 0:CAP], lhsT=wb[s][:, kc, 0:128], rhs=XeT[:, kc, :], start=(kc == 0), stop=(kc == KC - 1)),
                     r=[("wb", s), ("XeT", kc)], w=[("ps", bg_)])
            for kc in range(KC):
                P.op("pe", lambda e, b=bu_, s=s, kc=kc: e.matmul(C.ps[b][:, 0:CAP], lhsT=wb[s][:, kc, 128:256], rhs=XeT[:, kc, :], start=(kc == 0), stop=(kc == KC - 1)),
                     r=[("wb", s), ("XeT", kc)], w=[("ps", bu_)])
            u = ui % 2
            ui += 1
            cg = (ex_i * 4 + fc) * 2
            P.op("dve", lambda e, b=bg_, u=u, cg=cg: e.tensor_scalar(out=tg[u], in0=C.ps[b][:, 0:CAP], scalar1=bgu[:, cg:cg + 1], scalar2=7.0, op0=ALU.add, op1=ALU.min),
                 r=[("ps", bg_), "bgu"], w=[("tg", u)])
            P.op("act", lambda e, u=u: e.activation(out=tsg[u], in_=tg[u], func=AF.Sigmoid, scale=1.702), r=[("tg", u)], w=[("tsg", u)])
            P.op("dve", lambda e, b=bu_, u=u, cg=cg: e.tensor_scalar(out=tu[u], in0=C.ps[b][:, 0:CAP], scalar1=bgu[:, cg + 1:cg + 2], scalar2=7.0, op0=ALU.add, op1=ALU.min),
                 r=[("ps", bu_), "bgu"], w=[("tu", u)])
            P.op("dve", lambda e, u=u: e.tensor_scalar(out=tu[u], in0=tu[u], scalar1=-7.0, scalar2=1.0, op0=ALU.max, op1=ALU.add), r=[("tu", u)], w=[("tu", u)])
            P.op("dve", lambda e, u=u: e.tensor_tensor(out=tg[u], in0=tg[u], in1=tsg[u], op=ALU.mult), r=[("tg", u), ("tsg", u)], w=[("tg", u)])
            P.op("dve", lambda e, u=u, v=v, fc=fc: e.tensor_tensor(out=hTe[v][:, fc, :], in0=tg[u], in1=tu[u], op=ALU.mult), r=[("tg", u), ("tu", u)], w=[("hTe", v)])
        for qd in range(4):
            sq_ = wqi % 2
            wqi += 1
            P.dma("pool", wdq[sq_], W["w_down"][ex_i].rearrange("(fc p) n -> p fc n", p=128)[:, :, qd * 1024:(qd + 1) * 1024], r=["dram_wd"], w=[("wdq", sq_)])
            for st in range(2):
                for dgl in range(2):
                    b = C.bank()
                    for fc in range(4):
                        P.op("pe", lambda e, b=b, v=v, fc=fc, st=st, sq_=sq_, dgl=dgl: e.matmul(C.ps[b][:, :], lhsT=hTe[v][:, fc, st * 128:(st + 1) * 128],
                                                                                             rhs=wdq[sq_][:, fc, dgl * 512:(dgl + 1) * 512], start=(fc == 0), stop=(fc == 3)),
                             r=[("hTe", v), ("wdq", sq_)], w=[("ps", b)])
                    o = yi % 4
                    yi += 1
                    P.op("act", lambda e, b=b, o=o: e.activation(out=yeo[o], in_=C.ps[b][:, :], func=AF.Copy), r=[("ps", b)], w=[("yeo", o)])
                    row = (ex_i * 2 + st) * 128
                    col = qd * 1024 + dgl * 512
                    P.dma("sp", YE[row:row + 128, col:col + 512], yeo[o], r=[("yeo", o)], w=["dram_YE"], semkey=("yeost", o))
    P.barrier()
    P.release()
    P.mark()
    PT2 = P.alloc([128, NT], F32)
    P.op("dve", lambda e: e.tensor_copy(out=PT2[0:NE, :], in_=PTk[0:NE, :]), r=[], w=["PT2"])
    P.barrier()
    bd = P.alloc([128, D], F32)
    P.dma("sp", bd[0:NE, :], W["b_down"], r=["dram_bd"], w=["bd"])
    ye = [P.alloc([128, 512], BF16) for _ in range(4)]
    st_ = [P.alloc([128, NT], BF16) for _ in range(4)]
    xr = [P.alloc([128, 512], F32) for _ in range(4)]
    gq0 = base + 5 * 32
    NQ = NE * 2
    hi = 0
    xi = 0
    for dg in range(8):
        for q in range(NQ):
            s = hi % 4
            hi += 1
            P.dma("sp", ye[s], YE[q * 128:(q + 1) * 128, dg * 512:(dg + 1) * 512], r=["dram_YE"], w=[("ye", s)])
            P.dma("sp", st_[s], STP[q * 128:(q + 1) * 128, :], r=["dram_STP"], w=[("st", s)])
            for dc in range(4):
                for hb in range(2):
                    b = dc * 2 + hb
                    P.op("pe", lambda e, b=b, s=s, dc=dc, hb=hb, q=q: e.matmul(C.ps[b][:, :], lhsT=ye[s][:, dc * 128:(dc + 1) * 128], rhs=st_[s][:, hb * 512:(hb + 1) * 512],
                                                                              start=(q == 0), stop=False),
                         r=[("ye", s), ("st", s)], w=[("ps", b)])
        for dc in range(4):
            for hb in range(2):
                b = dc * 2 + hb
                col = dg * 512 + dc * 128
                P.op("pe", lambda e, b=b, col=col, hb=hb: e.matmul(C.ps[b][:, :], lhsT=bd[0:NE, col:col + 128], rhs=PT2[0:NE, hb * 512:(hb + 1) * 512], start=False, stop=True),
                     r=["bd", "PT2"], w=[("ps", b)])
                x = xi % 4
                xi += 1
                kcx = dg * 4 + dc
                P.dma("sp", xr[x], x1T[col:col + 128, hb * 512:(hb + 1) * 512], r=["dram_x1"], w=[("xr", x)])
                P.op("dve", lambda e, b=b, x=x, kcx=kcx: e.scalar_tensor_tensor(out=xr[x], in0=C.ps[b][:, :], scalar=modq[:, gq0 + kcx:gq0 + kcx + 1], in1=xr[x],
                                                                                op0=ALU.mult, op1=ALU.add),
                     r=[("ps", b), ("xr", x), "c_modq"], w=[("xr", x)])
                P.dma("sp", vT[col:col + 128, hb * 512:(hb + 1) * 512], xr[x], r=[("xr", x)], w=["dram_v"], semkey=("xrst", x))
    stage_end(C)
```
